# Optimizing a Trainium2 kernel written in Bass

```python
import jax, jax.numpy as jnp
from jax import lax
import numpy as np

D_MODEL = 1024
BATCH = 8
SEQ = 4096
DEPTH = 2

CTX_LEN = 256
GRID_W = 64
HEAD_DIM = 64
A_WIDTH = D_MODEL // 2
A_Q_HEADS = A_WIDTH // HEAD_DIM
A_KV_HEADS = A_Q_HEADS // 4
B_WIDTH = D_MODEL // 4
B_GROUPS = 4
B_GROUP_DIM = B_WIDTH // B_GROUPS
C_WIDTH = D_MODEL // 4
C_HEADS = C_WIDTH // HEAD_DIM
MIX_WIDTH = A_WIDTH + B_WIDTH + C_WIDTH
IN_WIDTH = A_WIDTH + 2 * A_KV_HEADS * HEAD_DIM + B_WIDTH + 3 * C_WIDTH
NA_WIN_R = 8
NA_WIN_C = 16
Q_BLOCK = 128
ROPE_THETA = 10000.0
N_EXPERTS = 16
EXPERT_FF = 2 * D_MODEL
CAPACITY_FACTOR = 2
EPS = 1e-6

kernel_name = 'hybrid_fourier_gqa_natten_ec_moe_dit'


def rms_norm(x, g):
    xf = x.astype(jnp.float32)
    y = xf * lax.rsqrt(jnp.mean(xf * xf, axis=-1, keepdims=True) + EPS)
    return (y * g.astype(jnp.float32)).astype(x.dtype)


def modulate(h, shift, scale):
    return h * (1 + scale) + shift


def split_heads(t, n_heads):
    b, n, _ = t.shape
    return t.reshape(b, n, n_heads, HEAD_DIM).transpose(0, 2, 1, 3)


def merge_heads(t):
    b, h, n, d = t.shape
    return t.transpose(0, 2, 1, 3).reshape(b, n, h * d)


def axial_rope_angles(n):
    t = jnp.arange(n, dtype=jnp.int32)
    row = (t // GRID_W).astype(jnp.float32)
    col = (t % GRID_W).astype(jnp.float32)
    n_freq = HEAD_DIM // 4
    inv = ROPE_THETA ** (-jnp.arange(n_freq, dtype=jnp.float32) / n_freq)
    ang = jnp.stack([row[:, None] * inv, col[:, None] * inv], axis=1)
    return jnp.cos(ang), jnp.sin(ang)


def apply_axial_rope(x, cos, sin):
    shp = x.shape
    xr = x.astype(jnp.float32).reshape(shp[:-1] + (2, 2, HEAD_DIM // 4))
    x1, x2 = xr[..., 0, :], xr[..., 1, :]
    out = jnp.stack([x1 * cos - x2 * sin, x1 * sin + x2 * cos], axis=-2)
    return out.reshape(shp).astype(x.dtype)


def gqa_dense(q, k, v):
    b, hq, n, d = q.shape
    hkv = k.shape[1]
    qg = q.reshape(b, hkv, hq // hkv, n, d)
    s = jnp.einsum('bkgnd,bkmd->bkgnm', qg, k, preferred_element_type=jnp.float32) * (d ** -0.5)
    p = jax.nn.softmax(s, axis=-1).astype(v.dtype)
    o = jnp.einsum('bkgnm,bkmd->bkgnd', p, v)
    return o.reshape(b, hq, n, d)


def gqa_blocked(q, k, v):
    b, hq, n, d = q.shape
    nb = n // Q_BLOCK
    qb = q.reshape(b, hq, nb, Q_BLOCK, d).transpose(2, 0, 1, 3, 4)
    out = lax.map(lambda qi: gqa_dense(qi, k, v), qb)
    return out.transpose(1, 2, 0, 3, 4).reshape(b, hq, n, d)


def fourier_mix(u):
    b, n, _ = u.shape
    ug = u.astype(jnp.float32).reshape(b, n, B_GROUPS, B_GROUP_DIM)
    f = jnp.fft.fft2(ug, axes=(1, 3), norm='ortho').real
    return f.reshape(b, n, B_WIDTH).astype(u.dtype)


def neighbourhood_attention(q, k, v, kc, vc, rel_bias):
    b, h, n, d = q.shape
    rows = n // GRID_W
    wr = min(NA_WIN_R, rows)
    wc = min(NA_WIN_C, GRID_W)
    rpb = Q_BLOCK // GRID_W
    nb = rows // rpb
    kg = k.reshape(b, h, rows, GRID_W, d)
    vg = v.reshape(b, h, rows, GRID_W, d)
    cols = jnp.arange(GRID_W)
    cs = jnp.clip(cols - wc // 2, 0, GRID_W - wc)
    col_idx = cs[:, None] + jnp.arange(wc)[None, :]
    col_off = col_idx - cols[:, None] + (NA_WIN_C - 1)
    qb = q.reshape(b, h, nb, rpb, GRID_W, d).transpose(2, 0, 1, 3, 4, 5)
    scale = d ** -0.5
    nwin = wr * wc

    def block(args):
        i, qi = args
        r = i * rpb + jnp.arange(rpb)
        rs = jnp.clip(r - wr // 2, 0, rows - wr)
        row_idx = rs[:, None] + jnp.arange(wr)[None, :]
        row_off = row_idx - r[:, None] + (NA_WIN_R - 1)
        k_win = jnp.take(jnp.take(kg, row_idx, axis=2), col_idx, axis=4)
        v_win = jnp.take(jnp.take(vg, row_idx, axis=2), col_idx, axis=4)
        s_win = jnp.einsum('bhrcd,bhricjd->bhrcij', qi, k_win, preferred_element_type=jnp.float32) * scale
        bias = rel_bias[:, row_off[:, None, :, None], col_off[None, :, None, :]]
        s_win = s_win + bias.astype(jnp.float32)[None]
        s_ctx = jnp.einsum('bhrcd,bhld->bhrcl', qi, kc, preferred_element_type=jnp.float32) * scale
        s = jnp.concatenate([s_win.reshape(b, h, rpb, GRID_W, nwin), s_ctx], axis=-1)
        p = jax.nn.softmax(s, axis=-1).astype(v.dtype)
        p_win = p[..., :nwin].reshape(b, h, rpb, GRID_W, wr, wc)
        p_ctx = p[..., nwin:]
        return (jnp.einsum('bhrcij,bhricjd->bhrcd', p_win, v_win)
                + jnp.einsum('bhrcl,bhld->bhrcd', p_ctx, vc))

    out = lax.map(block, (jnp.arange(nb), qb))
    return out.transpose(1, 2, 0, 3, 4, 5).reshape(b, h, n, d)


def merge_groups(o_a, o_b, o_c, g_out_a, g_out_b, g_out_c, w_out):
    y = jnp.concatenate([rms_norm(o_a, g_out_a), rms_norm(o_b, g_out_b), rms_norm(o_c, g_out_c)], axis=-1)
    return y @ w_out


def mixer_sublayer(hl, hc, w_in, g_q, g_k, rel_bias, g_out_a, g_out_b, g_out_c, w_out, cos, sin, ctx_out):
    kv = A_KV_HEADS * HEAD_DIM
    splits = np.cumsum([A_WIDTH, kv, kv, B_WIDTH, C_WIDTH, C_WIDTH]).tolist()
    qa, ka, va, ub, qn, kn, vn = jnp.split(hl @ w_in, splits, axis=-1)
    qa_c, ka_c, va_c, ub_c, qn_c, kn_c, vn_c = jnp.split(hc @ w_in, splits, axis=-1)
    ka_c = rms_norm(split_heads(ka_c, A_KV_HEADS), g_k)
    va_c = split_heads(va_c, A_KV_HEADS)
    kn_c = split_heads(kn_c, C_HEADS)
    vn_c = split_heads(vn_c, C_HEADS)
    qa = apply_axial_rope(rms_norm(split_heads(qa, A_Q_HEADS), g_q), cos, sin)
    ka = apply_axial_rope(rms_norm(split_heads(ka, A_KV_HEADS), g_k), cos, sin)
    va = split_heads(va, A_KV_HEADS)
    o_a = gqa_blocked(qa, jnp.concatenate([ka, ka_c], axis=2), jnp.concatenate([va, va_c], axis=2))
    o_b = fourier_mix(ub)
    o_c = neighbourhood_attention(split_heads(qn, C_HEADS), split_heads(kn, C_HEADS), split_heads(vn, C_HEADS),
                                  kn_c, vn_c, rel_bias)
    yl = merge_groups(merge_heads(o_a), o_b, merge_heads(o_c), g_out_a, g_out_b, g_out_c, w_out)
    if not ctx_out:
        return yl, None
    o_a_c = gqa_dense(rms_norm(split_heads(qa_c, A_Q_HEADS), g_q), ka_c, va_c)
    o_b_c = fourier_mix(ub_c)
    o_c_c = gqa_dense(split_heads(qn_c, C_HEADS), kn_c, vn_c)
    yc = merge_groups(merge_heads(o_a_c), o_b_c, merge_heads(o_c_c), g_out_a, g_out_b, g_out_c, w_out)
    return yl, yc


def expert_choice_moe(h, w_router, w_gate, w_up, w_down):
    b, n, d = h.shape
    cap = max(1, CAPACITY_FACTOR * n // N_EXPERTS)
    logits = jnp.einsum('bnd,de->ben', h, w_router, preferred_element_type=jnp.float32)
    aff = jax.nn.softmax(logits, axis=1)
    gate, idx = lax.top_k(aff, cap)
    xe = jax.vmap(lambda hb, ib: hb[ib])(h, idx)
    g = jnp.einsum('becd,edf->becf', xe, w_gate)
    u = jnp.einsum('becd,edf->becf', xe, w_up)
    y = jnp.einsum('becf,efd->becd', jax.nn.silu(g) * u, w_down) * gate[..., None].astype(h.dtype)

    def scatter(ib, yb):
        return jnp.zeros((n, d), h.dtype).at[ib.reshape(-1)].add(yb.reshape(-1, d))

    return jax.vmap(scatter)(idx, y)


def setup_inputs(seed: int = 0) -> dict:
    key = jax.random.key(seed)
    ks = jax.random.split(key, 24)
    D = D_MODEL
    nrm = jax.random.normal

    def gain(k, shape):
        return 1.0 + 0.02 * nrm(k, shape, jnp.float32)

    return {
        'x': nrm(ks[0], (BATCH, SEQ, D), jnp.float32),
        'c': nrm(ks[1], (BATCH, D), jnp.float32),
        'ctx': nrm(ks[2], (BATCH, CTX_LEN, D), jnp.float32),
        'c_ctx': nrm(ks[3], (D,), jnp.float32),
        'w_ada': nrm(ks[4], (DEPTH, D, 6 * D), jnp.float32) * D ** -0.5,
        'b_ada': 0.01 * nrm(ks[5], (DEPTH, 6 * D), jnp.float32),
        'g_mix': gain(ks[6], (DEPTH, D)),
        'g_ffn': gain(ks[7], (DEPTH, D)),
        'w_in': nrm(ks[8], (DEPTH, D, IN_WIDTH), jnp.float32) * D ** -0.5,
        'g_q': gain(ks[9], (DEPTH, HEAD_DIM)),
        'g_k': gain(ks[10], (DEPTH, HEAD_DIM)),
        'rel_bias': 0.1 * nrm(ks[11], (DEPTH, C_HEADS, 2 * NA_WIN_R - 1, 2 * NA_WIN_C - 1), jnp.float32),
        'g_out_a': gain(ks[12], (DEPTH, A_WIDTH)),
        'g_out_b': gain(ks[13], (DEPTH, B_WIDTH)),
        'g_out_c': gain(ks[14], (DEPTH, C_WIDTH)),
        'w_out': nrm(ks[15], (DEPTH, MIX_WIDTH, D), jnp.float32) * MIX_WIDTH ** -0.5,
        'w_router': nrm(ks[16], (DEPTH, D, N_EXPERTS), jnp.float32) * D ** -0.5,
        'w_gate': nrm(ks[17], (DEPTH, N_EXPERTS, D, EXPERT_FF), jnp.float32) * D ** -0.5,
        'w_up': nrm(ks[18], (DEPTH, N_EXPERTS, D, EXPERT_FF), jnp.float32) * D ** -0.5,
        'w_down': nrm(ks[19], (DEPTH, N_EXPERTS, EXPERT_FF, D), jnp.float32) * EXPERT_FF ** -0.5,
        'g_final': gain(ks[20], (D,)),
    }


def reference(x, c, ctx, c_ctx, w_ada, b_ada, g_mix, g_ffn, w_in, g_q, g_k, rel_bias, g_out_a, g_out_b,
              g_out_c, w_out, w_router, w_gate, w_up, w_down, g_final):
    n = x.shape[1]
    cos, sin = axial_rope_angles(n)
    sc = jax.nn.silu(c)
    sx = jax.nn.silu(c_ctx)
    xl, xc = x, ctx
    for l in range(DEPTH):
        last = l == DEPTH - 1
        ml = [m[:, None, :] for m in jnp.split(sc @ w_ada[l] + b_ada[l], 6, axis=-1)]
        mc = jnp.split(sx @ w_ada[l] + b_ada[l], 6, axis=-1)
        hl = modulate(rms_norm(xl, g_mix[l]), ml[0], ml[1])
        hc = modulate(rms_norm(xc, g_mix[l]), mc[0], mc[1])
        yl, yc = mixer_sublayer(hl, hc, w_in[l], g_q[l], g_k[l], rel_bias[l], g_out_a[l], g_out_b[l],
                                g_out_c[l], w_out[l], cos, sin, not last)
        xl = xl + ml[2] * yl
        hl = modulate(rms_norm(xl, g_ffn[l]), ml[3], ml[4])
        xl = xl + ml[5] * expert_choice_moe(hl, w_router[l], w_gate[l], w_up[l], w_down[l])
        if not last:
            xc = xc + mc[2] * yc
            hc = modulate(rms_norm(xc, g_ffn[l]), mc[3], mc[4])
            xc = xc + mc[5] * expert_choice_moe(hc, w_router[l], w_gate[l], w_up[l], w_down[l])
    return rms_norm(xl, g_final)
```

```python
from contextlib import ExitStack
import numpy as np
import ml_dtypes
import concourse.bass as bass
import concourse.mybir as mybir
from concourse.bass_utils import run_bass_kernel_spmd

F32 = mybir.dt.float32
BF16 = mybir.dt.bfloat16
I32 = mybir.dt.int32
ALU = mybir.AluOpType
ACT = mybir.ActivationFunctionType
AX = mybir.AxisListType

D = 1024
SEQ = 4096
CTX = 256
NTOK = SEQ + CTX
NT = NTOK // 128
DEPTH = 2
INW = 1792
NEXP = 16
FF = 2048
EPS = 1e-6


class Buf:
    def __init__(self, t):
        self.t = t
        self.w = None
        self.r = {}

    def __getitem__(self, idx):
        return self.t[idx]


class KB:
    NDS = {"sp": 16, "act": 8, "pool": 8}

    def __init__(self, nc, es):
        self.nc = nc
        self.es = es
        self.eng = dict(pe=nc.tensor, dve=nc.vector, act=nc.scalar, pool=nc.gpsimd, sp=nc.sync)
        self.csem = {e: es.enter_context(nc.semaphore("c_" + e)) for e in ("pe", "dve", "act", "pool")}
        self.ccnt = {e: 0 for e in self.csem}
        self.seen = {e: {} for e in self.eng}
        self.dsem = {q: [es.enter_context(nc.semaphore(f"d_{q}{i}")) for i in range(n)]
                     for q, n in self.NDS.items()}
        self.dcnt = {q: [0] * n for q, n in self.NDS.items()}
        self.dnext = {q: 0 for q in self.NDS}
        self.n_inst = 0

    def sbuf(self, st, name, shape, dtype):
        self.uid = getattr(self, "uid", 0) + 1
        return Buf(st.enter_context(self.nc.sbuf_tensor(f"{name}_{self.uid}", list(shape), dtype)))

    def psum(self, st, name, shape, dtype):
        self.uid = getattr(self, "uid", 0) + 1
        return Buf(st.enter_context(self.nc.psum_tensor(f"{name}_{self.uid}", list(shape), dtype)))

    def wait(self, eng, tok):
        sem, val = tok
        if self.seen[eng].get(sem, 0) >= val:
            return
        self.eng[eng].wait_ge(sem, val)
        self.seen[eng][sem] = val
        self.n_inst += 1

    def _deps(self, eng, reads, writes):
        toks = {}
        own = self.csem.get(eng)

        def add(tok, raw):
            if tok is None:
                return
            sem, val = tok
            if sem is own and eng == "pe":
                return
            if toks.get(sem, 0) < val:
                toks[sem] = val

        for b in reads:
            add(b.w, True)
        for b in writes:
            add(b.w, False)
            for sem, val in b.r.items():
                add((sem, val), False)
        for sem, val in toks.items():
            self.wait(eng, (sem, val))

    def _mark(self, tok, reads, writes):
        sem, val = tok
        for b in reads:
            if b.r.get(sem, 0) < val:
                b.r[sem] = val
        for b in writes:
            b.w = tok
            b.r = {}

    def op(self, eng, fn, reads=(), writes=(), inc=True):
        self._deps(eng, reads, writes)
        inst = fn()
        self.n_inst += 1
        if inc:
            self.ccnt[eng] += 1
            inst.then_inc(self.csem[eng], 1)
            tok = (self.csem[eng], self.ccnt[eng])
        else:
            tok = (self.csem[eng], self.ccnt[eng] + 1)
        self._mark(tok, reads, writes)
        return tok

    def dma(self, q, out, in_, reads=(), writes=(), fn=None, **kw):
        self._deps(q, reads, writes)
        i = self.dnext[q]
        self.dnext[q] = (i + 1) % len(self.dsem[q])
        sem = self.dsem[q][i]
        if self.dcnt[q][i] > 0:
            self.wait(q, (sem, self.dcnt[q][i]))
        if fn is None:
            inst = self.eng[q].dma_start(out=out, in_=in_, **kw)
        else:
            inst = fn()
        self.n_inst += 1
        self.dcnt[q][i] += 16
        inst.then_inc(sem, 16)
        tok = (sem, self.dcnt[q][i])
        self._mark(tok, reads, writes)
        return tok

    def barrier(self):
        toks = [(self.csem[e], self.ccnt[e]) for e in self.csem if self.ccnt[e] > 0]
        for q in self.dsem:
            for i, s in enumerate(self.dsem[q]):
                if self.dcnt[q][i] > 0:
                    toks.append((s, self.dcnt[q][i]))
        for e in self.eng:
            for t in toks:
                self.wait(e, t)

    def mm(self, out, lhsT, rhs, start, stop, reads, writes, inc=None, **kw):
        return self.op("pe", lambda: self.nc.tensor.matmul(out, lhsT=lhsT, rhs=rhs, start=start, stop=stop, **kw),
                       reads, writes, inc=(stop if inc is None else inc))

    def tr(self, out, in_, ident, reads, writes):
        return self.op("pe", lambda: self.nc.tensor.transpose(out, in_, ident), reads, writes)

    def actf(self, out, in_, func, reads, writes, **kw):
        return self.op("act", lambda: self.nc.scalar.activation(out=out, in_=in_, func=func, **kw), reads, writes)

    def cp(self, eng, out, in_, reads, writes):
        if eng == "act":
            return self.op("act", lambda: self.nc.scalar.copy(out=out, in_=in_), reads, writes)
        e = self.eng[eng]
        return self.op(eng, lambda: e.tensor_copy(out=out, in_=in_), reads, writes)

    def tt(self, eng, out, in0, in1, op, reads, writes):
        e = self.eng[eng]
        return self.op(eng, lambda: e.tensor_tensor(out=out, in0=in0, in1=in1, op=op), reads, writes)

    def ts(self, eng, out, in0, s1, s2, op0, op1, reads, writes, **kw):
        e = self.eng[eng]
        if s2 is None:
            return self.op(eng, lambda: e.tensor_scalar(out=out, in0=in0, scalar1=s1, scalar2=None, op0=op0, **kw),
                           reads, writes)
        return self.op(eng, lambda: e.tensor_scalar(out=out, in0=in0, scalar1=s1, scalar2=s2, op0=op0, op1=op1, **kw),
                       reads, writes)

    def stt(self, eng, out, in0, scalar, in1, op0, op1, reads, writes):
        e = self.eng[eng]
        return self.op(eng, lambda: e.scalar_tensor_tensor(out=out, in0=in0, scalar=scalar, in1=in1, op0=op0, op1=op1),
                       reads, writes)


def host_consts():
    c = {}
    c["ident_bf"] = np.eye(128, dtype=np.float32).astype(ml_dtypes.bfloat16)
    c["ident_f"] = np.eye(128, dtype=np.float32)
    t = np.arange(SEQ)
    row = (t // 64).astype(np.float64)
    col = (t % 64).astype(np.float64)
    inv = 10000.0 ** (-np.arange(16, dtype=np.float64) / 16)
    ang = np.stack([row[:, None] * inv, col[:, None] * inv], axis=1)
    c["rope_cos"] = np.cos(ang).reshape(SEQ, 32).astype(np.float32)
    c["rope_sin"] = np.sin(ang).reshape(SEQ, 32).astype(np.float32)
    j = np.arange(64)
    ph = 2 * np.pi * np.outer(j, j) / 64.0
    bd = np.zeros((256, 512), np.float64)
    for g in range(4):
        bd[g * 64:(g + 1) * 64, g * 64:(g + 1) * 64] = np.cos(ph)
        bd[g * 64:(g + 1) * 64, 256 + g * 64:256 + (g + 1) * 64] = np.sin(ph)
    c["chan_dft"] = bd.astype(np.float32).astype(ml_dtypes.bfloat16)

    def dft_tiles(n):
        nt = n // 128
        m = np.arange(n, dtype=np.int64)
        tab_c = np.cos(2 * np.pi * m / n)
        tab_s = -np.sin(2 * np.pi * m / n)
        kk = np.arange(n, dtype=np.int64).reshape(nt, 1, 1, 128)
        nn = (np.arange(nt, dtype=np.int64)[None, :] * 128 + np.arange(128, dtype=np.int64)[:, None]).reshape(1, 128, nt, 1)
        idx = (kk * nn) % n
        return (tab_c[idx].astype(np.float32).astype(ml_dtypes.bfloat16),
                tab_s[idx].astype(np.float32).astype(ml_dtypes.bfloat16))

    c["tri_f"] = np.triu(np.ones((128, 128), np.float32), 1)
    c["iota_slots"] = np.broadcast_to(np.arange(512, dtype=np.float32), (128, 512)).copy()
    p = np.arange(128)
    tk = np.zeros((128, NT, 2), np.float32)
    tk[:, :, 0] = 2 * np.arange(NT)[None, :] + (p // 64)[:, None]
    tk[:, :, 1] = (p % 64)[:, None]
    c["tokhl"] = tk
    c["dftc"], c["dfts"] = dft_tiles(SEQ)
    c["dftxc"], c["dftxs"] = dft_tiles(CTX)
    return c


_NA_IDX = None


def na_bias_layout(rel_bias):
    global _NA_IDX
    if _NA_IDX is None:
        variants = [(0, 0), (1, 0), (2, 0), (30, 27), (31, 27)]
        ro = np.zeros((5, 128, 5, 128), np.int64)
        co = np.zeros((5, 128, 5, 128), np.int64)
        ok = np.zeros((5, 128, 5, 128), bool)
        kl = np.arange(128).reshape(128, 1, 1)
        kj = np.arange(5).reshape(1, 5, 1)
        ql = np.arange(128).reshape(1, 1, 128)
        for v, (qi, s0) in enumerate(variants):
            r = 2 * qi + ql // 64
            cq = ql % 64
            kr = 2 * (s0 + kj) + kl // 64
            kc = kl % 64
            rs = np.clip(r - 4, 0, 56)
            cs = np.clip(cq - 8, 0, 48)
            inwin = (kr >= rs) & (kr < rs + 8) & (kc >= cs) & (kc < cs + 16)
            ok[v] = np.broadcast_to(inwin, (128, 5, 128))
            ro[v] = np.clip(np.broadcast_to(kr - r + 7, (128, 5, 128)), 0, 14)
            co[v] = np.clip(np.broadcast_to(kc - cq + 15, (128, 5, 128)), 0, 30)
        _NA_IDX = (ro, co, ok)
    ro, co, ok = _NA_IDX
    rb = np.asarray(rel_bias, np.float32)
    g = rb[:, :, ro, co]
    g = np.where(ok[None, None], g, np.float32(-30000.0))
    g = g.transpose(0, 2, 3, 4, 1, 5).reshape(rb.shape[0], 5, 128, 5, 512)
    return np.ascontiguousarray(g, dtype=np.float32)


def build_program(stop_after=None, debug=False, layers=DEPTH, skip=(), cqb=None, cvar=0, ext=(), outs=None):
    nc = bass.Bass("TRN2", target_bir_lowering=False)
    es = ExitStack()
    IN = {}
    SPEC = {
        "x": ([SEQ, D], F32),
        "c": ([D], F32),
        "ctx": ([CTX, D], F32),
        "c_ctx": ([D], F32),
        "w_ada": ([DEPTH, D, 6 * D], F32),
        "b_ada": ([DEPTH, 6 * D], F32),
        "g_mix": ([DEPTH, D], F32),
        "g_ffn": ([DEPTH, D], F32),
        "w_in": ([DEPTH, D, INW], F32),
        "g_q": ([DEPTH, 64], F32),
        "g_k": ([DEPTH, 64], F32),
        "g_out": ([DEPTH, D], F32),
        "w_out": ([DEPTH, D, D], F32),
        "w_router": ([DEPTH, D, NEXP], F32),
        "w_gate": ([DEPTH, NEXP, D, FF], F32),
        "w_up": ([DEPTH, NEXP, D, FF], F32),
        "w_down": ([DEPTH, NEXP, FF, D], F32),
        "g_final": ([D], F32),
        "ident_bf": ([128, 128], BF16),
        "ident_f": ([128, 128], F32),
        "rope_cos": ([SEQ, 32], F32),
        "rope_sin": ([SEQ, 32], F32),
        "chan_dft": ([256, 512], BF16),
        "dftc": ([32, 128, 32, 128], BF16),
        "dfts": ([32, 128, 32, 128], BF16),
        "dftxc": ([2, 128, 2, 128], BF16),
        "dftxs": ([2, 128, 2, 128], BF16),
        "na_bias": ([DEPTH, 5, 128, 5, 512], F32),
        "tri_f": ([128, 128], F32),
        "iota_slots": ([128, 512], F32),
        "tokhl": ([128, NT, 2], F32),
    }

    class _Lazy:
        def __getattr__(self, name):
            if name not in IN:
                shape, dt = SPEC[name]
                IN[name] = nc.dram_tensor(name, list(shape), dt, kind="ExternalInput").ap()
            return IN[name]

    I = _Lazy()
    nc._used_inputs = IN

    out_d = nc.dram_tensor("out", [SEQ, D], F32, kind="ExternalOutput").ap()

    skind = "ExternalOutput" if debug else "Internal"

    def dscr(name, shape, dtype):
        if name in ext:
            IN[name] = nc.dram_tensor(name, list(shape), dtype, kind="ExternalInput").ap()
            return IN[name]
        kind = skind if (outs is None or name in outs) else "Internal"
        return nc.dram_tensor(name, list(shape), dtype, kind=kind).ap()

    xres = dscr("xres", [NTOK, D], F32)
    modd = dscr("modd", [DEPTH, 2, 6 * D], F32)
    qat = dscr("qat", [128, NT, 4, 128], BF16)
    kat = dscr("kat", [128, NTOK], BF16)
    vav = dscr("vav", [128, NT, 384], BF16)
    ud = dscr("ud", [128, NT, 512], BF16)
    qnt = dscr("qnt", [64, 4, NTOK], BF16)
    knt = dscr("knt", [64, 4, NTOK], BF16)
    od = dscr("od", [NTOK, D], F32)
    hbf = dscr("hbf", [NTOK, D], BF16)
    xtd = dscr("xtd", [NEXP, 128, 8, 544], BF16)
    dbg_aff = dscr("dbg_aff", [128, NT, NEXP], F32) if debug else None
    dbg_x = dscr("dbg_x", [NTOK, D], F32) if debug else None
    dbg_pos = dscr("dbg_pos", [128, NT, NEXP], F32) if debug else None
    dbg_idx = dscr("dbg_idx", [128, NEXP, 5], I32) if debug else None
    dbg_gate = dscr("dbg_gate", [128, NEXP, 5], F32) if debug else None

    k = KB(nc, es)
    breg = nc.gpsimd.to_reg(NTOK - 1)

    ident_bf = k.sbuf(es, "ident_bf_s", [128, 128], BF16)
    ident_f = k.sbuf(es, "ident_f_s", [128, 128], F32)
    k.dma("sp", ident_bf[:], I.ident_bf, writes=[ident_bf])
    k.dma("sp", ident_f[:], I.ident_f, writes=[ident_f])

    for i in range(8 if "init" not in skip else 0):
        k.dma("sp", xres[i * 512:(i + 1) * 512, :].rearrange("(p a) d -> p (a d)", p=128),
              I.x[i * 512:(i + 1) * 512, :].rearrange("(p a) d -> p (a d)", p=128))
    if "init" not in skip:
        k.dma("sp", xres[SEQ:NTOK, :].rearrange("(p a) d -> p (a d)", p=128),
              I.ctx.rearrange("(p a) d -> p (a d)", p=128))

    def phase_ada():
        with ExitStack() as st:
            craw = k.sbuf(st, "craw", [128, 2, 8], F32)
            scT = k.sbuf(st, "scT", [128, 8, 2], F32)
            k.dma("sp", craw[:, 0, :], I.c.rearrange("(p c) -> p c", c=8), writes=[craw])
            k.dma("sp", craw[:, 1, :], I.c_ctx.rearrange("(p c) -> p c", c=8), writes=[craw])
            k.actf(scT[:].rearrange("p c r -> p r c"), craw[:], ACT.Silu, [craw], [scT])
            bias = k.sbuf(st, "adab", [2, DEPTH, 6 * D], F32)
            for l in range(DEPTH):
                k.dma("sp", bias[:, l, :], I.b_ada[l].partition_broadcast(2), writes=[bias])
            wts = [k.sbuf(st, f"adaw{i}", [128, 8, 512], F32) for i in range(2)]
            pss = [k.psum(st, f"adaps{i}", [2, 512], F32) for i in range(2)]
            mod = k.sbuf(st, "adamod", [2, DEPTH, 6 * D], F32)
            it = 0
            for l in range(DEPTH):
                wv = I.w_ada[l].rearrange("(p c) n -> p c n", c=8)
                for j in range(12):
                    wt = wts[it % 2]
                    ps = pss[it % 2]
                    it += 1
                    k.dma("sp" if it % 2 else "act", wt[:], wv[:, :, j * 512:(j + 1) * 512], writes=[wt])
                    for c8 in range(8):
                        k.mm(ps[:], scT[:, c8, :], wt[:, c8, :], c8 == 0, c8 == 7, [scT, wt], [ps])
                    k.tt("dve", mod[:, l, j * 512:(j + 1) * 512], ps[:], bias[:, l, j * 512:(j + 1) * 512], ALU.add,
                         [ps, bias], [mod])
                k.dma("sp", modd[l], mod[:, l, :], reads=[mod])
            k.barrier()

    if "ada" not in skip:
        phase_ada()
    if stop_after == "ada":
        return finish(nc, k, es)

    def load_bcast(st, name, src_ap, q="sp"):
        b = k.sbuf(st, name, [128, src_ap.shape[-1]], F32)
        k.dma(q, b[:], src_ap.partition_broadcast(128), writes=[b])
        return b

    def modvec(l, r, c):
        return modd[l, r, c * D:(c + 1) * D]

    def make_gm(st, name, l, r, c_scale, gvec):
        sc = load_bcast(st, name + "_s", modvec(l, r, c_scale))
        g = load_bcast(st, name + "_g", gvec)
        k.stt("dve", sc[:], sc[:], 1.0, g[:], ALU.add, ALU.mult, [sc, g], [sc])
        return sc

    def rms_rstd(xt, ss, rstd, junk, n, reads):
        k.actf(junk[:, 0:n], xt, ACT.Square, reads, [junk, ss], accum_out=ss[:, 0:1])
        k.actf(ss[:, 1:2], ss[:, 0:1], ACT.Sqrt, [ss, epsb], [ss], scale=1.0 / n, bias=epsb[:, 0:1])
        k.op("dve", lambda: nc.vector.reciprocal(out=rstd[:, 0:1], in_=ss[:, 1:2]), [ss], [rstd])

    epsb = k.sbuf(es, "epsb", [128, 1], F32)
    k.op("dve", lambda: nc.vector.memset(epsb[:], EPS), [], [epsb])

    def phase_proj(l):
        with ExitStack() as st:
            wsb = k.sbuf(st, "w_in_s", [128, 8, INW], BF16)
            k.dma("pool", wsb[:], I.w_in[l].rearrange("(c p) n -> p c n", p=128), writes=[wsb])
            cdft = k.sbuf(st, "cdft_s", [128, 2, 512], BF16)
            k.dma("sp", cdft[:], I.chan_dft.rearrange("(c p) n -> p c n", p=128), writes=[cdft])
            gm = [make_gm(st, f"gm1_{r}", l, r, 1, I.g_mix[l]) for r in range(2)]
            sh = [load_bcast(st, f"sh1_{r}", modvec(l, r, 0)) for r in range(2)]
            gq = load_bcast(st, "gq_b", I.g_q[l])
            gk = load_bcast(st, "gk_b", I.g_k[l])
            gqk = k.sbuf(st, "gqk", [128, 10, 64], F32)
            k.cp("dve", gqk[:, 0:8, :], gq[:].unsqueeze(1).broadcast_to([128, 8, 64]), [gq], [gqk])
            k.cp("dve", gqk[:, 8:10, :], gk[:].unsqueeze(1).broadcast_to([128, 2, 64]), [gk], [gqk])

            def dbl(name, shape, dt):
                return [k.sbuf(st, f"{name}{i}", shape, dt) for i in range(2)]

            xts = [k.sbuf(st, f"xt{i}", [128, D], F32) for i in range(4)]
            junk_ = dbl("junk", [128, D], BF16)
            ss_ = dbl("ss", [128, 2], F32)
            rstd_ = dbl("rstd", [128, 1], F32)
            hn_ = dbl("hn", [128, D], F32)
            hb_ = dbl("hb", [128, D], BF16)
            hT_ = dbl("hT", [128, 8, 512], BF16)
            hTv_ = [[Buf(h.t) for _ in range(4)] for h in hT_]
            tps_ = [k.psum(st, f"tps{i}", [128, 8, 128], BF16) for i in range(2)]
            pps_ = [k.psum(st, f"pps{i}", [128, 1024], F32) for i in range(2)]
            fps = [k.psum(st, f"fps{i}", [128, 512], F32) for i in range(2)]
            sq_ = dbl("sq", [128, 640], F32)
            ss10_ = dbl("ss10", [128, 2, 10], F32)
            qkn_ = dbl("qkn", [128, 10, 64], F32)
            rp_ = [[k.sbuf(st, f"rp{i}_{j}", [128, 10, 2, 16], F32) for i in range(4)] for j in range(2)]
            qkr_ = dbl("qkr", [128, 640], BF16)
            qst_ = dbl("qst", [128, 5, 128], BF16)
            vst_ = dbl("vst", [128, 384], BF16)
            cs = [k.sbuf(st, f"ropec{i}", [128, 2, 32], F32) for i in range(2)]
            ubT_ = dbl("ubT", [128, 2, 512], BF16)
            fst = [k.sbuf(st, f"fst{i}", [128, 512], BF16) for i in range(2)]
            ust_ = dbl("ust", [128, 512], BF16)
            fi = 0

            macros = [(m * 4, 4) for m in range(8)] + [(32, 2)]
            tiles = []
            for mi, (t0, ntl) in enumerate(macros):
                for tl in range(ntl):
                    tiles.append((mi, t0, ntl, tl))

            def setup(ti):
                mi, t0, ntl, tl = tiles[ti]
                t = t0 + tl
                pz = ti % 2
                return (mi, t0, ntl, tl, t, 0 if t < 32 else 1, pz, hT_[mi % 2], hTv_[mi % 2], xts[ti % 4], junk_[pz], ss_[pz],
                        rstd_[pz], hn_[pz], hb_[pz], tps_[pz], pps_[pz], sq_[pz], ss10_[pz], qkn_[pz], rp_[pz], qkr_[pz],
                        qst_[pz], vst_[pz], tps_[1 - pz])

            def stage_a(ti):
                (mi, t0, ntl, tl, t, r, pz, hT, hTv, xt, junk, ss, rstd, hn, hb, tps, pps, sq, ss10, qkn, rp, qkr, qst, vst,
                 qps) = setup(ti)
                for t2 in ([0, 1, 2] if ti == 0 else [ti + 2]):
                    if t2 < NT:
                        k.dma("pool", xts[t2 % 4][:], xres[t2 * 128:(t2 + 1) * 128, :], writes=[xts[t2 % 4]])
                rms_rstd(xt[:], ss, rstd, junk, D, [xt])
                k.stt("dve", hn[:], xt[:], rstd[:, 0:1], gm[r][:], ALU.mult, ALU.mult, [xt, rstd, gm[r]], [hn])
                k.tt("dve", hb[:], hn[:], sh[r][:], ALU.add, [hn, sh[r]], [hb])
                for dc in range(8):
                    k.tr(tps[:, dc, :], hb[:, dc * 128:(dc + 1) * 128], ident_bf[:], [hb, ident_bf], [tps])
                k.cp("act", hT[:, :, tl * 128:(tl + 1) * 128], tps[:], [tps], [hTv[tl]])
                for (c0, c1, o0) in ((0, 512, 0), (512, 768, 512), (1536, 1792, 768)):
                    for dc in range(8):
                        k.mm(pps[:, o0:o0 + (c1 - c0)], hT[:, dc, tl * 128:(tl + 1) * 128], wsb[:, dc, c0:c1],
                             dc == 0, dc == 7, [hTv[tl], wsb], [pps])
                k.cp("act", vst[:], pps[:, 640:1024], [pps], [vst])
                k.dma("sp", vav[:, t, :], vst[:], reads=[vst])
                k.actf(sq[:], pps[:, 0:640], ACT.Square, [pps], [sq])

            def stage_b(ti):
                (mi, t0, ntl, tl, t, r, pz, hT, hTv, xt, junk, ss, rstd, hn, hb, tps, pps, sq, ss10, qkn, rp, qkr, qst, vst,
                 qps) = setup(ti)
                k.op("dve", lambda: nc.vector.tensor_reduce(out=ss10[:, 0, :], in_=sq[:].rearrange("p (h d) -> p h d", d=64),
                                                            axis=AX.X, op=ALU.add), [sq], [ss10])
                k.actf(ss10[:, 1, :], ss10[:, 0, :], ACT.Sqrt, [ss10, epsb], [ss10], scale=1.0 / 64, bias=epsb[:, 0:1])
                k.op("dve", lambda: nc.vector.reciprocal(out=ss10[:, 0, :], in_=ss10[:, 1, :]), [ss10], [ss10])
                k.tt("dve", qkn[:], pps[:, 0:640].rearrange("p (h d) -> p h d", d=64),
                     ss10[:, 0, :].unsqueeze(2).broadcast_to([128, 10, 64]), ALU.mult, [pps, ss10], [qkn])
                qdst = qkr[:, 0:512].rearrange("p (hh g d) -> p g hh d", hh=4, g=2)
                kdst = qkr[:, 512:640].rearrange("p (g d) -> p g d", g=2)
                if r == 1:
                    k.tt("dve", qdst, qkn[:, 0:8, :].rearrange("p (g hh) d -> p g hh d", g=2),
                         gqk[:, 0:8, :].rearrange("p (g hh) d -> p g hh d", g=2), ALU.mult, [qkn, gqk], [qkr])
                    k.tt("dve", kdst, qkn[:, 8:10, :], gqk[:, 8:10, :], ALU.mult, [qkn, gqk], [qkr])
                else:
                    k.tt("dve", qkn[:], qkn[:], gqk[:], ALU.mult, [qkn, gqk], [qkn])
                    cst = cs[pz]
                    k.dma("act", cst[:, 0, :], I.rope_cos[t * 128:(t + 1) * 128, :], writes=[cst])
                    k.dma("act", cst[:, 1, :], I.rope_sin[t * 128:(t + 1) * 128, :], writes=[cst])
                    qv = qkn[:].rearrange("p h (a s f) -> p h a s f", a=2, s=2)
                    x1 = qv[:, :, :, 0, :]
                    x2 = qv[:, :, :, 1, :]
                    cosb = cst[:, 0, :].rearrange("p (a f) -> p a f", a=2).unsqueeze(1).broadcast_to([128, 10, 2, 16])
                    sinb = cst[:, 1, :].rearrange("p (a f) -> p a f", a=2).unsqueeze(1).broadcast_to([128, 10, 2, 16])
                    k.tt("dve", rp[0][:], x1, cosb, ALU.mult, [qkn, cst], [rp[0]])
                    k.tt("dve", rp[1][:], x2, sinb, ALU.mult, [qkn, cst], [rp[1]])
                    k.tt("dve", rp[2][:], x1, sinb, ALU.mult, [qkn, cst], [rp[2]])
                    k.tt("dve", rp[3][:], x2, cosb, ALU.mult, [qkn, cst], [rp[3]])
                    qd5 = qkr[:, 0:512].rearrange("p (hh g a s f) -> p g hh a s f", hh=4, g=2, a=2, s=2)
                    kd5 = qkr[:, 512:640].rearrange("p (g a s f) -> p g a s f", g=2, a=2, s=2)
                    for s_, (ra, rb, op_) in enumerate(((0, 1, ALU.subtract), (2, 3, ALU.add))):
                        for g in range(2):
                            k.tt("dve", qd5[:, g, :, :, s_, :], rp[ra][:, g * 4:(g + 1) * 4], rp[rb][:, g * 4:(g + 1) * 4],
                                 op_, [rp[ra], rp[rb]], [qkr])
                        k.tt("dve", kd5[:, :, :, s_, :], rp[ra][:, 8:10], rp[rb][:, 8:10], op_, [rp[ra], rp[rb]], [qkr])
                for j5 in range(5):
                    k.tr(qps[:, j5, :], qkr[:, j5 * 128:(j5 + 1) * 128], ident_bf[:], [qkr, ident_bf], [qps])
                k.cp("act", qst[:], qps[:, 0:5, :], [qps], [qst])
                k.dma("sp", qat[:, t, :, :], qst[:, 0:4, :], reads=[qst])
                k.dma("sp", kat[:, t * 128:(t + 1) * 128], qst[:, 4, :], reads=[qst])

            def stage_f(mi):
                t0, ntl = macros[mi]
                ncol = ntl * 128
                hT, hTv, ubT = hT_[mi % 2], hTv_[mi % 2], ubT_[mi % 2]
                for ci, c0 in enumerate((768, 896)):
                    fp = fps[ci % 2]
                    for dc in range(8):
                        k.mm(fp[:, 0:ncol], wsb[:, dc, c0:c0 + 128], hT[:, dc, 0:ncol], dc == 0, dc == 7, [wsb] + hTv[0:ntl], [fp])
                    k.cp("dve", ubT[:, ci, 0:ncol], fp[:, 0:ncol], [fp], [ubT])
                for ci in range(4):
                    c0 = (1024 if ci < 2 else 1280) + 128 * (ci % 2)
                    fp = fps[ci % 2]
                    for dc in range(8):
                        k.mm(fp[:, 0:ncol], wsb[:, dc, c0:c0 + 128], hT[:, dc, 0:ncol], dc == 0, dc == 7, [wsb] + hTv[0:ntl], [fp])
                    fs = fst[ci % 2]
                    k.cp("dve" if ci % 2 else "act", fs[:, 0:ncol], fp[:, 0:ncol], [fp], [fs])
                    dst = qnt if ci < 2 else knt
                    for hh in range(2):
                        k.dma("sp", dst[:, 2 * (ci % 2) + hh, t0 * 128:t0 * 128 + ncol], fs[hh * 64:(hh + 1) * 64, 0:ncol], reads=[fs])
                for tl in range(ntl):
                    t = t0 + tl
                    cps = fps[tl % 2]
                    ust = ust_[tl % 2]
                    for c2 in range(2):
                        k.mm(cps[:], ubT[:, c2, tl * 128:(tl + 1) * 128], cdft[:, c2, :], c2 == 0, c2 == 1, [ubT, cdft], [cps])
                    k.cp("act", ust[:], cps[:], [cps], [ust])
                    k.dma("sp", ud[:, t, :], ust[:], reads=[ust])

            stage_a(0)
            for ti in range(len(tiles)):
                if ti + 1 < len(tiles):
                    stage_a(ti + 1)
                stage_b(ti)
                mi, t0, ntl, tl = tiles[ti]
                if tl == ntl - 1:
                    stage_f(mi)
            k.barrier()

    def phase_attn_a(l, ctx_q):
        with ExitStack() as st:
            KaT = k.sbuf(st, "KaT", [128, NTOK], BF16)
            k.dma("sp", KaT[:], kat, writes=[KaT])
            vstg = k.sbuf(st, "vstg", [128, NT, 384], BF16)
            k.dma("sp", vstg[:], vav, writes=[vstg])
            Vs = k.sbuf(st, "Vs", [128, NT, 2, 65], BF16)
            k.op("dve", lambda: nc.vector.memset(Vs[:, :, :, 64:65], 1.0), [], [Vs])
            k.cp("dve", Vs[:, :, :, 0:64], vstg[:, :, 0:128].rearrange("p t (g d) -> p t g d", g=2), [vstg], [Vs])
            qts = [k.sbuf(st, f"qt{i}", [128, 512], BF16) for i in range(2)]
            sps = [[k.psum(st, f"sps{i}_{g}", [128, 512], F32) for g in range(2)] for i in range(2)]
            ops_ = [[k.psum(st, f"ops{i}_{g}", [128, 4, 65], F32) for g in range(2)] for i in range(2)]
            pts = [[k.sbuf(st, f"pt{i}_{g}", [128, 512], BF16) for g in range(2)] for i in range(3)]
            rec = k.sbuf(st, "rec", [128, 4], F32)
            osts = [k.sbuf(st, f"ost{i}", [128, 4, 64], F32) for i in range(2)]
            iters = []
            nqb = NT if ctx_q else 32
            for qb in range(nqb):
                kts = list(range(NT)) if qb < 32 else [32, 33]
                for i, kt in enumerate(kts):
                    iters.append((qb, i, kt, len(kts)))

            def stage_qk(j):
                qb, i, kt, n = iters[j]
                qt = qts[qb % 2]
                if i == 0:
                    for q2 in ([qb, qb + 1] if qb == 0 else [qb + 1]):
                        if q2 < nqb:
                            k.dma("sp", qts[q2 % 2][:], qat[:, q2, :, :].rearrange("p h n -> p (h n)"), writes=[qts[q2 % 2]])
                for g in range(2):
                    k.mm(sps[j % 2][g][:], KaT[g * 64:(g + 1) * 64, kt * 128:(kt + 1) * 128], qt[g * 64:(g + 1) * 64, :],
                         True, True, [KaT, qt], [sps[j % 2][g]])
                for g in range(2):
                    k.actf(pts[j % 3][g][:], sps[j % 2][g][:], ACT.Exp, [sps[j % 2][g]], [pts[j % 3][g]], scale=0.125)

            def stage_pv(j):
                qb, i, kt, n = iters[j]
                for g in range(2):
                    pt = pts[j % 3][g]
                    op_ = ops_[qb % 2][g]
                    for hh in range(4):
                        k.mm(op_[:, hh, :], pt[:, hh * 128:(hh + 1) * 128], Vs[:, kt, g, :],
                             i == 0 and hh == 0, i == n - 1, [pt, Vs], [op_], skip_group_check=True)
                if i == n - 1:
                    for g in range(2):
                        op_ = ops_[qb % 2][g]
                        ost = osts[g]
                        k.op("dve", lambda: nc.vector.reciprocal(out=rec[:], in_=op_[:, :, 64]), [op_], [rec])
                        k.tt("dve", ost[:], op_[:, :, 0:64], rec[:].unsqueeze(2).broadcast_to([128, 4, 64]), ALU.mult,
                             [op_, rec], [ost])
                        k.dma("sp", od[qb * 128:(qb + 1) * 128, g * 256:(g + 1) * 256], ost[:].rearrange("p h d -> p (h d)"),
                              reads=[ost])

            stage_qk(0)
            for j in range(len(iters)):
                if j + 1 < len(iters):
                    stage_qk(j + 1)
                stage_pv(j)
            k.barrier()

    def phase_fourier(l, ctx_q):
        with ExitStack() as st:
            U = k.sbuf(st, "Ures", [128, NT, 512], BF16)
            k.dma("sp", U[:], ud, writes=[U])
            cb = [k.sbuf(st, f"dftcb{i}", [128, 32, 128], BF16) for i in range(3)]
            sb = [k.sbuf(st, f"dftsb{i}", [128, 32, 128], BF16) for i in range(3)]
            fps_ = [k.psum(st, f"fops{i}", [128, 256], F32) for i in range(2)]
            fos = [k.sbuf(st, f"fos{i}", [128, 256], F32) for i in range(2)]
            jobs = [(kt, 32, 0, I.dftc[kt], I.dfts[kt], 1.0 / 512) for kt in range(32)]
            if ctx_q:
                jobs += [(32 + kt, 2, 32, I.dftxc[kt], I.dftxs[kt], 1.0 / 128) for kt in range(2)]
            for ji, (ot, nnt, t0, cd, sd, scl) in enumerate(jobs):
                c_, s_ = cb[ji % 3], sb[ji % 3]
                k.dma("sp", c_[:, 0:nnt, :], cd, writes=[c_])
                k.dma("pool", s_[:, 0:nnt, :], sd, writes=[s_])
                fp = fps_[ji % 2]
                for nt_ in range(nnt):
                    k.mm(fp[:], c_[:, nt_, :], U[:, t0 + nt_, 0:256], nt_ == 0, False, [c_, U], [fp])
                    k.mm(fp[:], s_[:, nt_, :], U[:, t0 + nt_, 256:512], False, nt_ == nnt - 1, [s_, U], [fp])
                fo = fos[ji % 2]
                k.actf(fo[:], fp[:], ACT.Copy, [fp], [fo], scale=scl)
                k.dma("sp", od[ot * 128:(ot + 1) * 128, 512:768], fo[:], reads=[fo])
            k.barrier()

    def phase_attn_c(l, ctx_q):
        with ExitStack() as st:
            import os
            CF = os.environ.get("CF", "kqvmcn")
            KnT = k.sbuf(st, "KnT", [64, 4, NTOK], BF16)
            QnT = k.sbuf(st, "QnT", [64, 4, NTOK], BF16)
            if "k" in CF:
                k.dma("sp", KnT[:], knt, writes=[KnT])
            if "q" in CF:
                k.dma("act", QnT[:], qnt, writes=[QnT])
            vstg = k.sbuf(st, "vstg", [128, NT, 384], BF16)
            if "v" in CF:
                k.dma("sp", vstg[:], vav, writes=[vstg])
            Vn = k.sbuf(st, "Vn", [128, NT, 4, 65], BF16)
            if "m" in CF:
                k.op("dve", lambda: nc.vector.memset(Vn[:, :, :, 64:65], 1.0), [], [Vn])
            if "c" in CF:
                k.cp("dve", Vn[:, :, :, 0:64], vstg[:, :, 128:384].rearrange("p t (h d) -> p t h d", h=4), [vstg], [Vn])
            nab_i = k.sbuf(st, "nab_i", [128, 5, 512], F32)
            nab_s = k.sbuf(st, "nab_s", [128, 5, 512], F32)
            for j in range(5):
                k.dma("sp", nab_i[:, j, :], I.na_bias[l, 2, :, j, :], writes=[nab_i])
            sps = [k.psum(st, f"csps{i}", [128, 4, 128], F32) for i in range(2)]
            ops_ = [k.psum(st, f"cops{i}", [128, 4, 65], F32) for i in range(2)]
            scs = [k.sbuf(st, f"csc{i}", [128, 512], F32) for i in range(2)]
            pts = [k.sbuf(st, f"cpt{i}", [128, 512], BF16) for i in range(3)]
            rec = k.sbuf(st, "crec", [128, 4], F32)
            osts = [k.sbuf(st, f"cost{i}", [128, 4, 64], F32) for i in range(2)]
            iters = []
            for qb in (cqb if cqb is not None else range(NT if ctx_q else 32)):
                if qb < 32:
                    s0 = min(max(qb - 2, 0), 27)
                    var = {0: 0, 1: 1, 30: 3, 31: 4}.get(qb, 2)
                    kts = [(s0 + j, j) for j in range(5)] + [(32, None), (33, None)]
                else:
                    var = 2
                    kts = [(32, None), (33, None)]
                for i, (kt, bj) in enumerate(kts):
                    iters.append((qb, i, kt, bj, var, len(kts)))

            def stage_qk(j):
                qb, i, kt, bj, var, n = iters[j]
                sp_, pt, sc = sps[j % 2], pts[j % 3], scs[j % 2]
                nab = nab_i
                if var != 2:
                    nab = nab_s
                    if i == 0:
                        for jj in range(5):
                            k.dma("sp", nab_s[:, jj, :], I.na_bias[l, var, :, jj, :], writes=[nab_s])
                for h in range(4):
                    k.mm(sp_[:, h, :], KnT[:, h, kt * 128:(kt + 1) * 128],
                         QnT[:, h, qb * 128:(qb + 1) * 128], True, True, [KnT, QnT], [sp_])
                spf = sp_[:].rearrange("p h n -> p (h n)")
                if bj is None:
                    k.actf(pt[:], spf, ACT.Exp, [sp_], [pt], scale=0.125)
                else:
                    k.stt("dve", sc[:], spf, 0.125, nab[:, bj, :], ALU.mult, ALU.add, [sp_, nab], [sc])
                    k.actf(pt[:], sc[:], ACT.Exp, [sc], [pt])

            def stage_pv(j):
                qb, i, kt, bj, var, n = iters[j]
                pt = pts[j % 3]
                op_ = ops_[qb % 2]
                for h in range(4):
                    k.mm(op_[:, h, :], pt[:, h * 128:(h + 1) * 128], Vn[:, kt, h, :],
                         i == 0 and h == 0, i == n - 1, [pt, Vn], [op_], skip_group_check=True)
                if i == n - 1:
                    ost = osts[qb % 2]
                    k.op("dve", lambda: nc.vector.reciprocal(out=rec[:], in_=op_[:, :, 64]), [op_], [rec])
                    k.tt("dve", ost[:], op_[:, :, 0:64], rec[:].unsqueeze(2).broadcast_to([128, 4, 64]), ALU.mult,
                         [op_, rec], [ost])
                    k.dma("sp", od[qb * 128:(qb + 1) * 128, 768:1024], ost[:].rearrange("p h d -> p (h d)"), reads=[ost])

            if iters:
                stage_qk(0)
            for j in range(len(iters)):
                if j + 1 < len(iters):
                    stage_qk(j + 1)
                stage_pv(j)
            k.barrier()

    aff_all = k.sbuf(es, "aff_all", [128, NT, NEXP], F32)

    def phase_merge(l, ntiles):
        with ExitStack() as st:
            wo = k.sbuf(st, "w_out_s", [128, 8, D], BF16)
            k.dma("pool", wo[:], I.w_out[l].rearrange("(c p) n -> p c n", p=128), writes=[wo])
            wr = k.sbuf(st, "w_r_s", [128, 8, NEXP], F32)
            k.dma("sp", wr[:], I.w_router[l].rearrange("(c p) e -> p c e", p=128), writes=[wr])
            gout = load_bcast(st, "gout_b", I.g_out[l])
            nr = 2 if ntiles > 32 else 1
            gate2 = [load_bcast(st, f"gate2_{r}", modvec(l, r, 2)) for r in range(nr)]
            gm4 = [make_gm(st, f"gm4_{r}", l, r, 4, I.g_ffn[l]) for r in range(nr)]
            sh4 = [load_bcast(st, f"sh4_{r}", modvec(l, r, 3)) for r in range(nr)]
            inv3 = k.sbuf(st, "inv3", [128, 3], F32)
            k.op("dve", lambda: nc.vector.memset(inv3[:, 0:1], 1.0 / 512), [], [inv3])
            k.op("dve", lambda: nc.vector.memset(inv3[:, 1:3], 1.0 / 256), [], [inv3])
            ots = [k.sbuf(st, f"ot{i}", [128, D], F32) for i in range(4)]
            xts = [k.sbuf(st, f"mxt{i}", [128, D], F32) for i in range(4)]
            def dbl(name, shape, dt):
                return [k.sbuf(st, f"{name}{i}", shape, dt) for i in range(2)]

            junk_ = dbl("mjunk", [128, D], BF16)
            ss3_ = dbl("ss3", [128, 3, 3], F32)
            yb_ = dbl("yb", [128, D], BF16)
            yT_ = dbl("yT", [128, 8, 128], BF16)
            t1_ = dbl("t1", [128, D], F32)
            xn_ = dbl("xn", [128, D], F32)
            ss_ = dbl("mss", [128, 2], F32)
            rstd_ = dbl("mrstd", [128, 1], F32)
            hn2_ = dbl("hn2", [128, D], F32)
            h2f_ = dbl("h2f", [128, D], F32)
            h2b_ = dbl("h2b", [128, D], BF16)
            h2T_ = dbl("h2T", [128, 8, 128], F32)
            sm_ = dbl("smx", [128, 4], F32)
            ex_ = dbl("sex", [128, NEXP], F32)
            tps_ = [k.psum(st, f"mtps{i}", [128, 8, 128], BF16) for i in range(2)]
            pps_ = [k.psum(st, f"mpps{i}", [128, D], F32) for i in range(2)]
            rtp = k.psum(st, "rtp", [128, 4, 128], F32)
            lps1 = k.psum(st, "lps", [128, NEXP], F32)
            lps_ = [lps1, lps1]
            grp = ((0, 512), (512, 768), (768, 1024))
            def setup(t):
                pz = t % 2
                return (0 if t < 32 else 1, ots[t % 4], xts[t % 4], junk_[pz], ss3_[pz], yb_[pz], yT_[pz], t1_[pz], xn_[pz], ss_[pz],
                        rstd_[pz], hn2_[pz], h2f_[pz], h2b_[pz], h2T_[pz], sm_[pz], ex_[pz], tps_[pz], lps_[pz], pps_[pz])

            def stage_a(t):
                (r, ot, xt, junk, ss3, yb, yT, t1, xn, ss, rstd, hn2, h2f, h2b, h2T, sm, ex, tps, lps, pps) = setup(t)
                for t2 in ([0, 1, 2] if t == 0 else [t + 2]):
                    if t2 < ntiles:
                        k.dma("pool", ots[t2 % 4][:], od[t2 * 128:(t2 + 1) * 128, :], writes=[ots[t2 % 4]])
                        k.dma("pool", xts[t2 % 4][:], xres[t2 * 128:(t2 + 1) * 128, :], writes=[xts[t2 % 4]])
                for gi, (a, b) in enumerate(grp):
                    k.actf(junk[:, a:b], ot[:, a:b], ACT.Square, [ot], [junk, ss3], accum_out=ss3[:, 0, gi:gi + 1])
                k.tt("dve", ss3[:, 1, :], ss3[:, 0, :], inv3[:], ALU.mult, [ss3, inv3], [ss3])
                k.actf(ss3[:, 2, :], ss3[:, 1, :], ACT.Sqrt, [ss3, epsb], [ss3], bias=epsb[:, 0:1])
                k.op("dve", lambda: nc.vector.reciprocal(out=ss3[:, 0, :], in_=ss3[:, 2, :]), [ss3], [ss3])
                for gi, (a, b) in enumerate(grp):
                    k.stt("dve", yb[:, a:b], ot[:, a:b], ss3[:, 0, gi:gi + 1], gout[:, a:b],
                          ALU.mult, ALU.mult, [ot, ss3, gout], [yb])
                for dc in range(8):
                    k.tr(tps[:, dc, :], yb[:, dc * 128:(dc + 1) * 128], ident_bf[:], [yb, ident_bf], [tps])
                k.cp("act", yT[:], tps[:], [tps], [yT])
                for hf in range(2):
                    for dc in range(8):
                        k.mm(pps[:, hf * 512:(hf + 1) * 512], yT[:, dc, :], wo[:, dc, hf * 512:(hf + 1) * 512],
                             dc == 0, dc == 7, [yT, wo], [pps])

            def stage_b(t):
                (r, ot, xt, junk, ss3, yb, yT, t1, xn, ss, rstd, hn2, h2f, h2b, h2T, sm, ex, tps, lps, pps) = setup(t)
                k.tt("dve", t1[:], pps[:], gate2[r][:], ALU.mult, [pps, gate2[r]], [t1])
                k.tt("dve", xn[:], t1[:], xt[:], ALU.add, [t1, xt], [xn])
                k.dma("sp", xres[t * 128:(t + 1) * 128, :], xn[:], reads=[xn])
                rms_rstd(xn[:], ss, rstd, junk, D, [xn])
                k.stt("dve", hn2[:], xn[:], rstd[:, 0:1], gm4[r][:], ALU.mult, ALU.mult, [xn, rstd, gm4[r]], [hn2])
                k.tt("dve", h2f[:], hn2[:], sh4[r][:], ALU.add, [hn2, sh4[r]], [h2f])
                k.cp("act", h2b[:], h2f[:], [h2f], [h2b])
                k.dma("sp", hbf[t * 128:(t + 1) * 128, :], h2b[:], reads=[h2b])
                for hp in range(2):
                    for dc in range(4):
                        k.tr(rtp[:, dc, :], h2f[:, (hp * 4 + dc) * 128:(hp * 4 + dc + 1) * 128], ident_f[:], [h2f, ident_f], [rtp])
                    k.cp("dve" if hp else "act", h2T[:, hp * 4:(hp + 1) * 4, :], rtp[:], [rtp], [h2T])
                for dc in range(8):
                    k.mm(lps[:], h2T[:, dc, :], wr[:, dc, :], dc == 0, dc == 7, [h2T, wr], [lps])
                k.op("dve", lambda: nc.vector.reduce_max(out=sm[:, 0:1], in_=lps[:], axis=AX.X), [lps], [sm])
                k.ts("dve", sm[:, 1:2], sm[:, 0:1], -1.0, None, ALU.mult, None, [sm], [sm])
                k.actf(ex[:], lps[:], ACT.Exp, [lps, sm], [ex, sm], bias=sm[:, 1:2], accum_out=sm[:, 2:3])
                k.op("dve", lambda: nc.vector.reciprocal(out=sm[:, 3:4], in_=sm[:, 2:3]), [sm], [sm])
                k.ts("dve", aff_all[:, t, :], ex[:], sm[:, 3:4], None, ALU.mult, None, [ex, sm], [aff_all])

            stage_a(0)
            for t in range(ntiles):
                if t + 1 < ntiles:
                    stage_a(t + 1)
                stage_b(t)
            k.barrier()

    sel_all = k.sbuf(es, "sel_all", [128, NT, NEXP], F32)
    posm_all = k.sbuf(es, "posm_all", [128, NT, NEXP], F32)
    nv = k.sbuf(es, "nv", [128, NT, NEXP, 5], BF16)
    idx_all = k.sbuf(es, "idx_all", [128, NEXP, 5], I32)
    gate_all = k.sbuf(es, "gate_all", [128, NEXP, 5], F32)
    k.op("dve", lambda: nc.vector.memset(idx_all[:], 0), [], [idx_all])
    k.op("dve", lambda: nc.vector.memset(gate_all[:], 0.0), [], [gate_all])
    LAT = (0, 32, 512, 0)
    CTXS = (32, 2, 32, 4)

    def phase_route(l, sets):
        with ExitStack() as st:
            ones_bf = k.sbuf(st, "ones_bf", [128, 128], BF16)
            k.op("dve", lambda: nc.vector.memset(ones_bf[:], 1.0), [], [ones_bf])
            ones_f = k.sbuf(st, "ones_f", [128, 128], F32)
            k.op("dve", lambda: nc.vector.memset(ones_f[:], 1.0), [], [ones_f])
            tri = k.sbuf(st, "tri_s", [128, 128], F32)
            k.dma("sp", tri[:], I.tri_f, writes=[tri])
            tok = k.sbuf(st, "tok_s", [128, NT, 2], F32)
            k.dma("sp", tok[:], I.tokhl, writes=[tok])
            cum = k.sbuf(st, "r_cum", [128, NEXP], F32)
            pps_ = [k.psum(st, f"r_pps{i}", [128, NEXP], F32) for i in range(2)]
            r1 = k.sbuf(st, "r_r1", [128, NT, NEXP], F32)
            r2 = k.sbuf(st, "r_r2", [128, NT, NEXP], F32)
            chains = []
            for (t0, ntl, cap, _c) in sets:
                for (e0, ne) in (((0, 8), (8, 8)) if ntl > 8 else ((0, NEXP),)):
                    ci = len(chains)
                    c = dict(t0=t0, ntl=ntl, cap=cap, e0=e0, ne=ne)
                    for nm in ("lo", "hi", "sm", "mid", "cnt", "t", "u"):
                        c[nm] = k.sbuf(st, f"r_{nm}{ci}", [128, ne], F32)
                    c["cmp"] = k.sbuf(st, f"r_cmp{ci}", [128, ntl, ne], BF16)
                    c["cps"] = k.psum(st, f"r_cps{ci}", [128, 512], F32)
                    chains.append(c)
            for c in chains:
                k.op("dve", lambda: nc.vector.memset(c["lo"][:], 0.0), [], [c["lo"]])
                k.op("dve", lambda: nc.vector.memset(c["hi"][:], 1.0), [], [c["hi"]])

            def affs_of(c):
                return aff_all[:, c["t0"]:c["t0"] + c["ntl"], c["e0"]:c["e0"] + c["ne"]]

            for it in range(32):
                for c in chains:
                    k.tt("dve", c["sm"][:], c["lo"][:], c["hi"][:], ALU.add, [c["lo"], c["hi"]], [c["sm"]])
                for c in chains:
                    k.ts("dve", c["mid"][:], c["sm"][:], 0.5, None, ALU.mult, None, [c["sm"]], [c["mid"]])
                for c in chains:
                    k.tt("dve", c["cmp"][:], affs_of(c), c["mid"][:].unsqueeze(1).broadcast_to([128, c["ntl"], c["ne"]]),
                         ALU.is_ge, [aff_all, c["mid"]], [c["cmp"]])
                for c in chains:
                    n_ = c["ntl"] * c["ne"]
                    k.mm(c["cps"][:, 0:n_], ones_bf[:], c["cmp"][:].rearrange("p t e -> p (t e)"), True, True,
                         [ones_bf, c["cmp"]], [c["cps"]])
                for c in chains:
                    n_ = c["ntl"] * c["ne"]
                    k.op("dve", lambda: nc.vector.tensor_reduce(
                        out=c["cnt"][:], in_=c["cps"][:, 0:n_].rearrange("p (t e) -> p e t", e=c["ne"]), axis=AX.X, op=ALU.add),
                        [c["cps"]], [c["cnt"]])
                for c in chains:
                    k.stt("dve", c["t"][:], c["cnt"][:], float(c["cap"]), c["mid"][:], ALU.is_ge, ALU.mult, [c["cnt"], c["mid"]], [c["t"]])
                for c in chains:
                    k.stt("dve", c["u"][:], c["cnt"][:], float(c["cap"]), c["mid"][:], ALU.is_ge, ALU.max, [c["cnt"], c["mid"]], [c["u"]])
                for c in chains:
                    k.tt("dve", c["lo"][:], c["lo"][:], c["t"][:], ALU.max, [c["lo"], c["t"]], [c["lo"]])
                for c in chains:
                    k.tt("dve", c["hi"][:], c["hi"][:], c["u"][:], ALU.min, [c["hi"], c["u"]], [c["hi"]])
            for c in chains:
                k.tt("dve", sel_all[:, c["t0"]:c["t0"] + c["ntl"], c["e0"]:c["e0"] + c["ne"]], affs_of(c),
                     c["lo"][:].unsqueeze(1).broadcast_to([128, c["ntl"], c["ne"]]), ALU.is_ge, [aff_all, c["lo"]], [sel_all])
            for (t0, ntl, cap, _c) in sets:
                k.op("dve", lambda: nc.vector.memset(cum[:], 0.0), [], [cum])
                for ti in range(ntl):
                    t = t0 + ti
                    pp = pps_[ti % 2]
                    k.mm(pp[:], tri[:], sel_all[:, t, :], True, False, [tri, sel_all], [pp])
                    k.mm(pp[:], ones_f[:], cum[:], False, True, [ones_f, cum], [pp])
                    k.stt("dve", posm_all[:, t, :], pp[:], 1.0, sel_all[:, t, :], ALU.add, ALU.mult, [pp, sel_all], [posm_all])
                    k.tt("dve", cum[:], cum[:], sel_all[:, t, :], ALU.add, [cum, sel_all], [cum])
                k.ts("dve", posm_all[:, t0:t0 + ntl, :], posm_all[:, t0:t0 + ntl, :], -1.0, None, ALU.add, None,
                     [posm_all], [posm_all])
            tl = NT if len(sets) > 1 else 32
            k.cp("dve", nv[:, 0:tl, :, 0:2], tok[:, 0:tl, :].unsqueeze(2).broadcast_to([128, tl, NEXP, 2]), [tok], [nv])
            k.cp("dve", nv[:, 0:tl, :, 2], aff_all[:, 0:tl, :], [aff_all], [nv])
            k.tt("dve", r1[:, 0:tl, :], aff_all[:, 0:tl, :], nv[:, 0:tl, :, 2], ALU.subtract, [aff_all, nv], [r1])
            k.cp("dve", nv[:, 0:tl, :, 3], r1[:, 0:tl, :], [r1], [nv])
            k.tt("dve", r2[:, 0:tl, :], r1[:, 0:tl, :], nv[:, 0:tl, :, 3], ALU.subtract, [r1, nv], [r2])
            k.cp("dve", nv[:, 0:tl, :, 4], r2[:, 0:tl, :], [r2], [nv])
            k.barrier()

    def phase_dispatch(l, sets):
        with ExitStack() as st:
            iota = k.sbuf(st, "iota_s", [128, 512], F32)
            k.dma("sp", iota[:], I.iota_slots, writes=[iota])
            ohs = [k.sbuf(st, f"oh{i}", [128, 512], BF16) for i in range(4)]
            igp = [k.psum(st, f"igp{i}", [128, 4, 8], F32) for i in range(2)]
            igs = k.sbuf(st, "igs", [128, 4, 8], F32)
            idxf = k.sbuf(st, "idxf", [128, 4], F32)
            g2 = k.sbuf(st, "g2", [128, 4], F32)
            Xs = [k.sbuf(st, f"Xs{i}", [128, 4, D], BF16) for i in range(2)]
            tpp = [k.psum(st, f"dtp{i}", [128, 512], BF16) for i in range(2)]
            XTs = [k.sbuf(st, f"XTs{i}", [128, 8, 512], BF16) for i in range(2)]
            oi = 0
            ji = 0
            for e in range(NEXP):
                for (t0, ntl, cap, c0) in sets:
                    nsc = (cap + 127) // 128
                    ig = igp[ji % 2]
                    X = Xs[ji % 2]
                    XT = XTs[ji % 2]
                    ji += 1
                    for ti in range(ntl):
                        t = t0 + ti
                        oh = ohs[oi % 4]
                        k.ts("dve", oh[:, 0:cap], iota[:, 0:cap], posm_all[:, t, e:e + 1], None,
                             ALU.is_equal, None, [iota, posm_all], [oh])
                        oi += 1
                        for sc in range(nsc):
                            sl = min(128, cap - sc * 128)
                            k.mm(ig[0:sl, sc, 0:5], oh[:, sc * 128:sc * 128 + sl], nv[:, t, e, :],
                                 ti == 0 and sc == 0, ti == ntl - 1, [oh, nv], [ig], inc=(sc == nsc - 1),
                                 skip_group_check=True)
                    sl0 = min(128, cap)
                    k.cp("act", igs[0:sl0, 0:nsc, 0:5], ig[0:sl0, 0:nsc, 0:5], [ig], [igs])
                    k.stt("dve", idxf[0:sl0, 0:nsc], igs[0:sl0, 0:nsc, 0], 64.0, igs[0:sl0, 0:nsc, 1], ALU.mult, ALU.add,
                          [igs], [idxf])
                    k.cp("dve", idx_all[0:sl0, e, c0:c0 + nsc], idxf[0:sl0, 0:nsc], [idxf], [idx_all])
                    k.tt("dve", g2[0:sl0, 0:nsc], igs[0:sl0, 0:nsc, 2], igs[0:sl0, 0:nsc, 3], ALU.add, [igs], [g2])
                    k.tt("dve", gate_all[0:sl0, e, c0:c0 + nsc], g2[0:sl0, 0:nsc], igs[0:sl0, 0:nsc, 4], ALU.add,
                         [g2, igs], [gate_all])
                    for sc in range(nsc):
                        sl = min(128, cap - sc * 128)
                        k.dma("pool", None, None, reads=[idx_all], writes=[X], fn=lambda: nc.gpsimd.indirect_dma_start(
                            out=X[0:sl, sc, :], out_offset=None, in_=hbf,
                            in_offset=bass.IndirectOffsetOnAxis(ap=idx_all[0:sl, e, c0 + sc:c0 + sc + 1], axis=0),
                            bounds_check=breg, oob_is_err=False))
                    for dc in range(8):
                        tp = tpp[dc % 2]
                        for sc in range(nsc):
                            sl = min(128, cap - sc * 128)
                            k.tr(tp[:, sc * 128:sc * 128 + sl], X[0:sl, sc, dc * 128:(dc + 1) * 128], ident_bf[0:sl, 0:sl],
                                 [X, ident_bf], [tp])
                        k.cp("act" if dc % 2 else "dve", XT[:, dc, 0:cap], tp[:, 0:cap], [tp], [XT])
                    col0 = 0 if c0 == 0 else 512
                    k.dma("sp", xtd[e, :, :, col0:col0 + cap], XT[:, :, 0:cap], reads=[XT])
            k.barrier()

    def phase_ffn(l, sets):
        with ExitStack() as st:
            g5 = [load_bcast(st, f"gate5_{r}", modvec(l, r, 5)) for r in range(len(sets))]
            xts = [k.sbuf(st, f"fxt{i}", [128, 8, 544], BF16) for i in range(2)]
            AT = k.sbuf(st, "AT", [128, 16, 544], BF16)
            stg = [k.sbuf(st, f"wstg{i}", [128, 2048], F32) for i in range(5)]
            wgb = [k.sbuf(st, f"wgb{i}", [128, 8, 512], BF16) for i in range(2)]
            wub = [k.sbuf(st, f"wub{i}", [128, 8, 512], BF16) for i in range(2)]
            wdb = k.sbuf(st, "wdb", [128, 16, D], BF16)
            sgs = [k.sbuf(st, f"sg{i}", [128, 512], F32) for i in range(2)]
            ysb = [k.sbuf(st, f"ysb{i}", [128, D], F32) for i in range(10)]
            gps = [k.psum(st, f"gps{i}", [128, 512], F32) for i in range(2)]
            ups = [k.psum(st, f"ups{i}", [128, 512], F32) for i in range(2)]
            cps_ = k.psum(st, "fcps", [128, 2, 32], F32)
            yps = [k.psum(st, f"yps{i}", [128, 512], F32) for i in range(2)]
            nctx = 32 if len(sets) > 1 else 0
            ncols = 512 + nctx
            si = [0]
            pending = []
            prev_sc_toks = []

            def stage_cast(src_ap, dst_ap, dstbuf, shape3):
                sg_ = stg[si[0] % 5]
                si[0] += 1
                v = sg_[:].rearrange("p (a b) -> p a b", a=shape3[0])
                k.dma("sp", v, src_ap, writes=[sg_])
                k.cp("dve" if si[0] % 2 else "act", dst_ap, v, [sg_], [dstbuf])

            def flush():
                nonlocal prev_sc_toks
                if not pending:
                    return
                for tok in prev_sc_toks:
                    k.wait("pool", tok)
                toks = []
                for (fn_, rd) in pending:
                    toks.append(k.dma("pool", None, None, reads=rd, fn=fn_))
                prev_sc_toks = toks
                pending.clear()

            it = 0
            yi = 0
            for e in range(NEXP):
                xt = xts[e % 2]
                k.dma("act", xt[:, :, 0:ncols], xtd[e, :, :, 0:ncols], writes=[xt])
                wgv = I.w_gate[l, e].rearrange("(c p) f -> p c f", p=128)
                wuv = I.w_up[l, e].rearrange("(c p) f -> p c f", p=128)
                wdv = I.w_down[l, e].rearrange("(c p) d -> p c d", p=128)
                for fb in range(4):
                    wg_, wu_ = wgb[fb % 2], wub[fb % 2]
                    for ch in range(2):
                        stage_cast(wgv[:, ch * 4:(ch + 1) * 4, fb * 512:(fb + 1) * 512], wg_[:, ch * 4:(ch + 1) * 4, :], wg_, (4, 512))
                    for ch in range(2):
                        stage_cast(wuv[:, ch * 4:(ch + 1) * 4, fb * 512:(fb + 1) * 512], wu_[:, ch * 4:(ch + 1) * 4, :], wu_, (4, 512))
                    for u in (2 * fb, 2 * fb + 1):
                        stage_cast(wdv[:, 2 * u:2 * u + 2, :], wdb[:, 2 * u:2 * u + 2, :], wdb, (2, 1024))
                    if fb == 1:
                        flush()
                    for fc in range(4):
                        f = fb * 4 + fc
                        gp, up = gps[it % 2], ups[it % 2]
                        sg = sgs[it % 2]
                        it += 1
                        for dc in range(8):
                            k.mm(gp[:], wg_[:, dc, fc * 128:(fc + 1) * 128], xt[:, dc, 0:512], dc == 0, dc == 7, [wg_, xt], [gp])
                        for dc in range(8):
                            k.mm(up[:], wu_[:, dc, fc * 128:(fc + 1) * 128], xt[:, dc, 0:512], dc == 0, dc == 7, [wu_, xt], [up])
                        k.actf(sg[:], gp[:], ACT.Silu, [gp], [sg])
                        k.tt("dve", AT[:, f, 0:512], sg[:], up[:], ALU.mult, [sg, up], [AT])
                        if nctx:
                            for dc in range(8):
                                k.mm(cps_[:, 0, :], wg_[:, dc, fc * 128:(fc + 1) * 128], xt[:, dc, 512:544], dc == 0, dc == 7,
                                     [wg_, xt], [cps_])
                            for dc in range(8):
                                k.mm(cps_[:, 1, :], wu_[:, dc, fc * 128:(fc + 1) * 128], xt[:, dc, 512:544], dc == 0, dc == 7,
                                     [wu_, xt], [cps_])
                            k.actf(sg[:, 0:32], cps_[:, 0, :], ACT.Silu, [cps_], [sg])
                            k.tt("dve", AT[:, f, 512:544], sg[:, 0:32], cps_[:, 1, :], ALU.mult, [sg, cps_], [AT])
                for si_, (t0, ntl, cap, c0) in enumerate(sets):
                    nsc = (cap + 127) // 128
                    acol = 0 if c0 == 0 else 512
                    for sc in range(nsc):
                        sl = min(128, cap - sc * 128)
                        ys = ysb[yi % 10]
                        yi += 1
                        for hf in range(2):
                            yp = yps[hf]
                            for f in range(16):
                                k.mm(yp[0:sl, :], AT[:, f, acol + sc * 128:acol + sc * 128 + sl], wdb[:, f, hf * 512:(hf + 1) * 512],
                                     f == 0, f == 15, [AT, wdb], [yp])
                            k.stt("dve", ys[0:sl, hf * 512:(hf + 1) * 512], yp[0:sl, :], gate_all[0:sl, e, c0 + sc:c0 + sc + 1],
                                  g5[si_][0:sl, hf * 512:(hf + 1) * 512], ALU.mult, ALU.mult, [yp, gate_all, g5[si_]], [ys])

                        def mk(ys=ys, sl=sl, e=e, col=c0 + sc):
                            return lambda: nc.gpsimd.indirect_dma_start(
                                out=xres, out_offset=bass.IndirectOffsetOnAxis(ap=idx_all[0:sl, e, col:col + 1], axis=0),
                                in_=ys[0:sl, :], in_offset=None, compute_op=ALU.add, bounds_check=breg)
                        pending.append((mk(), [ys, idx_all]))
            flush()
            k.barrier()

    def phase_final():
        with ExitStack() as st:
            gf = load_bcast(st, "gfin_b", I.g_final)
            xts = [k.sbuf(st, f"fnx{i}", [128, D], F32) for i in range(2)]
            junk = k.sbuf(st, "fnjunk", [128, D], BF16)
            ss = k.sbuf(st, "fnss", [128, 2], F32)
            rstd = k.sbuf(st, "fnrstd", [128, 1], F32)
            ys = [k.sbuf(st, f"fny{i}", [128, D], F32) for i in range(2)]
            for t in range(32):
                xt, y = xts[t % 2], ys[t % 2]
                k.dma("sp", xt[:], xres[t * 128:(t + 1) * 128, :], writes=[xt])
                rms_rstd(xt[:], ss, rstd, junk, D, [xt])
                k.stt("dve", y[:], xt[:], rstd[:, 0:1], gf[:], ALU.mult, ALU.mult, [xt, rstd, gf], [y])
                k.dma("act", out_d[t * 128:(t + 1) * 128, :], y[:], reads=[y])
            k.barrier()

    for l in range(layers):
        last = l == DEPTH - 1
        if "proj" not in skip:
            phase_proj(l)
        if stop_after == f"proj{l}":
            return finish(nc, k, es)
        if "attna" not in skip:
            phase_attn_a(l, not last)
        if stop_after == f"attna{l}":
            return finish(nc, k, es)
        if "fourier" not in skip:
            phase_fourier(l, not last)
        if stop_after == f"fourier{l}":
            return finish(nc, k, es)
        if "attnc" not in skip:
            phase_attn_c(l, not last)
        if stop_after == f"attnc{l}":
            return finish(nc, k, es)
        if "merge" not in skip:
            phase_merge(l, 32 if last else NT)
        if stop_after == f"merge{l}":
            if debug:
                k.dma("sp", dbg_aff, aff_all[:], reads=[aff_all])
                k.dma("sp", dbg_x.rearrange("(p a) d -> p (a d)", p=128), xres.rearrange("(p a) d -> p (a d)", p=128))
            return finish(nc, k, es)
        sets = [LAT] if last else [LAT, CTXS]
        if "moe" not in skip:
            phase_route(l, sets)
            if stop_after == f"route{l}":
                if debug:
                    k.dma("sp", dbg_pos, posm_all[:], reads=[posm_all])
                return finish(nc, k, es)
            phase_dispatch(l, sets)
            if stop_after == f"disp{l}":
                return finish(nc, k, es)
            phase_ffn(l, sets)
        if stop_after == f"moe{l}":
            if debug:
                k.dma("sp", dbg_x.rearrange("(p a) d -> p (a d)", p=128), xres.rearrange("(p a) d -> p (a d)", p=128))
                k.dma("sp", dbg_aff, aff_all[:], reads=[aff_all])
                k.dma("sp", dbg_pos, posm_all[:], reads=[posm_all])
                k.dma("sp", dbg_idx, idx_all[:], reads=[idx_all])
                k.dma("sp", dbg_gate, gate_all[:], reads=[gate_all])
            return finish(nc, k, es)
    if "final" not in skip:
        phase_final()
    return finish(nc, k, es)


def finish(nc, k, es):
    k.barrier()
    es.close()
    return nc


def make_in_maps(inputs, ncores=8):
    cst = host_consts()
    shared = {}
    for nm in ("c_ctx", "w_ada", "b_ada", "g_mix", "g_ffn", "w_in", "g_q", "g_k", "w_out", "w_router",
               "w_gate", "w_up", "w_down", "g_final"):
        shared[nm] = np.ascontiguousarray(inputs[nm], dtype=np.float32)
    shared["g_out"] = np.ascontiguousarray(
        np.concatenate([inputs["g_out_a"], inputs["g_out_b"], inputs["g_out_c"]], axis=-1), dtype=np.float32)
    shared["na_bias"] = na_bias_layout(inputs["rel_bias"])
    shared.update(cst)
    maps = []
    for b in range(ncores):
        m = dict(shared)
        m["x"] = np.ascontiguousarray(inputs["x"][b], dtype=np.float32)
        m["c"] = np.ascontiguousarray(inputs["c"][b], dtype=np.float32)
        m["ctx"] = np.ascontiguousarray(inputs["ctx"][b], dtype=np.float32)
        maps.append(m)
    return maps


def kernel(**inputs):
    nc = build_program()
    maps = make_in_maps(inputs, 8)
    maps = [{n: m[n] for n in nc._used_inputs} for m in maps]
    res = run_bass_kernel_spmd(nc, maps, core_ids=list(range(8)))
    return np.stack([np.asarray(r["out"], dtype=np.float32) for r in res.results], axis=0)
```

```python
from contextlib import ExitStack
import numpy as np
import ml_dtypes
import concourse.bass as bass
import concourse.mybir as mybir
from concourse.bass_utils import run_bass_kernel_spmd

F32 = mybir.dt.float32
BF16 = mybir.dt.bfloat16
I32 = mybir.dt.int32
ALU = mybir.AluOpType
ACT = mybir.ActivationFunctionType
AX = mybir.AxisListType

D = 1024
SEQ = 4096
CTX = 256
NTOK = SEQ + CTX
NT = NTOK // 128
DEPTH = 2
INW = 1792
NEXP = 16
FF = 2048
EPS = 1e-6


class Buf:
    def __init__(self, t):
        self.t = t
        self.w = None
        self.r = {}

    def __getitem__(self, idx):
        return self.t[idx]


class KB:
    NDS = {"sp": 16, "act": 8, "pool": 8}

    def __init__(self, nc, es):
        self.nc = nc
        self.es = es
        self.eng = dict(pe=nc.tensor, dve=nc.vector, act=nc.scalar, pool=nc.gpsimd, sp=nc.sync)
        self.csem = {e: es.enter_context(nc.semaphore("c_" + e)) for e in ("pe", "dve", "act", "pool")}
        self.ccnt = {e: 0 for e in self.csem}
        self.seen = {e: {} for e in self.eng}
        self.dsem = {q: [es.enter_context(nc.semaphore(f"d_{q}{i}")) for i in range(n)]
                     for q, n in self.NDS.items()}
        self.dcnt = {q: [0] * n for q, n in self.NDS.items()}
        self.dnext = {q: 0 for q in self.NDS}
        self.n_inst = 0

    def sbuf(self, st, name, shape, dtype):
        self.uid = getattr(self, "uid", 0) + 1
        return Buf(st.enter_context(self.nc.sbuf_tensor(f"{name}_{self.uid}", list(shape), dtype)))

    def psum(self, st, name, shape, dtype):
        self.uid = getattr(self, "uid", 0) + 1
        return Buf(st.enter_context(self.nc.psum_tensor(f"{name}_{self.uid}", list(shape), dtype)))

    def wait(self, eng, tok):
        sem, val = tok
        if self.seen[eng].get(sem, 0) >= val:
            return
        self.eng[eng].wait_ge(sem, val)
        self.seen[eng][sem] = val
        self.n_inst += 1

    def _deps(self, eng, reads, writes):
        toks = {}
        own = self.csem.get(eng)

        def add(tok, raw):
            if tok is None:
                return
            sem, val = tok
            if sem is own and eng == "pe":
                return
            if toks.get(sem, 0) < val:
                toks[sem] = val

        for b in reads:
            add(b.w, True)
        for b in writes:
            add(b.w, False)
            for sem, val in b.r.items():
                add((sem, val), False)
        for sem, val in toks.items():
            self.wait(eng, (sem, val))

    def _mark(self, tok, reads, writes):
        sem, val = tok
        for b in reads:
            if b.r.get(sem, 0) < val:
                b.r[sem] = val
        for b in writes:
            b.w = tok
            b.r = {}

    def op(self, eng, fn, reads=(), writes=(), inc=True):
        self._deps(eng, reads, writes)
        inst = fn()
        self.n_inst += 1
        if inc:
            self.ccnt[eng] += 1
            inst.then_inc(self.csem[eng], 1)
            tok = (self.csem[eng], self.ccnt[eng])
        else:
            tok = (self.csem[eng], self.ccnt[eng] + 1)
        self._mark(tok, reads, writes)
        return tok

    def dma(self, q, out, in_, reads=(), writes=(), fn=None, **kw):
        self._deps(q, reads, writes)
        i = self.dnext[q]
        self.dnext[q] = (i + 1) % len(self.dsem[q])
        sem = self.dsem[q][i]
        if self.dcnt[q][i] > 0:
            self.wait(q, (sem, self.dcnt[q][i]))
        if fn is None:
            inst = self.eng[q].dma_start(out=out, in_=in_, **kw)
        else:
            inst = fn()
        self.n_inst += 1
        self.dcnt[q][i] += 16
        inst.then_inc(sem, 16)
        tok = (sem, self.dcnt[q][i])
        self._mark(tok, reads, writes)
        return tok

    def barrier(self):
        toks = [(self.csem[e], self.ccnt[e]) for e in self.csem if self.ccnt[e] > 0]
        for q in self.dsem:
            for i, s in enumerate(self.dsem[q]):
                if self.dcnt[q][i] > 0:
                    toks.append((s, self.dcnt[q][i]))
        for e in self.eng:
            for t in toks:
                self.wait(e, t)

    def mm(self, out, lhsT, rhs, start, stop, reads, writes, inc=None, **kw):
        return self.op("pe", lambda: self.nc.tensor.matmul(out, lhsT=lhsT, rhs=rhs, start=start, stop=stop, **kw),
                       reads, writes, inc=(stop if inc is None else inc))

    def tr(self, out, in_, ident, reads, writes):
        return self.op("pe", lambda: self.nc.tensor.transpose(out, in_, ident), reads, writes)

    def actf(self, out, in_, func, reads, writes, **kw):
        return self.op("act", lambda: self.nc.scalar.activation(out=out, in_=in_, func=func, **kw), reads, writes)

    def cp(self, eng, out, in_, reads, writes):
        if eng == "act":
            return self.op("act", lambda: self.nc.scalar.copy(out=out, in_=in_), reads, writes)
        e = self.eng[eng]
        return self.op(eng, lambda: e.tensor_copy(out=out, in_=in_), reads, writes)

    def tt(self, eng, out, in0, in1, op, reads, writes):
        e = self.eng[eng]
        return self.op(eng, lambda: e.tensor_tensor(out=out, in0=in0, in1=in1, op=op), reads, writes)

    def ts(self, eng, out, in0, s1, s2, op0, op1, reads, writes, **kw):
        e = self.eng[eng]
        if s2 is None:
            return self.op(eng, lambda: e.tensor_scalar(out=out, in0=in0, scalar1=s1, scalar2=None, op0=op0, **kw),
                           reads, writes)
        return self.op(eng, lambda: e.tensor_scalar(out=out, in0=in0, scalar1=s1, scalar2=s2, op0=op0, op1=op1, **kw),
                       reads, writes)

    def stt(self, eng, out, in0, scalar, in1, op0, op1, reads, writes):
        e = self.eng[eng]
        return self.op(eng, lambda: e.scalar_tensor_tensor(out=out, in0=in0, scalar=scalar, in1=in1, op0=op0, op1=op1),
                       reads, writes)


def host_consts():
    c = {}
    c["ident_bf"] = np.eye(128, dtype=np.float32).astype(ml_dtypes.bfloat16)
    c["ident_f"] = np.eye(128, dtype=np.float32)
    t = np.arange(SEQ)
    row = (t // 64).astype(np.float64)
    col = (t % 64).astype(np.float64)
    inv = 10000.0 ** (-np.arange(16, dtype=np.float64) / 16)
    ang = np.stack([row[:, None] * inv, col[:, None] * inv], axis=1)
    c["rope_cos"] = np.cos(ang).reshape(SEQ, 32).astype(np.float32)
    c["rope_sin"] = np.sin(ang).reshape(SEQ, 32).astype(np.float32)
    j = np.arange(64)
    ph = 2 * np.pi * np.outer(j, j) / 64.0
    bd = np.zeros((256, 512), np.float64)
    for g in range(4):
        bd[g * 64:(g + 1) * 64, g * 64:(g + 1) * 64] = np.cos(ph)
        bd[g * 64:(g + 1) * 64, 256 + g * 64:256 + (g + 1) * 64] = np.sin(ph)
    c["chan_dft"] = bd.astype(np.float32).astype(ml_dtypes.bfloat16)

    def dft_tiles(n):
        nt = n // 128
        m = np.arange(n, dtype=np.int64)
        tab_c = np.cos(2 * np.pi * m / n)
        tab_s = -np.sin(2 * np.pi * m / n)
        kk = np.arange(n, dtype=np.int64).reshape(nt, 1, 1, 128)
        nn = (np.arange(nt, dtype=np.int64)[None, :] * 128 + np.arange(128, dtype=np.int64)[:, None]).reshape(1, 128, nt, 1)
        idx = (kk * nn) % n
        return (tab_c[idx].astype(np.float32).astype(ml_dtypes.bfloat16),
                tab_s[idx].astype(np.float32).astype(ml_dtypes.bfloat16))

    c["tri_f"] = np.triu(np.ones((128, 128), np.float32), 1)
    c["iota_slots"] = np.broadcast_to(np.arange(512, dtype=np.float32), (128, 512)).copy()
    p = np.arange(128)
    tk = np.zeros((128, NT, 2), np.float32)
    tk[:, :, 0] = 2 * np.arange(NT)[None, :] + (p // 64)[:, None]
    tk[:, :, 1] = (p % 64)[:, None]
    c["tokhl"] = tk
    c["dftc"], c["dfts"] = dft_tiles(SEQ)
    c["dftxc"], c["dftxs"] = dft_tiles(CTX)
    return c


_NA_IDX = None


def na_bias_layout(rel_bias):
    global _NA_IDX
    if _NA_IDX is None:
        variants = [(0, 0), (1, 0), (2, 0), (30, 27), (31, 27)]
        ro = np.zeros((5, 128, 5, 128), np.int64)
        co = np.zeros((5, 128, 5, 128), np.int64)
        ok = np.zeros((5, 128, 5, 128), bool)
        kl = np.arange(128).reshape(128, 1, 1)
        kj = np.arange(5).reshape(1, 5, 1)
        ql = np.arange(128).reshape(1, 1, 128)
        for v, (qi, s0) in enumerate(variants):
            r = 2 * qi + ql // 64
            cq = ql % 64
            kr = 2 * (s0 + kj) + kl // 64
            kc = kl % 64
            rs = np.clip(r - 4, 0, 56)
            cs = np.clip(cq - 8, 0, 48)
            inwin = (kr >= rs) & (kr < rs + 8) & (kc >= cs) & (kc < cs + 16)
            ok[v] = np.broadcast_to(inwin, (128, 5, 128))
            ro[v] = np.clip(np.broadcast_to(kr - r + 7, (128, 5, 128)), 0, 14)
            co[v] = np.clip(np.broadcast_to(kc - cq + 15, (128, 5, 128)), 0, 30)
        _NA_IDX = (ro, co, ok)
    ro, co, ok = _NA_IDX
    rb = np.asarray(rel_bias, np.float32)
    g = rb[:, :, ro, co]
    g = np.where(ok[None, None], g, np.float32(-30000.0))
    g = g.transpose(0, 2, 3, 4, 1, 5).reshape(rb.shape[0], 5, 128, 5, 512)
    return np.ascontiguousarray(g, dtype=np.float32)


def build_program(stop_after=None, debug=False, layers=DEPTH, skip=(), cqb=None, cvar=0, ext=(), outs=None):
    nc = bass.Bass("TRN2", target_bir_lowering=False)
    es = ExitStack()
    IN = {}
    SPEC = {
        "x": ([SEQ, D], F32),
        "c": ([D], F32),
        "ctx": ([CTX, D], F32),
        "c_ctx": ([D], F32),
        "w_ada": ([DEPTH, D, 6 * D], F32),
        "b_ada": ([DEPTH, 6 * D], F32),
        "g_mix": ([DEPTH, D], F32),
        "g_ffn": ([DEPTH, D], F32),
        "w_in": ([DEPTH, D, INW], F32),
        "g_q": ([DEPTH, 64], F32),
        "g_k": ([DEPTH, 64], F32),
        "g_out": ([DEPTH, D], F32),
        "w_out": ([DEPTH, D, D], F32),
        "w_router": ([DEPTH, D, NEXP], F32),
        "w_gate": ([DEPTH, NEXP, D, FF], F32),
        "w_up": ([DEPTH, NEXP, D, FF], F32),
        "w_down": ([DEPTH, NEXP, FF, D], F32),
        "g_final": ([D], F32),
        "ident_bf": ([128, 128], BF16),
        "ident_f": ([128, 128], F32),
        "rope_cos": ([SEQ, 32], F32),
        "rope_sin": ([SEQ, 32], F32),
        "chan_dft": ([256, 512], BF16),
        "dftc": ([32, 128, 32, 128], BF16),
        "dfts": ([32, 128, 32, 128], BF16),
        "dftxc": ([2, 128, 2, 128], BF16),
        "dftxs": ([2, 128, 2, 128], BF16),
        "na_bias": ([DEPTH, 5, 128, 5, 512], F32),
        "tri_f": ([128, 128], F32),
        "iota_slots": ([128, 512], F32),
        "tokhl": ([128, NT, 2], F32),
    }

    class _Lazy:
        def __getattr__(self, name):
            if name not in IN:
                shape, dt = SPEC[name]
                IN[name] = nc.dram_tensor(name, list(shape), dt, kind="ExternalInput").ap()
            return IN[name]

    I = _Lazy()
    nc._used_inputs = IN

    out_d = nc.dram_tensor("out", [SEQ, D], F32, kind="ExternalOutput").ap()

    skind = "ExternalOutput" if debug else "Internal"

    def dscr(name, shape, dtype):
        if name in ext:
            IN[name] = nc.dram_tensor(name, list(shape), dtype, kind="ExternalInput").ap()
            return IN[name]
        kind = skind if (outs is None or name in outs) else "Internal"
        return nc.dram_tensor(name, list(shape), dtype, kind=kind).ap()

    xres = dscr("xres", [NTOK, D], F32)
    modd = dscr("modd", [DEPTH, 2, 6 * D], F32)
    qat = dscr("qat", [128, NT, 4, 128], BF16)
    kat = dscr("kat", [128, NTOK], BF16)
    vav = dscr("vav", [128, NT, 384], BF16)
    ud = dscr("ud", [128, NT, 512], BF16)
    qnt = dscr("qnt", [64, 4, NTOK], BF16)
    knt = dscr("knt", [64, 4, NTOK], BF16)
    od = dscr("od", [NTOK, D], F32)
    hbf = dscr("hbf", [NTOK, D], BF16)
    xtd = dscr("xtd", [NEXP, 128, 8, 544], BF16)
    dbg_aff = dscr("dbg_aff", [128, NT, NEXP], F32) if debug else None
    dbg_x = dscr("dbg_x", [NTOK, D], F32) if debug else None
    dbg_pos = dscr("dbg_pos", [128, NT, NEXP], F32) if debug else None
    dbg_idx = dscr("dbg_idx", [128, NEXP, 5], I32) if debug else None
    dbg_gate = dscr("dbg_gate", [128, NEXP, 5], F32) if debug else None

    k = KB(nc, es)
    breg = nc.gpsimd.to_reg(NTOK - 1)

    ident_bf = k.sbuf(es, "ident_bf_s", [128, 128], BF16)
    ident_f = k.sbuf(es, "ident_f_s", [128, 128], F32)
    k.dma("sp", ident_bf[:], I.ident_bf, writes=[ident_bf])
    k.dma("sp", ident_f[:], I.ident_f, writes=[ident_f])

    for i in range(8 if "init" not in skip else 0):
        k.dma("sp", xres[i * 512:(i + 1) * 512, :].rearrange("(p a) d -> p (a d)", p=128),
              I.x[i * 512:(i + 1) * 512, :].rearrange("(p a) d -> p (a d)", p=128))
    if "init" not in skip:
        k.dma("sp", xres[SEQ:NTOK, :].rearrange("(p a) d -> p (a d)", p=128),
              I.ctx.rearrange("(p a) d -> p (a d)", p=128))

    def phase_ada():
        with ExitStack() as st:
            craw = k.sbuf(st, "craw", [128, 2, 8], F32)
            scT = k.sbuf(st, "scT", [128, 8, 2], F32)
            k.dma("sp", craw[:, 0, :], I.c.rearrange("(p c) -> p c", c=8), writes=[craw])
            k.dma("sp", craw[:, 1, :], I.c_ctx.rearrange("(p c) -> p c", c=8), writes=[craw])
            k.actf(scT[:].rearrange("p c r -> p r c"), craw[:], ACT.Silu, [craw], [scT])
            bias = k.sbuf(st, "adab", [2, DEPTH, 6 * D], F32)
            for l in range(DEPTH):
                k.dma("sp", bias[:, l, :], I.b_ada[l].partition_broadcast(2), writes=[bias])
            wts = [k.sbuf(st, f"adaw{i}", [128, 8, 512], F32) for i in range(2)]
            pss = [k.psum(st, f"adaps{i}", [2, 512], F32) for i in range(2)]
            mod = k.sbuf(st, "adamod", [2, DEPTH, 6 * D], F32)
            it = 0
            for l in range(DEPTH):
                wv = I.w_ada[l].rearrange("(p c) n -> p c n", c=8)
                for j in range(12):
                    wt = wts[it % 2]
                    ps = pss[it % 2]
                    it += 1
                    k.dma("sp" if it % 2 else "act", wt[:], wv[:, :, j * 512:(j + 1) * 512], writes=[wt])
                    for c8 in range(8):
                        k.mm(ps[:], scT[:, c8, :], wt[:, c8, :], c8 == 0, c8 == 7, [scT, wt], [ps])
                    k.tt("dve", mod[:, l, j * 512:(j + 1) * 512], ps[:], bias[:, l, j * 512:(j + 1) * 512], ALU.add,
                         [ps, bias], [mod])
                k.dma("sp", modd[l], mod[:, l, :], reads=[mod])
            k.barrier()

    if "ada" not in skip:
        phase_ada()
    if stop_after == "ada":
        return finish(nc, k, es)

    def load_bcast(st, name, src_ap, q="sp"):
        b = k.sbuf(st, name, [128, src_ap.shape[-1]], F32)
        k.dma(q, b[:], src_ap.partition_broadcast(128), writes=[b])
        return b

    def modvec(l, r, c):
        return modd[l, r, c * D:(c + 1) * D]

    def make_gm(st, name, l, r, c_scale, gvec):
        sc = load_bcast(st, name + "_s", modvec(l, r, c_scale))
        g = load_bcast(st, name + "_g", gvec)
        k.stt("dve", sc[:], sc[:], 1.0, g[:], ALU.add, ALU.mult, [sc, g], [sc])
        return sc

    def rms_rstd(xt, ss, rstd, junk, n, reads):
        k.actf(junk[:, 0:n], xt, ACT.Square, reads, [junk, ss], accum_out=ss[:, 0:1])
        k.actf(ss[:, 1:2], ss[:, 0:1], ACT.Sqrt, [ss, epsb], [ss], scale=1.0 / n, bias=epsb[:, 0:1])
        k.op("dve", lambda: nc.vector.reciprocal(out=rstd[:, 0:1], in_=ss[:, 1:2]), [ss], [rstd])

    epsb = k.sbuf(es, "epsb", [128, 1], F32)
    k.op("dve", lambda: nc.vector.memset(epsb[:], EPS), [], [epsb])

    def phase_proj(l):
        with ExitStack() as st:
            wsb = k.sbuf(st, "w_in_s", [128, 8, INW], BF16)
            k.dma("pool", wsb[:], I.w_in[l].rearrange("(c p) n -> p c n", p=128), writes=[wsb])
            cdft = k.sbuf(st, "cdft_s", [128, 2, 512], BF16)
            k.dma("sp", cdft[:], I.chan_dft.rearrange("(c p) n -> p c n", p=128), writes=[cdft])
            gm = [make_gm(st, f"gm1_{r}", l, r, 1, I.g_mix[l]) for r in range(2)]
            sh = [load_bcast(st, f"sh1_{r}", modvec(l, r, 0)) for r in range(2)]
            gq = load_bcast(st, "gq_b", I.g_q[l])
            gk = load_bcast(st, "gk_b", I.g_k[l])
            gqk = k.sbuf(st, "gqk", [128, 10, 64], F32)
            k.cp("dve", gqk[:, 0:8, :], gq[:].unsqueeze(1).broadcast_to([128, 8, 64]), [gq], [gqk])
            k.cp("dve", gqk[:, 8:10, :], gk[:].unsqueeze(1).broadcast_to([128, 2, 64]), [gk], [gqk])

            def dbl(name, shape, dt):
                return [k.sbuf(st, f"{name}{i}", shape, dt) for i in range(2)]

            xts = [k.sbuf(st, f"xt{i}", [128, D], F32) for i in range(4)]
            junk_ = dbl("junk", [128, D], BF16)
            ss_ = dbl("ss", [128, 2], F32)
            rstd_ = dbl("rstd", [128, 1], F32)
            hn_ = dbl("hn", [128, D], F32)
            hb_ = dbl("hb", [128, D], BF16)
            hT_ = dbl("hT", [128, 8, 512], BF16)
            hTv_ = [[Buf(h.t) for _ in range(4)] for h in hT_]
            tps_ = [k.psum(st, f"tps{i}", [128, 8, 128], BF16) for i in range(2)]
            pps_ = [k.psum(st, f"pps{i}", [128, 1024], F32) for i in range(2)]
            fps = [k.psum(st, f"fps{i}", [128, 512], F32) for i in range(2)]
            sq_ = dbl("sq", [128, 640], F32)
            ss10_ = dbl("ss10", [128, 2, 10], F32)
            qkn_ = dbl("qkn", [128, 10, 64], F32)
            rp_ = [[k.sbuf(st, f"rp{i}_{j}", [128, 10, 2, 16], F32) for i in range(4)] for j in range(2)]
            qkr_ = dbl("qkr", [128, 640], BF16)
            qst_ = dbl("qst", [128, 5, 128], BF16)
            vst_ = dbl("vst", [128, 384], BF16)
            cs = [k.sbuf(st, f"ropec{i}", [128, 2, 32], F32) for i in range(2)]
            ubT_ = dbl("ubT", [128, 2, 512], BF16)
            fst = [k.sbuf(st, f"fst{i}", [128, 512], BF16) for i in range(2)]
            ust_ = dbl("ust", [128, 512], BF16)
            fi = 0

            macros = [(m * 4, 4) for m in range(8)] + [(32, 2)]
            tiles = []
            for mi, (t0, ntl) in enumerate(macros):
                for tl in range(ntl):
                    tiles.append((mi, t0, ntl, tl))

            def setup(ti):
                mi, t0, ntl, tl = tiles[ti]
                t = t0 + tl
                pz = ti % 2
                return (mi, t0, ntl, tl, t, 0 if t < 32 else 1, pz, hT_[mi % 2], hTv_[mi % 2], xts[ti % 4], junk_[pz], ss_[pz],
                        rstd_[pz], hn_[pz], hb_[pz], tps_[pz], pps_[pz], sq_[pz], ss10_[pz], qkn_[pz], rp_[pz], qkr_[pz],
                        qst_[pz], vst_[pz], tps_[1 - pz])

            def stage_a(ti):
                (mi, t0, ntl, tl, t, r, pz, hT, hTv, xt, junk, ss, rstd, hn, hb, tps, pps, sq, ss10, qkn, rp, qkr, qst, vst,
                 qps) = setup(ti)
                for t2 in ([0, 1, 2] if ti == 0 else [ti + 2]):
                    if t2 < NT:
                        k.dma("pool", xts[t2 % 4][:], xres[t2 * 128:(t2 + 1) * 128, :], writes=[xts[t2 % 4]])
                rms_rstd(xt[:], ss, rstd, junk, D, [xt])
                k.stt("dve", hn[:], xt[:], rstd[:, 0:1], gm[r][:], ALU.mult, ALU.mult, [xt, rstd, gm[r]], [hn])
                k.tt("dve", hb[:], hn[:], sh[r][:], ALU.add, [hn, sh[r]], [hb])
                for dc in range(8):
                    k.tr(tps[:, dc, :], hb[:, dc * 128:(dc + 1) * 128], ident_bf[:], [hb, ident_bf], [tps])
                k.cp("act", hT[:, :, tl * 128:(tl + 1) * 128], tps[:], [tps], [hTv[tl]])
                for (c0, c1, o0) in ((0, 512, 0), (512, 768, 512), (1536, 1792, 768)):
                    for dc in range(8):
                        k.mm(pps[:, o0:o0 + (c1 - c0)], hT[:, dc, tl * 128:(tl + 1) * 128], wsb[:, dc, c0:c1],
                             dc == 0, dc == 7, [hTv[tl], wsb], [pps])
                k.cp("act", vst[:], pps[:, 640:1024], [pps], [vst])
                k.dma("sp", vav[:, t, :], vst[:], reads=[vst])
                k.actf(sq[:], pps[:, 0:640], ACT.Square, [pps], [sq])

            def stage_b(ti):
                (mi, t0, ntl, tl, t, r, pz, hT, hTv, xt, junk, ss, rstd, hn, hb, tps, pps, sq, ss10, qkn, rp, qkr, qst, vst,
                 qps) = setup(ti)
                k.op("dve", lambda: nc.vector.tensor_reduce(out=ss10[:, 0, :], in_=sq[:].rearrange("p (h d) -> p h d", d=64),
                                                            axis=AX.X, op=ALU.add), [sq], [ss10])
                k.actf(ss10[:, 1, :], ss10[:, 0, :], ACT.Sqrt, [ss10, epsb], [ss10], scale=1.0 / 64, bias=epsb[:, 0:1])
                k.op("dve", lambda: nc.vector.reciprocal(out=ss10[:, 0, :], in_=ss10[:, 1, :]), [ss10], [ss10])
                k.tt("dve", qkn[:], pps[:, 0:640].rearrange("p (h d) -> p h d", d=64),
                     ss10[:, 0, :].unsqueeze(2).broadcast_to([128, 10, 64]), ALU.mult, [pps, ss10], [qkn])
                qdst = qkr[:, 0:512].rearrange("p (hh g d) -> p g hh d", hh=4, g=2)
                kdst = qkr[:, 512:640].rearrange("p (g d) -> p g d", g=2)
                if r == 1:
                    k.tt("dve", qdst, qkn[:, 0:8, :].rearrange("p (g hh) d -> p g hh d", g=2),
                         gqk[:, 0:8, :].rearrange("p (g hh) d -> p g hh d", g=2), ALU.mult, [qkn, gqk], [qkr])
                    k.tt("dve", kdst, qkn[:, 8:10, :], gqk[:, 8:10, :], ALU.mult, [qkn, gqk], [qkr])
                else:
                    k.tt("dve", qkn[:], qkn[:], gqk[:], ALU.mult, [qkn, gqk], [qkn])
                    cst = cs[pz]
                    k.dma("act", cst[:, 0, :], I.rope_cos[t * 128:(t + 1) * 128, :], writes=[cst])
                    k.dma("act", cst[:, 1, :], I.rope_sin[t * 128:(t + 1) * 128, :], writes=[cst])
                    qv = qkn[:].rearrange("p h (a s f) -> p h a s f", a=2, s=2)
                    x1 = qv[:, :, :, 0, :]
                    x2 = qv[:, :, :, 1, :]
                    cosb = cst[:, 0, :].rearrange("p (a f) -> p a f", a=2).unsqueeze(1).broadcast_to([128, 10, 2, 16])
                    sinb = cst[:, 1, :].rearrange("p (a f) -> p a f", a=2).unsqueeze(1).broadcast_to([128, 10, 2, 16])
                    k.tt("dve", rp[0][:], x1, cosb, ALU.mult, [qkn, cst], [rp[0]])
                    k.tt("dve", rp[1][:], x2, sinb, ALU.mult, [qkn, cst], [rp[1]])
                    k.tt("dve", rp[2][:], x1, sinb, ALU.mult, [qkn, cst], [rp[2]])
                    k.tt("dve", rp[3][:], x2, cosb, ALU.mult, [qkn, cst], [rp[3]])
                    qd5 = qkr[:, 0:512].rearrange("p (hh g a s f) -> p g hh a s f", hh=4, g=2, a=2, s=2)
                    kd5 = qkr[:, 512:640].rearrange("p (g a s f) -> p g a s f", g=2, a=2, s=2)
                    for s_, (ra, rb, op_) in enumerate(((0, 1, ALU.subtract), (2, 3, ALU.add))):
                        for g in range(2):
                            k.tt("dve", qd5[:, g, :, :, s_, :], rp[ra][:, g * 4:(g + 1) * 4], rp[rb][:, g * 4:(g + 1) * 4],
                                 op_, [rp[ra], rp[rb]], [qkr])
                        k.tt("dve", kd5[:, :, :, s_, :], rp[ra][:, 8:10], rp[rb][:, 8:10], op_, [rp[ra], rp[rb]], [qkr])
                for j5 in range(5):
                    k.tr(qps[:, j5, :], qkr[:, j5 * 128:(j5 + 1) * 128], ident_bf[:], [qkr, ident_bf], [qps])
                k.cp("act", qst[:], qps[:, 0:5, :], [qps], [qst])
                k.dma("sp", qat[:, t, :, :], qst[:, 0:4, :], reads=[qst])
                k.dma("sp", kat[:, t * 128:(t + 1) * 128], qst[:, 4, :], reads=[qst])

            def stage_f(mi):
                t0, ntl = macros[mi]
                ncol = ntl * 128
                hT, hTv, ubT = hT_[mi % 2], hTv_[mi % 2], ubT_[mi % 2]
                for ci, c0 in enumerate((768, 896)):
                    fp = fps[ci % 2]
                    for dc in range(8):
                        k.mm(fp[:, 0:ncol], wsb[:, dc, c0:c0 + 128], hT[:, dc, 0:ncol], dc == 0, dc == 7, [wsb] + hTv[0:ntl], [fp])
                    k.cp("dve", ubT[:, ci, 0:ncol], fp[:, 0:ncol], [fp], [ubT])
                for ci in range(4):
                    c0 = (1024 if ci < 2 else 1280) + 128 * (ci % 2)
                    fp = fps[ci % 2]
                    for dc in range(8):
                        k.mm(fp[:, 0:ncol], wsb[:, dc, c0:c0 + 128], hT[:, dc, 0:ncol], dc == 0, dc == 7, [wsb] + hTv[0:ntl], [fp])
                    fs = fst[ci % 2]
                    k.cp("dve" if ci % 2 else "act", fs[:, 0:ncol], fp[:, 0:ncol], [fp], [fs])
                    dst = qnt if ci < 2 else knt
                    for hh in range(2):
                        k.dma("sp", dst[:, 2 * (ci % 2) + hh, t0 * 128:t0 * 128 + ncol], fs[hh * 64:(hh + 1) * 64, 0:ncol], reads=[fs])
                for tl in range(ntl):
                    t = t0 + tl
                    cps = fps[tl % 2]
                    ust = ust_[tl % 2]
                    for c2 in range(2):
                        k.mm(cps[:], ubT[:, c2, tl * 128:(tl + 1) * 128], cdft[:, c2, :], c2 == 0, c2 == 1, [ubT, cdft], [cps])
                    k.cp("act", ust[:], cps[:], [cps], [ust])
                    k.dma("sp", ud[:, t, :], ust[:], reads=[ust])

            stage_a(0)
            for ti in range(len(tiles)):
                if ti + 1 < len(tiles):
                    stage_a(ti + 1)
                stage_b(ti)
                mi, t0, ntl, tl = tiles[ti]
                if tl == ntl - 1:
                    stage_f(mi)
            k.barrier()

    def phase_attn_a(l, ctx_q):
        with ExitStack() as st:
            KaT = k.sbuf(st, "KaT", [128, NTOK], BF16)
            k.dma("sp", KaT[:], kat, writes=[KaT])
            vstg = k.sbuf(st, "vstg", [128, NT, 384], BF16)
            k.dma("sp", vstg[:], vav, writes=[vstg])
            Vs = k.sbuf(st, "Vs", [128, NT, 2, 65], BF16)
            k.op("dve", lambda: nc.vector.memset(Vs[:, :, :, 64:65], 1.0), [], [Vs])
            k.cp("dve", Vs[:, :, :, 0:64], vstg[:, :, 0:128].rearrange("p t (g d) -> p t g d", g=2), [vstg], [Vs])
            qts = [k.sbuf(st, f"qt{i}", [128, 512], BF16) for i in range(2)]
            sps = [[k.psum(st, f"sps{i}_{g}", [128, 512], F32) for g in range(2)] for i in range(2)]
            ops_ = [[k.psum(st, f"ops{i}_{g}", [128, 4, 65], F32) for g in range(2)] for i in range(2)]
            pts = [[k.sbuf(st, f"pt{i}_{g}", [128, 512], BF16) for g in range(2)] for i in range(3)]
            rec = k.sbuf(st, "rec", [128, 4], F32)
            osts = [k.sbuf(st, f"ost{i}", [128, 4, 64], F32) for i in range(2)]
            iters = []
            nqb = NT if ctx_q else 32
            for qb in range(nqb):
                kts = list(range(NT)) if qb < 32 else [32, 33]
                for i, kt in enumerate(kts):
                    iters.append((qb, i, kt, len(kts)))

            def stage_qk(j):
                qb, i, kt, n = iters[j]
                qt = qts[qb % 2]
                if i == 0:
                    for q2 in ([qb, qb + 1] if qb == 0 else [qb + 1]):
                        if q2 < nqb:
                            k.dma("sp", qts[q2 % 2][:], qat[:, q2, :, :].rearrange("p h n -> p (h n)"), writes=[qts[q2 % 2]])
                for g in range(2):
                    k.mm(sps[j % 2][g][:], KaT[g * 64:(g + 1) * 64, kt * 128:(kt + 1) * 128], qt[g * 64:(g + 1) * 64, :],
                         True, True, [KaT, qt], [sps[j % 2][g]])
                for g in range(2):
                    k.actf(pts[j % 3][g][:], sps[j % 2][g][:], ACT.Exp, [sps[j % 2][g]], [pts[j % 3][g]], scale=0.125)

            def stage_pv(j):
                qb, i, kt, n = iters[j]
                for g in range(2):
                    pt = pts[j % 3][g]
                    op_ = ops_[qb % 2][g]
                    for hh in range(4):
                        k.mm(op_[:, hh, :], pt[:, hh * 128:(hh + 1) * 128], Vs[:, kt, g, :],
                             i == 0 and hh == 0, i == n - 1, [pt, Vs], [op_], skip_group_check=True)
                if i == n - 1:
                    for g in range(2):
                        op_ = ops_[qb % 2][g]
                        ost = osts[g]
                        k.op("dve", lambda: nc.vector.reciprocal(out=rec[:], in_=op_[:, :, 64]), [op_], [rec])
                        k.tt("dve", ost[:], op_[:, :, 0:64], rec[:].unsqueeze(2).broadcast_to([128, 4, 64]), ALU.mult,
                             [op_, rec], [ost])
                        k.dma("sp", od[qb * 128:(qb + 1) * 128, g * 256:(g + 1) * 256], ost[:].rearrange("p h d -> p (h d)"),
                              reads=[ost])

            stage_qk(0)
            for j in range(len(iters)):
                if j + 1 < len(iters):
                    stage_qk(j + 1)
                stage_pv(j)
            k.barrier()

    def phase_fourier(l, ctx_q):
        with ExitStack() as st:
            U = k.sbuf(st, "Ures", [128, NT, 512], BF16)
            k.dma("sp", U[:], ud, writes=[U])
            cb = [k.sbuf(st, f"dftcb{i}", [128, 32, 128], BF16) for i in range(4)]
            sb = [k.sbuf(st, f"dftsb{i}", [128, 32, 128], BF16) for i in range(4)]
            fps_ = [k.psum(st, f"fops{i}", [128, 256], F32) for i in range(2)]
            fos = [k.sbuf(st, f"fos{i}", [128, 256], F32) for i in range(2)]
            jobs = [(kt, 32, 0, I.dftc[kt], I.dfts[kt], 1.0 / 512) for kt in range(32)]
            if ctx_q:
                jobs += [(32 + kt, 2, 32, I.dftxc[kt], I.dftxs[kt], 1.0 / 128) for kt in range(2)]
            def load_job(jj):
                (_ot, nnt_, _t0, cd_, sd_, _scl) = jobs[jj]
                k.dma("sp", cb[jj % 4][:, 0:nnt_, :], cd_, writes=[cb[jj % 4]])
                k.dma("pool", sb[jj % 4][:, 0:nnt_, :], sd_, writes=[sb[jj % 4]])

            for jj in range(min(3, len(jobs))):
                load_job(jj)
            for ji, (ot, nnt, t0, cd, sd, scl) in enumerate(jobs):
                c_, s_ = cb[ji % 4], sb[ji % 4]
                if ji + 3 < len(jobs):
                    load_job(ji + 3)
                fp = fps_[ji % 2]
                for nt_ in range(nnt):
                    k.mm(fp[:], c_[:, nt_, :], U[:, t0 + nt_, 0:256], nt_ == 0, False, [c_, U], [fp])
                    k.mm(fp[:], s_[:, nt_, :], U[:, t0 + nt_, 256:512], False, nt_ == nnt - 1, [s_, U], [fp])
                fo = fos[ji % 2]
                k.actf(fo[:], fp[:], ACT.Copy, [fp], [fo], scale=scl)
                k.dma("sp", od[ot * 128:(ot + 1) * 128, 512:768], fo[:], reads=[fo])
            k.barrier()

    def phase_attn_c(l, ctx_q):
        with ExitStack() as st:
            import os
            CF = os.environ.get("CF", "kqvmcn")
            KnT = k.sbuf(st, "KnT", [64, 4, NTOK], BF16)
            QnT = k.sbuf(st, "QnT", [64, 4, NTOK], BF16)
            if "k" in CF:
                k.dma("sp", KnT[:], knt, writes=[KnT])
            if "q" in CF:
                k.dma("act", QnT[:], qnt, writes=[QnT])
            vstg = k.sbuf(st, "vstg", [128, NT, 384], BF16)
            if "v" in CF:
                k.dma("sp", vstg[:], vav, writes=[vstg])
            Vn = k.sbuf(st, "Vn", [128, NT, 4, 65], BF16)
            if "m" in CF:
                k.op("dve", lambda: nc.vector.memset(Vn[:, :, :, 64:65], 1.0), [], [Vn])
            if "c" in CF:
                k.cp("dve", Vn[:, :, :, 0:64], vstg[:, :, 128:384].rearrange("p t (h d) -> p t h d", h=4), [vstg], [Vn])
            nab_i = k.sbuf(st, "nab_i", [128, 5, 512], F32)
            nab_s = k.sbuf(st, "nab_s", [128, 5, 512], F32)
            for j in range(5):
                k.dma("sp", nab_i[:, j, :], I.na_bias[l, 2, :, j, :], writes=[nab_i])
            sps = [k.psum(st, f"csps{i}", [128, 4, 128], F32) for i in range(2)]
            ops_ = [k.psum(st, f"cops{i}", [128, 4, 65], F32) for i in range(2)]
            scs = [k.sbuf(st, f"csc{i}", [128, 512], F32) for i in range(2)]
            pts = [k.sbuf(st, f"cpt{i}", [128, 512], BF16) for i in range(3)]
            rec = k.sbuf(st, "crec", [128, 4], F32)
            osts = [k.sbuf(st, f"cost{i}", [128, 4, 64], F32) for i in range(2)]
            iters = []
            for qb in (cqb if cqb is not None else range(NT if ctx_q else 32)):
                if qb < 32:
                    s0 = min(max(qb - 2, 0), 27)
                    var = {0: 0, 1: 1, 30: 3, 31: 4}.get(qb, 2)
                    kts = [(s0 + j, j) for j in range(5)] + [(32, None), (33, None)]
                else:
                    var = 2
                    kts = [(32, None), (33, None)]
                for i, (kt, bj) in enumerate(kts):
                    iters.append((qb, i, kt, bj, var, len(kts)))

            def stage_qk(j):
                qb, i, kt, bj, var, n = iters[j]
                sp_, pt, sc = sps[j % 2], pts[j % 3], scs[j % 2]
                nab = nab_i
                if var != 2:
                    nab = nab_s
                    if i == 0:
                        for jj in range(5):
                            k.dma("sp", nab_s[:, jj, :], I.na_bias[l, var, :, jj, :], writes=[nab_s])
                for h in range(4):
                    k.mm(sp_[:, h, :], KnT[:, h, kt * 128:(kt + 1) * 128],
                         QnT[:, h, qb * 128:(qb + 1) * 128], True, True, [KnT, QnT], [sp_])
                spf = sp_[:].rearrange("p h n -> p (h n)")
                if bj is None:
                    k.actf(pt[:], spf, ACT.Exp, [sp_], [pt], scale=0.125)
                else:
                    k.stt("dve", sc[:], spf, 0.125, nab[:, bj, :], ALU.mult, ALU.add, [sp_, nab], [sc])
                    k.actf(pt[:], sc[:], ACT.Exp, [sc], [pt])

            def stage_pv(j):
                qb, i, kt, bj, var, n = iters[j]
                pt = pts[j % 3]
                op_ = ops_[qb % 2]
                for h in range(4):
                    k.mm(op_[:, h, :], pt[:, h * 128:(h + 1) * 128], Vn[:, kt, h, :],
                         i == 0 and h == 0, i == n - 1, [pt, Vn], [op_], skip_group_check=True)
                if i == n - 1:
                    ost = osts[qb % 2]
                    k.op("dve", lambda: nc.vector.reciprocal(out=rec[:], in_=op_[:, :, 64]), [op_], [rec])
                    k.tt("dve", ost[:], op_[:, :, 0:64], rec[:].unsqueeze(2).broadcast_to([128, 4, 64]), ALU.mult,
                         [op_, rec], [ost])
                    k.dma("sp", od[qb * 128:(qb + 1) * 128, 768:1024], ost[:].rearrange("p h d -> p (h d)"), reads=[ost])

            if iters:
                stage_qk(0)
            for j in range(len(iters)):
                if j + 1 < len(iters):
                    stage_qk(j + 1)
                stage_pv(j)
            k.barrier()

    aff_all = k.sbuf(es, "aff_all", [128, NT, NEXP], F32)

    def phase_merge(l, ntiles):
        with ExitStack() as st:
            wo = k.sbuf(st, "w_out_s", [128, 8, D], BF16)
            k.dma("pool", wo[:], I.w_out[l].rearrange("(c p) n -> p c n", p=128), writes=[wo])
            wr = k.sbuf(st, "w_r_s", [128, 8, NEXP], F32)
            k.dma("sp", wr[:], I.w_router[l].rearrange("(c p) e -> p c e", p=128), writes=[wr])
            gout = load_bcast(st, "gout_b", I.g_out[l])
            nr = 2 if ntiles > 32 else 1
            gate2 = [load_bcast(st, f"gate2_{r}", modvec(l, r, 2)) for r in range(nr)]
            gm4 = [make_gm(st, f"gm4_{r}", l, r, 4, I.g_ffn[l]) for r in range(nr)]
            sh4 = [load_bcast(st, f"sh4_{r}", modvec(l, r, 3)) for r in range(nr)]
            inv3 = k.sbuf(st, "inv3", [128, 3], F32)
            k.op("dve", lambda: nc.vector.memset(inv3[:, 0:1], 1.0 / 512), [], [inv3])
            k.op("dve", lambda: nc.vector.memset(inv3[:, 1:3], 1.0 / 256), [], [inv3])
            ots = [k.sbuf(st, f"ot{i}", [128, D], F32) for i in range(4)]
            xts = [k.sbuf(st, f"mxt{i}", [128, D], F32) for i in range(4)]
            def dbl(name, shape, dt):
                return [k.sbuf(st, f"{name}{i}", shape, dt) for i in range(2)]

            junk_ = dbl("mjunk", [128, D], BF16)
            ss3_ = dbl("ss3", [128, 3, 3], F32)
            yb_ = dbl("yb", [128, D], BF16)
            yT_ = dbl("yT", [128, 8, 128], BF16)
            t1_ = dbl("t1", [128, D], F32)
            xn_ = dbl("xn", [128, D], F32)
            ss_ = dbl("mss", [128, 2], F32)
            rstd_ = dbl("mrstd", [128, 1], F32)
            hn2_ = dbl("hn2", [128, D], F32)
            h2f_ = dbl("h2f", [128, D], F32)
            h2b_ = dbl("h2b", [128, D], BF16)
            h2T_ = dbl("h2T", [128, 8, 128], F32)
            sm_ = dbl("smx", [128, 4], F32)
            ex_ = dbl("sex", [128, NEXP], F32)
            tps_ = [k.psum(st, f"mtps{i}", [128, 8, 128], BF16) for i in range(2)]
            pps_ = [k.psum(st, f"mpps{i}", [128, D], F32) for i in range(2)]
            rtp = k.psum(st, "rtp", [128, 4, 128], F32)
            lps1 = k.psum(st, "lps", [128, NEXP], F32)
            lps_ = [lps1, lps1]
            grp = ((0, 512), (512, 768), (768, 1024))
            def setup(t):
                pz = t % 2
                return (0 if t < 32 else 1, ots[t % 4], xts[t % 4], junk_[pz], ss3_[pz], yb_[pz], yT_[pz], t1_[pz], xn_[pz], ss_[pz],
                        rstd_[pz], hn2_[pz], h2f_[pz], h2b_[pz], h2T_[pz], sm_[pz], ex_[pz], tps_[pz], lps_[pz], pps_[pz])

            def stage_a(t):
                (r, ot, xt, junk, ss3, yb, yT, t1, xn, ss, rstd, hn2, h2f, h2b, h2T, sm, ex, tps, lps, pps) = setup(t)
                for t2 in ([0, 1, 2] if t == 0 else [t + 2]):
                    if t2 < ntiles:
                        k.dma("pool", ots[t2 % 4][:], od[t2 * 128:(t2 + 1) * 128, :], writes=[ots[t2 % 4]])
                        k.dma("pool", xts[t2 % 4][:], xres[t2 * 128:(t2 + 1) * 128, :], writes=[xts[t2 % 4]])
                for gi, (a, b) in enumerate(grp):
                    k.actf(junk[:, a:b], ot[:, a:b], ACT.Square, [ot], [junk, ss3], accum_out=ss3[:, 0, gi:gi + 1])
                k.tt("dve", ss3[:, 1, :], ss3[:, 0, :], inv3[:], ALU.mult, [ss3, inv3], [ss3])
                k.actf(ss3[:, 2, :], ss3[:, 1, :], ACT.Sqrt, [ss3, epsb], [ss3], bias=epsb[:, 0:1])
                k.op("dve", lambda: nc.vector.reciprocal(out=ss3[:, 0, :], in_=ss3[:, 2, :]), [ss3], [ss3])
                for gi, (a, b) in enumerate(grp):
                    k.stt("dve", yb[:, a:b], ot[:, a:b], ss3[:, 0, gi:gi + 1], gout[:, a:b],
                          ALU.mult, ALU.mult, [ot, ss3, gout], [yb])
                for dc in range(8):
                    k.tr(tps[:, dc, :], yb[:, dc * 128:(dc + 1) * 128], ident_bf[:], [yb, ident_bf], [tps])
                k.cp("act", yT[:], tps[:], [tps], [yT])
                for hf in range(2):
                    for dc in range(8):
                        k.mm(pps[:, hf * 512:(hf + 1) * 512], yT[:, dc, :], wo[:, dc, hf * 512:(hf + 1) * 512],
                             dc == 0, dc == 7, [yT, wo], [pps])

            def stage_b(t):
                (r, ot, xt, junk, ss3, yb, yT, t1, xn, ss, rstd, hn2, h2f, h2b, h2T, sm, ex, tps, lps, pps) = setup(t)
                k.tt("dve", t1[:], pps[:], gate2[r][:], ALU.mult, [pps, gate2[r]], [t1])
                k.tt("dve", xn[:], t1[:], xt[:], ALU.add, [t1, xt], [xn])
                k.dma("sp", xres[t * 128:(t + 1) * 128, :], xn[:], reads=[xn])
                rms_rstd(xn[:], ss, rstd, junk, D, [xn])
                k.stt("dve", hn2[:], xn[:], rstd[:, 0:1], gm4[r][:], ALU.mult, ALU.mult, [xn, rstd, gm4[r]], [hn2])
                k.tt("dve", h2f[:], hn2[:], sh4[r][:], ALU.add, [hn2, sh4[r]], [h2f])
                k.cp("act", h2b[:], h2f[:], [h2f], [h2b])
                k.dma("sp", hbf[t * 128:(t + 1) * 128, :], h2b[:], reads=[h2b])
                for hp in range(2):
                    for dc in range(4):
                        k.tr(rtp[:, dc, :], h2f[:, (hp * 4 + dc) * 128:(hp * 4 + dc + 1) * 128], ident_f[:], [h2f, ident_f], [rtp])
                    k.cp("dve" if hp else "act", h2T[:, hp * 4:(hp + 1) * 4, :], rtp[:], [rtp], [h2T])
                for dc in range(8):
                    k.mm(lps[:], h2T[:, dc, :], wr[:, dc, :], dc == 0, dc == 7, [h2T, wr], [lps])
                k.op("dve", lambda: nc.vector.reduce_max(out=sm[:, 0:1], in_=lps[:], axis=AX.X), [lps], [sm])
                k.ts("dve", sm[:, 1:2], sm[:, 0:1], -1.0, None, ALU.mult, None, [sm], [sm])
                k.actf(ex[:], lps[:], ACT.Exp, [lps, sm], [ex, sm], bias=sm[:, 1:2], accum_out=sm[:, 2:3])
                k.op("dve", lambda: nc.vector.reciprocal(out=sm[:, 3:4], in_=sm[:, 2:3]), [sm], [sm])
                k.ts("dve", aff_all[:, t, :], ex[:], sm[:, 3:4], None, ALU.mult, None, [ex, sm], [aff_all])

            stage_a(0)
            for t in range(ntiles):
                if t + 1 < ntiles:
                    stage_a(t + 1)
                stage_b(t)
            k.barrier()

    sel_all = k.sbuf(es, "sel_all", [128, NT, NEXP], F32)
    posm_all = k.sbuf(es, "posm_all", [128, NT, NEXP], F32)
    nv = k.sbuf(es, "nv", [128, NT, NEXP, 5], BF16)
    idx_all = k.sbuf(es, "idx_all", [128, NEXP, 5], I32)
    gate_all = k.sbuf(es, "gate_all", [128, NEXP, 5], F32)
    k.op("dve", lambda: nc.vector.memset(idx_all[:], 0), [], [idx_all])
    k.op("dve", lambda: nc.vector.memset(gate_all[:], 0.0), [], [gate_all])
    LAT = (0, 32, 512, 0)
    CTXS = (32, 2, 32, 4)

    def phase_route(l, sets):
        with ExitStack() as st:
            ones_bf = k.sbuf(st, "ones_bf", [128, 128], BF16)
            k.op("dve", lambda: nc.vector.memset(ones_bf[:], 1.0), [], [ones_bf])
            ones_f = k.sbuf(st, "ones_f", [128, 128], F32)
            k.op("dve", lambda: nc.vector.memset(ones_f[:], 1.0), [], [ones_f])
            tri = k.sbuf(st, "tri_s", [128, 128], F32)
            k.dma("sp", tri[:], I.tri_f, writes=[tri])
            tok = k.sbuf(st, "tok_s", [128, NT, 2], F32)
            k.dma("sp", tok[:], I.tokhl, writes=[tok])
            cum = k.sbuf(st, "r_cum", [128, NEXP], F32)
            pps_ = [k.psum(st, f"r_pps{i}", [128, NEXP], F32) for i in range(2)]
            r1 = k.sbuf(st, "r_r1", [128, NT, NEXP], F32)
            r2 = k.sbuf(st, "r_r2", [128, NT, NEXP], F32)
            chains = []
            for (t0, ntl, cap, _c) in sets:
                for (e0, ne) in (((0, 8), (8, 8)) if ntl > 8 else ((0, NEXP),)):
                    ci = len(chains)
                    c = dict(t0=t0, ntl=ntl, cap=cap, e0=e0, ne=ne)
                    for nm in ("lo", "hi", "sm", "mid", "cnt", "t", "u"):
                        c[nm] = k.sbuf(st, f"r_{nm}{ci}", [128, ne], F32)
                    c["cmp"] = k.sbuf(st, f"r_cmp{ci}", [128, ntl, ne], BF16)
                    c["cps"] = k.psum(st, f"r_cps{ci}", [128, 512], F32)
                    chains.append(c)
            for c in chains:
                k.op("dve", lambda: nc.vector.memset(c["lo"][:], 0.0), [], [c["lo"]])
                k.op("dve", lambda: nc.vector.memset(c["hi"][:], 1.0), [], [c["hi"]])

            def affs_of(c):
                return aff_all[:, c["t0"]:c["t0"] + c["ntl"], c["e0"]:c["e0"] + c["ne"]]

            for it in range(32):
                for c in chains:
                    k.tt("dve", c["sm"][:], c["lo"][:], c["hi"][:], ALU.add, [c["lo"], c["hi"]], [c["sm"]])
                for c in chains:
                    k.ts("dve", c["mid"][:], c["sm"][:], 0.5, None, ALU.mult, None, [c["sm"]], [c["mid"]])
                for c in chains:
                    k.tt("dve", c["cmp"][:], affs_of(c), c["mid"][:].unsqueeze(1).broadcast_to([128, c["ntl"], c["ne"]]),
                         ALU.is_ge, [aff_all, c["mid"]], [c["cmp"]])
                for c in chains:
                    n_ = c["ntl"] * c["ne"]
                    k.mm(c["cps"][:, 0:n_], ones_bf[:], c["cmp"][:].rearrange("p t e -> p (t e)"), True, True,
                         [ones_bf, c["cmp"]], [c["cps"]])
                for c in chains:
                    n_ = c["ntl"] * c["ne"]
                    k.op("dve", lambda: nc.vector.tensor_reduce(
                        out=c["cnt"][:], in_=c["cps"][:, 0:n_].rearrange("p (t e) -> p e t", e=c["ne"]), axis=AX.X, op=ALU.add),
                        [c["cps"]], [c["cnt"]])
                for c in chains:
                    k.stt("dve", c["t"][:], c["cnt"][:], float(c["cap"]), c["mid"][:], ALU.is_ge, ALU.mult, [c["cnt"], c["mid"]], [c["t"]])
                for c in chains:
                    k.stt("dve", c["u"][:], c["cnt"][:], float(c["cap"]), c["mid"][:], ALU.is_ge, ALU.max, [c["cnt"], c["mid"]], [c["u"]])
                for c in chains:
                    k.tt("dve", c["lo"][:], c["lo"][:], c["t"][:], ALU.max, [c["lo"], c["t"]], [c["lo"]])
                for c in chains:
                    k.tt("dve", c["hi"][:], c["hi"][:], c["u"][:], ALU.min, [c["hi"], c["u"]], [c["hi"]])
            for c in chains:
                k.tt("dve", sel_all[:, c["t0"]:c["t0"] + c["ntl"], c["e0"]:c["e0"] + c["ne"]], affs_of(c),
                     c["lo"][:].unsqueeze(1).broadcast_to([128, c["ntl"], c["ne"]]), ALU.is_ge, [aff_all, c["lo"]], [sel_all])
            for (t0, ntl, cap, _c) in sets:
                k.op("dve", lambda: nc.vector.memset(cum[:], 0.0), [], [cum])
                for ti in range(ntl):
                    t = t0 + ti
                    pp = pps_[ti % 2]
                    k.mm(pp[:], tri[:], sel_all[:, t, :], True, False, [tri, sel_all], [pp])
                    k.mm(pp[:], ones_f[:], cum[:], False, True, [ones_f, cum], [pp])
                    k.stt("dve", posm_all[:, t, :], pp[:], 1.0, sel_all[:, t, :], ALU.add, ALU.mult, [pp, sel_all], [posm_all])
                    k.tt("dve", cum[:], cum[:], sel_all[:, t, :], ALU.add, [cum, sel_all], [cum])
                k.ts("dve", posm_all[:, t0:t0 + ntl, :], posm_all[:, t0:t0 + ntl, :], -1.0, None, ALU.add, None,
                     [posm_all], [posm_all])
            tl = NT if len(sets) > 1 else 32
            k.cp("dve", nv[:, 0:tl, :, 0:2], tok[:, 0:tl, :].unsqueeze(2).broadcast_to([128, tl, NEXP, 2]), [tok], [nv])
            k.cp("dve", nv[:, 0:tl, :, 2], aff_all[:, 0:tl, :], [aff_all], [nv])
            k.tt("dve", r1[:, 0:tl, :], aff_all[:, 0:tl, :], nv[:, 0:tl, :, 2], ALU.subtract, [aff_all, nv], [r1])
            k.cp("dve", nv[:, 0:tl, :, 3], r1[:, 0:tl, :], [r1], [nv])
            k.tt("dve", r2[:, 0:tl, :], r1[:, 0:tl, :], nv[:, 0:tl, :, 3], ALU.subtract, [r1, nv], [r2])
            k.cp("dve", nv[:, 0:tl, :, 4], r2[:, 0:tl, :], [r2], [nv])
            k.barrier()

    def phase_dispatch(l, sets):
        with ExitStack() as st:
            iota = k.sbuf(st, "iota_s", [128, 512], F32)
            k.dma("sp", iota[:], I.iota_slots, writes=[iota])
            ohs = [k.sbuf(st, f"oh{i}", [128, 512], BF16) for i in range(4)]
            igp = [k.psum(st, f"igp{i}", [128, 4, 8], F32) for i in range(2)]
            igs = k.sbuf(st, "igs", [128, 4, 8], F32)
            idxf = k.sbuf(st, "idxf", [128, 4], F32)
            g2 = k.sbuf(st, "g2", [128, 4], F32)
            Xs = [k.sbuf(st, f"Xs{i}", [128, 4, D], BF16) for i in range(2)]
            tpp = [k.psum(st, f"dtp{i}", [128, 512], BF16) for i in range(2)]
            XTs = [k.sbuf(st, f"XTs{i}", [128, 8, 512], BF16) for i in range(2)]
            oi = 0
            ji = 0
            for e in range(NEXP):
                for (t0, ntl, cap, c0) in sets:
                    nsc = (cap + 127) // 128
                    ig = igp[ji % 2]
                    X = Xs[ji % 2]
                    XT = XTs[ji % 2]
                    ji += 1
                    for ti in range(ntl):
                        t = t0 + ti
                        oh = ohs[oi % 4]
                        k.ts("dve", oh[:, 0:cap], iota[:, 0:cap], posm_all[:, t, e:e + 1], None,
                             ALU.is_equal, None, [iota, posm_all], [oh])
                        oi += 1
                        for sc in range(nsc):
                            sl = min(128, cap - sc * 128)
                            k.mm(ig[0:sl, sc, 0:5], oh[:, sc * 128:sc * 128 + sl], nv[:, t, e, :],
                                 ti == 0 and sc == 0, ti == ntl - 1, [oh, nv], [ig], inc=(sc == nsc - 1),
                                 skip_group_check=True)
                    sl0 = min(128, cap)
                    k.cp("act", igs[0:sl0, 0:nsc, 0:5], ig[0:sl0, 0:nsc, 0:5], [ig], [igs])
                    k.stt("dve", idxf[0:sl0, 0:nsc], igs[0:sl0, 0:nsc, 0], 64.0, igs[0:sl0, 0:nsc, 1], ALU.mult, ALU.add,
                          [igs], [idxf])
                    k.cp("dve", idx_all[0:sl0, e, c0:c0 + nsc], idxf[0:sl0, 0:nsc], [idxf], [idx_all])
                    k.tt("dve", g2[0:sl0, 0:nsc], igs[0:sl0, 0:nsc, 2], igs[0:sl0, 0:nsc, 3], ALU.add, [igs], [g2])
                    k.tt("dve", gate_all[0:sl0, e, c0:c0 + nsc], g2[0:sl0, 0:nsc], igs[0:sl0, 0:nsc, 4], ALU.add,
                         [g2, igs], [gate_all])
                    for sc in range(nsc):
                        sl = min(128, cap - sc * 128)
                        k.dma("pool", None, None, reads=[idx_all], writes=[X], fn=lambda: nc.gpsimd.indirect_dma_start(
                            out=X[0:sl, sc, :], out_offset=None, in_=hbf,
                            in_offset=bass.IndirectOffsetOnAxis(ap=idx_all[0:sl, e, c0 + sc:c0 + sc + 1], axis=0),
                            bounds_check=breg, oob_is_err=False))
                    for dc in range(8):
                        tp = tpp[dc % 2]
                        for sc in range(nsc):
                            sl = min(128, cap - sc * 128)
                            k.tr(tp[:, sc * 128:sc * 128 + sl], X[0:sl, sc, dc * 128:(dc + 1) * 128], ident_bf[0:sl, 0:sl],
                                 [X, ident_bf], [tp])
                        k.cp("act" if dc % 2 else "dve", XT[:, dc, 0:cap], tp[:, 0:cap], [tp], [XT])
                    col0 = 0 if c0 == 0 else 512
                    k.dma("sp", xtd[e, :, :, col0:col0 + cap], XT[:, :, 0:cap], reads=[XT])
            k.barrier()

    def phase_ffn(l, sets):
        with ExitStack() as st:
            g5 = [load_bcast(st, f"gate5_{r}", modvec(l, r, 5)) for r in range(len(sets))]
            xts = [k.sbuf(st, f"fxt{i}", [128, 8, 544], BF16) for i in range(2)]
            AT = k.sbuf(st, "AT", [128, 16, 544], BF16)
            stg = [k.sbuf(st, f"wstg{i}", [128, 2048], F32) for i in range(5)]
            wgb = [k.sbuf(st, f"wgb{i}", [128, 8, 512], BF16) for i in range(2)]
            wub = [k.sbuf(st, f"wub{i}", [128, 8, 512], BF16) for i in range(2)]
            wdb = k.sbuf(st, "wdb", [128, 16, D], BF16)
            sgs = [k.sbuf(st, f"sg{i}", [128, 512], F32) for i in range(2)]
            ysb = [k.sbuf(st, f"ysb{i}", [128, D], F32) for i in range(10)]
            gps = [k.psum(st, f"gps{i}", [128, 512], F32) for i in range(2)]
            ups = [k.psum(st, f"ups{i}", [128, 512], F32) for i in range(2)]
            cps_ = k.psum(st, "fcps", [128, 2, 32], F32)
            yps = [k.psum(st, f"yps{i}", [128, 512], F32) for i in range(2)]
            nctx = 32 if len(sets) > 1 else 0
            ncols = 512 + nctx
            si = [0]
            pending = []
            prev_sc_toks = []

            def stage_cast(src_ap, dst_ap, dstbuf, shape3):
                sg_ = stg[si[0] % 5]
                si[0] += 1
                v = sg_[:].rearrange("p (a b) -> p a b", a=shape3[0])
                k.dma("sp", v, src_ap, writes=[sg_])
                k.cp("dve" if si[0] % 2 else "act", dst_ap, v, [sg_], [dstbuf])

            def flush():
                nonlocal prev_sc_toks
                if not pending:
                    return
                for tok in prev_sc_toks:
                    k.wait("pool", tok)
                toks = []
                for (fn_, rd) in pending:
                    toks.append(k.dma("pool", None, None, reads=rd, fn=fn_))
                prev_sc_toks = toks
                pending.clear()

            it = 0
            yi = 0
            for e in range(NEXP):
                xt = xts[e % 2]
                k.dma("act", xt[:, :, 0:ncols], xtd[e, :, :, 0:ncols], writes=[xt])
                wgv = I.w_gate[l, e].rearrange("(c p) f -> p c f", p=128)
                wuv = I.w_up[l, e].rearrange("(c p) f -> p c f", p=128)
                wdv = I.w_down[l, e].rearrange("(c p) d -> p c d", p=128)
                for fb in range(4):
                    wg_, wu_ = wgb[fb % 2], wub[fb % 2]
                    for ch in range(2):
                        stage_cast(wgv[:, ch * 4:(ch + 1) * 4, fb * 512:(fb + 1) * 512], wg_[:, ch * 4:(ch + 1) * 4, :], wg_, (4, 512))
                    for ch in range(2):
                        stage_cast(wuv[:, ch * 4:(ch + 1) * 4, fb * 512:(fb + 1) * 512], wu_[:, ch * 4:(ch + 1) * 4, :], wu_, (4, 512))
                    for u in (2 * fb, 2 * fb + 1):
                        stage_cast(wdv[:, 2 * u:2 * u + 2, :], wdb[:, 2 * u:2 * u + 2, :], wdb, (2, 1024))
                    if fb == 1:
                        flush()
                    for fc in range(4):
                        f = fb * 4 + fc
                        gp, up = gps[it % 2], ups[it % 2]
                        sg = sgs[it % 2]
                        it += 1
                        for dc in range(8):
                            k.mm(gp[:], wg_[:, dc, fc * 128:(fc + 1) * 128], xt[:, dc, 0:512], dc == 0, dc == 7, [wg_, xt], [gp])
                        for dc in range(8):
                            k.mm(up[:], wu_[:, dc, fc * 128:(fc + 1) * 128], xt[:, dc, 0:512], dc == 0, dc == 7, [wu_, xt], [up])
                        k.actf(sg[:], gp[:], ACT.Silu, [gp], [sg])
                        k.tt("dve", AT[:, f, 0:512], sg[:], up[:], ALU.mult, [sg, up], [AT])
                        if nctx:
                            for dc in range(8):
                                k.mm(cps_[:, 0, :], wg_[:, dc, fc * 128:(fc + 1) * 128], xt[:, dc, 512:544], dc == 0, dc == 7,
                                     [wg_, xt], [cps_])
                            for dc in range(8):
                                k.mm(cps_[:, 1, :], wu_[:, dc, fc * 128:(fc + 1) * 128], xt[:, dc, 512:544], dc == 0, dc == 7,
                                     [wu_, xt], [cps_])
                            k.actf(sg[:, 0:32], cps_[:, 0, :], ACT.Silu, [cps_], [sg])
                            k.tt("dve", AT[:, f, 512:544], sg[:, 0:32], cps_[:, 1, :], ALU.mult, [sg, cps_], [AT])
                for si_, (t0, ntl, cap, c0) in enumerate(sets):
                    nsc = (cap + 127) // 128
                    acol = 0 if c0 == 0 else 512
                    for sc in range(nsc):
                        sl = min(128, cap - sc * 128)
                        ys = ysb[yi % 10]
                        yi += 1
                        for hf in range(2):
                            yp = yps[hf]
                            for f in range(16):
                                k.mm(yp[0:sl, :], AT[:, f, acol + sc * 128:acol + sc * 128 + sl], wdb[:, f, hf * 512:(hf + 1) * 512],
                                     f == 0, f == 15, [AT, wdb], [yp])
                            k.stt("dve", ys[0:sl, hf * 512:(hf + 1) * 512], yp[0:sl, :], gate_all[0:sl, e, c0 + sc:c0 + sc + 1],
                                  g5[si_][0:sl, hf * 512:(hf + 1) * 512], ALU.mult, ALU.mult, [yp, gate_all, g5[si_]], [ys])

                        def mk(ys=ys, sl=sl, e=e, col=c0 + sc):
                            return lambda: nc.gpsimd.indirect_dma_start(
                                out=xres, out_offset=bass.IndirectOffsetOnAxis(ap=idx_all[0:sl, e, col:col + 1], axis=0),
                                in_=ys[0:sl, :], in_offset=None, compute_op=ALU.add, bounds_check=breg)
                        pending.append((mk(), [ys, idx_all]))
            flush()
            k.barrier()

    def phase_final():
        with ExitStack() as st:
            gf = load_bcast(st, "gfin_b", I.g_final)
            xts = [k.sbuf(st, f"fnx{i}", [128, D], F32) for i in range(2)]
            junk = k.sbuf(st, "fnjunk", [128, D], BF16)
            ss = k.sbuf(st, "fnss", [128, 2], F32)
            rstd = k.sbuf(st, "fnrstd", [128, 1], F32)
            ys = [k.sbuf(st, f"fny{i}", [128, D], F32) for i in range(2)]
            for t in range(32):
                xt, y = xts[t % 2], ys[t % 2]
                k.dma("sp", xt[:], xres[t * 128:(t + 1) * 128, :], writes=[xt])
                rms_rstd(xt[:], ss, rstd, junk, D, [xt])
                k.stt("dve", y[:], xt[:], rstd[:, 0:1], gf[:], ALU.mult, ALU.mult, [xt, rstd, gf], [y])
                k.dma("act", out_d[t * 128:(t + 1) * 128, :], y[:], reads=[y])
            k.barrier()

    for l in range(layers):
        last = l == DEPTH - 1
        if "proj" not in skip:
            phase_proj(l)
        if stop_after == f"proj{l}":
            return finish(nc, k, es)
        if "attna" not in skip:
            phase_attn_a(l, not last)
        if stop_after == f"attna{l}":
            return finish(nc, k, es)
        if "fourier" not in skip:
            phase_fourier(l, not last)
        if stop_after == f"fourier{l}":
            return finish(nc, k, es)
        if "attnc" not in skip:
            phase_attn_c(l, not last)
        if stop_after == f"attnc{l}":
            return finish(nc, k, es)
        if "merge" not in skip:
            phase_merge(l, 32 if last else NT)
        if stop_after == f"merge{l}":
            if debug:
                k.dma("sp", dbg_aff, aff_all[:], reads=[aff_all])
                k.dma("sp", dbg_x.rearrange("(p a) d -> p (a d)", p=128), xres.rearrange("(p a) d -> p (a d)", p=128))
            return finish(nc, k, es)
        sets = [LAT] if last else [LAT, CTXS]
        if "moe" not in skip:
            phase_route(l, sets)
            if stop_after == f"route{l}":
                if debug:
                    k.dma("sp", dbg_pos, posm_all[:], reads=[posm_all])
                return finish(nc, k, es)
            phase_dispatch(l, sets)
            if stop_after == f"disp{l}":
                return finish(nc, k, es)
            phase_ffn(l, sets)
        if stop_after == f"moe{l}":
            if debug:
                k.dma("sp", dbg_x.rearrange("(p a) d -> p (a d)", p=128), xres.rearrange("(p a) d -> p (a d)", p=128))
                k.dma("sp", dbg_aff, aff_all[:], reads=[aff_all])
                k.dma("sp", dbg_pos, posm_all[:], reads=[posm_all])
                k.dma("sp", dbg_idx, idx_all[:], reads=[idx_all])
                k.dma("sp", dbg_gate, gate_all[:], reads=[gate_all])
            return finish(nc, k, es)
    if "final" not in skip:
        phase_final()
    return finish(nc, k, es)


def finish(nc, k, es):
    k.barrier()
    es.close()
    return nc


def make_in_maps(inputs, ncores=8):
    cst = host_consts()
    shared = {}
    for nm in ("c_ctx", "w_ada", "b_ada", "g_mix", "g_ffn", "w_in", "g_q", "g_k", "w_out", "w_router",
               "w_gate", "w_up", "w_down", "g_final"):
        shared[nm] = np.ascontiguousarray(inputs[nm], dtype=np.float32)
    shared["g_out"] = np.ascontiguousarray(
        np.concatenate([inputs["g_out_a"], inputs["g_out_b"], inputs["g_out_c"]], axis=-1), dtype=np.float32)
    shared["na_bias"] = na_bias_layout(inputs["rel_bias"])
    shared.update(cst)
    maps = []
    for b in range(ncores):
        m = dict(shared)
        m["x"] = np.ascontiguousarray(inputs["x"][b], dtype=np.float32)
        m["c"] = np.ascontiguousarray(inputs["c"][b], dtype=np.float32)
        m["ctx"] = np.ascontiguousarray(inputs["ctx"][b], dtype=np.float32)
        maps.append(m)
    return maps


def kernel(**inputs):
    nc = build_program()
    maps = make_in_maps(inputs, 8)
    maps = [{n: m[n] for n in nc._used_inputs} for m in maps]
    res = run_bass_kernel_spmd(nc, maps, core_ids=list(range(8)))
    return np.stack([np.asarray(r["out"], dtype=np.float32) for r in res.results], axis=0)
```

```python
from contextlib import ExitStack
import numpy as np
import ml_dtypes
import concourse.bass as bass
import concourse.mybir as mybir
from concourse.bass_utils import run_bass_kernel_spmd

F32 = mybir.dt.float32
BF16 = mybir.dt.bfloat16
I32 = mybir.dt.int32
ALU = mybir.AluOpType
ACT = mybir.ActivationFunctionType
AX = mybir.AxisListType

D = 1024
SEQ = 4096
CTX = 256
NTOK = SEQ + CTX
NT = NTOK // 128
DEPTH = 2
INW = 1792
NEXP = 16
FF = 2048
EPS = 1e-6


class Buf:
    def __init__(self, t):
        self.t = t
        self.w = None
        self.r = {}

    def __getitem__(self, idx):
        return self.t[idx]


class KB:
    NDS = {"sp": 16, "act": 8, "pool": 8}

    def __init__(self, nc, es):
        self.nc = nc
        self.es = es
        self.eng = dict(pe=nc.tensor, dve=nc.vector, act=nc.scalar, pool=nc.gpsimd, sp=nc.sync)
        self.csem = {e: es.enter_context(nc.semaphore("c_" + e)) for e in ("pe", "dve", "act", "pool")}
        self.ccnt = {e: 0 for e in self.csem}
        self.seen = {e: {} for e in self.eng}
        self.dsem = {q: [es.enter_context(nc.semaphore(f"d_{q}{i}")) for i in range(n)]
                     for q, n in self.NDS.items()}
        self.dcnt = {q: [0] * n for q, n in self.NDS.items()}
        self.dnext = {q: 0 for q in self.NDS}
        self.n_inst = 0

    def sbuf(self, st, name, shape, dtype):
        self.uid = getattr(self, "uid", 0) + 1
        return Buf(st.enter_context(self.nc.sbuf_tensor(f"{name}_{self.uid}", list(shape), dtype)))

    def psum(self, st, name, shape, dtype):
        self.uid = getattr(self, "uid", 0) + 1
        return Buf(st.enter_context(self.nc.psum_tensor(f"{name}_{self.uid}", list(shape), dtype)))

    def wait(self, eng, tok):
        sem, val = tok
        if self.seen[eng].get(sem, 0) >= val:
            return
        self.eng[eng].wait_ge(sem, val)
        self.seen[eng][sem] = val
        self.n_inst += 1

    def _deps(self, eng, reads, writes):
        toks = {}
        own = self.csem.get(eng)

        def add(tok, raw):
            if tok is None:
                return
            sem, val = tok
            if sem is own and eng == "pe":
                return
            if toks.get(sem, 0) < val:
                toks[sem] = val

        for b in reads:
            add(b.w, True)
        for b in writes:
            add(b.w, False)
            for sem, val in b.r.items():
                add((sem, val), False)
        for sem, val in toks.items():
            self.wait(eng, (sem, val))

    def _mark(self, tok, reads, writes):
        sem, val = tok
        for b in reads:
            if b.r.get(sem, 0) < val:
                b.r[sem] = val
        for b in writes:
            b.w = tok
            b.r = {}

    def op(self, eng, fn, reads=(), writes=(), inc=True):
        self._deps(eng, reads, writes)
        inst = fn()
        self.n_inst += 1
        if inc:
            self.ccnt[eng] += 1
            inst.then_inc(self.csem[eng], 1)
            tok = (self.csem[eng], self.ccnt[eng])
        else:
            tok = (self.csem[eng], self.ccnt[eng] + 1)
        self._mark(tok, reads, writes)
        return tok

    def dma(self, q, out, in_, reads=(), writes=(), fn=None, **kw):
        self._deps(q, reads, writes)
        i = self.dnext[q]
        self.dnext[q] = (i + 1) % len(self.dsem[q])
        sem = self.dsem[q][i]
        if self.dcnt[q][i] > 0:
            self.wait(q, (sem, self.dcnt[q][i]))
        if fn is None:
            inst = self.eng[q].dma_start(out=out, in_=in_, **kw)
        else:
            inst = fn()
        self.n_inst += 1
        self.dcnt[q][i] += 16
        inst.then_inc(sem, 16)
        tok = (sem, self.dcnt[q][i])
        self._mark(tok, reads, writes)
        return tok

    def barrier(self):
        toks = [(self.csem[e], self.ccnt[e]) for e in self.csem if self.ccnt[e] > 0]
        for q in self.dsem:
            for i, s in enumerate(self.dsem[q]):
                if self.dcnt[q][i] > 0:
                    toks.append((s, self.dcnt[q][i]))
        for e in self.eng:
            for t in toks:
                self.wait(e, t)

    def mm(self, out, lhsT, rhs, start, stop, reads, writes, inc=None, **kw):
        return self.op("pe", lambda: self.nc.tensor.matmul(out, lhsT=lhsT, rhs=rhs, start=start, stop=stop, **kw),
                       reads, writes, inc=(stop if inc is None else inc))

    def tr(self, out, in_, ident, reads, writes):
        return self.op("pe", lambda: self.nc.tensor.transpose(out, in_, ident), reads, writes)

    def actf(self, out, in_, func, reads, writes, **kw):
        return self.op("act", lambda: self.nc.scalar.activation(out=out, in_=in_, func=func, **kw), reads, writes)

    def cp(self, eng, out, in_, reads, writes):
        if eng == "act":
            return self.op("act", lambda: self.nc.scalar.copy(out=out, in_=in_), reads, writes)
        e = self.eng[eng]
        return self.op(eng, lambda: e.tensor_copy(out=out, in_=in_), reads, writes)

    def tt(self, eng, out, in0, in1, op, reads, writes):
        e = self.eng[eng]
        return self.op(eng, lambda: e.tensor_tensor(out=out, in0=in0, in1=in1, op=op), reads, writes)

    def ts(self, eng, out, in0, s1, s2, op0, op1, reads, writes, **kw):
        e = self.eng[eng]
        if s2 is None:
            return self.op(eng, lambda: e.tensor_scalar(out=out, in0=in0, scalar1=s1, scalar2=None, op0=op0, **kw),
                           reads, writes)
        return self.op(eng, lambda: e.tensor_scalar(out=out, in0=in0, scalar1=s1, scalar2=s2, op0=op0, op1=op1, **kw),
                       reads, writes)

    def stt(self, eng, out, in0, scalar, in1, op0, op1, reads, writes):
        e = self.eng[eng]
        return self.op(eng, lambda: e.scalar_tensor_tensor(out=out, in0=in0, scalar=scalar, in1=in1, op0=op0, op1=op1),
                       reads, writes)


def host_consts():
    c = {}
    c["ident_bf"] = np.eye(128, dtype=np.float32).astype(ml_dtypes.bfloat16)
    c["ident_f"] = np.eye(128, dtype=np.float32)
    t = np.arange(SEQ)
    row = (t // 64).astype(np.float64)
    col = (t % 64).astype(np.float64)
    inv = 10000.0 ** (-np.arange(16, dtype=np.float64) / 16)
    ang = np.stack([row[:, None] * inv, col[:, None] * inv], axis=1)
    c["rope_cos"] = np.cos(ang).reshape(SEQ, 32).astype(np.float32)
    c["rope_sin"] = np.sin(ang).reshape(SEQ, 32).astype(np.float32)
    j = np.arange(64)
    ph = 2 * np.pi * np.outer(j, j) / 64.0
    bd = np.zeros((256, 512), np.float64)
    for g in range(4):
        bd[g * 64:(g + 1) * 64, g * 64:(g + 1) * 64] = np.cos(ph)
        bd[g * 64:(g + 1) * 64, 256 + g * 64:256 + (g + 1) * 64] = np.sin(ph)
    c["chan_dft"] = bd.astype(np.float32).astype(ml_dtypes.bfloat16)

    def dft_tiles(n):
        nt = n // 128
        m = np.arange(n, dtype=np.int64)
        tab_c = np.cos(2 * np.pi * m / n)
        tab_s = -np.sin(2 * np.pi * m / n)
        kk = np.arange(n, dtype=np.int64).reshape(nt, 1, 1, 128)
        nn = (np.arange(nt, dtype=np.int64)[None, :] * 128 + np.arange(128, dtype=np.int64)[:, None]).reshape(1, 128, nt, 1)
        idx = (kk * nn) % n
        return (tab_c[idx].astype(np.float32).astype(ml_dtypes.bfloat16),
                tab_s[idx].astype(np.float32).astype(ml_dtypes.bfloat16))

    c["tri_f"] = np.triu(np.ones((128, 128), np.float32), 1)
    c["iota_slots"] = np.broadcast_to(np.arange(512, dtype=np.float32), (128, 512)).copy()
    p = np.arange(128)
    tk = np.zeros((128, NT, 2), np.float32)
    tk[:, :, 0] = 2 * np.arange(NT)[None, :] + (p // 64)[:, None]
    tk[:, :, 1] = (p % 64)[:, None]
    c["tokhl"] = tk
    c["dftc"], c["dfts"] = dft_tiles(SEQ)
    c["dftxc"], c["dftxs"] = dft_tiles(CTX)
    return c


_NA_IDX = None


def na_bias_layout(rel_bias):
    global _NA_IDX
    if _NA_IDX is None:
        variants = [(0, 0), (1, 0), (2, 0), (30, 27), (31, 27)]
        ro = np.zeros((5, 128, 5, 128), np.int64)
        co = np.zeros((5, 128, 5, 128), np.int64)
        ok = np.zeros((5, 128, 5, 128), bool)
        kl = np.arange(128).reshape(128, 1, 1)
        kj = np.arange(5).reshape(1, 5, 1)
        ql = np.arange(128).reshape(1, 1, 128)
        for v, (qi, s0) in enumerate(variants):
            r = 2 * qi + ql // 64
            cq = ql % 64
            kr = 2 * (s0 + kj) + kl // 64
            kc = kl % 64
            rs = np.clip(r - 4, 0, 56)
            cs = np.clip(cq - 8, 0, 48)
            inwin = (kr >= rs) & (kr < rs + 8) & (kc >= cs) & (kc < cs + 16)
            ok[v] = np.broadcast_to(inwin, (128, 5, 128))
            ro[v] = np.clip(np.broadcast_to(kr - r + 7, (128, 5, 128)), 0, 14)
            co[v] = np.clip(np.broadcast_to(kc - cq + 15, (128, 5, 128)), 0, 30)
        _NA_IDX = (ro, co, ok)
    ro, co, ok = _NA_IDX
    rb = np.asarray(rel_bias, np.float32)
    g = rb[:, :, ro, co]
    g = np.where(ok[None, None], g, np.float32(-30000.0))
    g = g.transpose(0, 2, 3, 4, 1, 5).reshape(rb.shape[0], 5, 128, 5, 512)
    return np.ascontiguousarray(g, dtype=np.float32)


def build_program(stop_after=None, debug=False, layers=DEPTH, skip=(), cqb=None, cvar=0, ext=(), outs=None):
    nc = bass.Bass("TRN2", target_bir_lowering=False)
    es = ExitStack()
    IN = {}
    SPEC = {
        "x": ([SEQ, D], F32),
        "c": ([D], F32),
        "ctx": ([CTX, D], F32),
        "c_ctx": ([D], F32),
        "w_ada": ([DEPTH, D, 6 * D], F32),
        "b_ada": ([DEPTH, 6 * D], F32),
        "g_mix": ([DEPTH, D], F32),
        "g_ffn": ([DEPTH, D], F32),
        "w_in": ([DEPTH, D, INW], F32),
        "g_q": ([DEPTH, 64], F32),
        "g_k": ([DEPTH, 64], F32),
        "g_out": ([DEPTH, D], F32),
        "w_out": ([DEPTH, D, D], F32),
        "w_router": ([DEPTH, D, NEXP], F32),
        "w_gate": ([DEPTH, NEXP, D, FF], F32),
        "w_up": ([DEPTH, NEXP, D, FF], F32),
        "w_down": ([DEPTH, NEXP, FF, D], F32),
        "g_final": ([D], F32),
        "ident_bf": ([128, 128], BF16),
        "ident_f": ([128, 128], F32),
        "rope_cos": ([SEQ, 32], F32),
        "rope_sin": ([SEQ, 32], F32),
        "chan_dft": ([256, 512], BF16),
        "dftc": ([32, 128, 32, 128], BF16),
        "dfts": ([32, 128, 32, 128], BF16),
        "dftxc": ([2, 128, 2, 128], BF16),
        "dftxs": ([2, 128, 2, 128], BF16),
        "na_bias": ([DEPTH, 5, 128, 5, 512], F32),
        "tri_f": ([128, 128], F32),
        "iota_slots": ([128, 512], F32),
        "tokhl": ([128, NT, 2], F32),
    }

    class _Lazy:
        def __getattr__(self, name):
            if name not in IN:
                shape, dt = SPEC[name]
                IN[name] = nc.dram_tensor(name, list(shape), dt, kind="ExternalInput").ap()
            return IN[name]

    I = _Lazy()
    nc._used_inputs = IN

    out_d = nc.dram_tensor("out", [SEQ, D], F32, kind="ExternalOutput").ap()

    skind = "ExternalOutput" if debug else "Internal"

    def dscr(name, shape, dtype):
        if name in ext:
            IN[name] = nc.dram_tensor(name, list(shape), dtype, kind="ExternalInput").ap()
            return IN[name]
        kind = skind if (outs is None or name in outs) else "Internal"
        return nc.dram_tensor(name, list(shape), dtype, kind=kind).ap()

    xres = dscr("xres", [NTOK, D], F32)
    modd = dscr("modd", [DEPTH, 2, 6 * D], F32)
    qat = dscr("qat", [128, NT, 4, 128], BF16)
    kat = dscr("kat", [128, NTOK], BF16)
    vav = dscr("vav", [128, NT, 384], BF16)
    ud = dscr("ud", [128, NT, 512], BF16)
    qnt = dscr("qnt", [64, 4, NTOK], BF16)
    knt = dscr("knt", [64, 4, NTOK], BF16)
    od = dscr("od", [NTOK, D], F32)
    hbf = dscr("hbf", [NTOK, D], BF16)
    xtd = dscr("xtd", [NEXP, 128, 8, 544], BF16)
    dbg_aff = dscr("dbg_aff", [128, NT, NEXP], F32) if debug else None
    dbg_x = dscr("dbg_x", [NTOK, D], F32) if debug else None
    dbg_pos = dscr("dbg_pos", [128, NT, NEXP], F32) if debug else None
    dbg_idx = dscr("dbg_idx", [128, NEXP, 5], I32) if debug else None
    dbg_gate = dscr("dbg_gate", [128, NEXP, 5], F32) if debug else None

    k = KB(nc, es)
    breg = nc.gpsimd.to_reg(NTOK - 1)

    ident_bf = k.sbuf(es, "ident_bf_s", [128, 128], BF16)
    ident_f = k.sbuf(es, "ident_f_s", [128, 128], F32)
    k.dma("sp", ident_bf[:], I.ident_bf, writes=[ident_bf])
    k.dma("sp", ident_f[:], I.ident_f, writes=[ident_f])

    for i in range(8 if "init" not in skip else 0):
        k.dma("sp", xres[i * 512:(i + 1) * 512, :].rearrange("(p a) d -> p (a d)", p=128),
              I.x[i * 512:(i + 1) * 512, :].rearrange("(p a) d -> p (a d)", p=128))
    if "init" not in skip:
        k.dma("sp", xres[SEQ:NTOK, :].rearrange("(p a) d -> p (a d)", p=128),
              I.ctx.rearrange("(p a) d -> p (a d)", p=128))

    def phase_ada():
        with ExitStack() as st:
            craw = k.sbuf(st, "craw", [128, 2, 8], F32)
            scT = k.sbuf(st, "scT", [128, 8, 2], F32)
            k.dma("sp", craw[:, 0, :], I.c.rearrange("(p c) -> p c", c=8), writes=[craw])
            k.dma("sp", craw[:, 1, :], I.c_ctx.rearrange("(p c) -> p c", c=8), writes=[craw])
            k.actf(scT[:].rearrange("p c r -> p r c"), craw[:], ACT.Silu, [craw], [scT])
            bias = k.sbuf(st, "adab", [2, DEPTH, 6 * D], F32)
            for l in range(DEPTH):
                k.dma("sp", bias[:, l, :], I.b_ada[l].partition_broadcast(2), writes=[bias])
            wts = [k.sbuf(st, f"adaw{i}", [128, 8, 512], F32) for i in range(2)]
            pss = [k.psum(st, f"adaps{i}", [2, 512], F32) for i in range(2)]
            mod = k.sbuf(st, "adamod", [2, DEPTH, 6 * D], F32)
            it = 0
            for l in range(DEPTH):
                wv = I.w_ada[l].rearrange("(p c) n -> p c n", c=8)
                for j in range(12):
                    wt = wts[it % 2]
                    ps = pss[it % 2]
                    it += 1
                    k.dma("sp" if it % 2 else "act", wt[:], wv[:, :, j * 512:(j + 1) * 512], writes=[wt])
                    for c8 in range(8):
                        k.mm(ps[:], scT[:, c8, :], wt[:, c8, :], c8 == 0, c8 == 7, [scT, wt], [ps])
                    k.tt("dve", mod[:, l, j * 512:(j + 1) * 512], ps[:], bias[:, l, j * 512:(j + 1) * 512], ALU.add,
                         [ps, bias], [mod])
                k.dma("sp", modd[l], mod[:, l, :], reads=[mod])
            k.barrier()

    if "ada" not in skip:
        phase_ada()
    if stop_after == "ada":
        return finish(nc, k, es)

    def load_bcast(st, name, src_ap, q="sp"):
        b = k.sbuf(st, name, [128, src_ap.shape[-1]], F32)
        k.dma(q, b[:], src_ap.partition_broadcast(128), writes=[b])
        return b

    def modvec(l, r, c):
        return modd[l, r, c * D:(c + 1) * D]

    def make_gm(st, name, l, r, c_scale, gvec):
        sc = load_bcast(st, name + "_s", modvec(l, r, c_scale))
        g = load_bcast(st, name + "_g", gvec)
        k.stt("dve", sc[:], sc[:], 1.0, g[:], ALU.add, ALU.mult, [sc, g], [sc])
        return sc

    def rms_rstd(xt, ss, rstd, junk, n, reads):
        k.actf(junk[:, 0:n], xt, ACT.Square, reads, [junk, ss], accum_out=ss[:, 0:1])
        k.actf(ss[:, 1:2], ss[:, 0:1], ACT.Sqrt, [ss, epsb], [ss], scale=1.0 / n, bias=epsb[:, 0:1])
        k.op("dve", lambda: nc.vector.reciprocal(out=rstd[:, 0:1], in_=ss[:, 1:2]), [ss], [rstd])

    epsb = k.sbuf(es, "epsb", [128, 1], F32)
    k.op("dve", lambda: nc.vector.memset(epsb[:], EPS), [], [epsb])

    def phase_proj(l):
        with ExitStack() as st:
            wsb = k.sbuf(st, "w_in_s", [128, 8, INW], BF16)
            k.dma("pool", wsb[:], I.w_in[l].rearrange("(c p) n -> p c n", p=128), writes=[wsb])
            cdft = k.sbuf(st, "cdft_s", [128, 2, 512], BF16)
            k.dma("sp", cdft[:], I.chan_dft.rearrange("(c p) n -> p c n", p=128), writes=[cdft])
            gm = [make_gm(st, f"gm1_{r}", l, r, 1, I.g_mix[l]) for r in range(2)]
            sh = [load_bcast(st, f"sh1_{r}", modvec(l, r, 0)) for r in range(2)]
            gq = load_bcast(st, "gq_b", I.g_q[l])
            gk = load_bcast(st, "gk_b", I.g_k[l])
            gqk = k.sbuf(st, "gqk", [128, 10, 64], F32)
            k.cp("dve", gqk[:, 0:8, :], gq[:].unsqueeze(1).broadcast_to([128, 8, 64]), [gq], [gqk])
            k.cp("dve", gqk[:, 8:10, :], gk[:].unsqueeze(1).broadcast_to([128, 2, 64]), [gk], [gqk])

            def dbl(name, shape, dt):
                return [k.sbuf(st, f"{name}{i}", shape, dt) for i in range(2)]

            xts = [k.sbuf(st, f"xt{i}", [128, D], F32) for i in range(4)]
            junk_ = dbl("junk", [128, D], BF16)
            ss_ = dbl("ss", [128, 2], F32)
            rstd_ = dbl("rstd", [128, 1], F32)
            hn_ = dbl("hn", [128, D], F32)
            hb_ = dbl("hb", [128, D], BF16)
            hT_ = dbl("hT", [128, 8, 512], BF16)
            hTv_ = [[Buf(h.t) for _ in range(4)] for h in hT_]
            tps_ = [k.psum(st, f"tps{i}", [128, 8, 128], BF16) for i in range(2)]
            pps_ = [k.psum(st, f"pps{i}", [128, 1024], F32) for i in range(2)]
            fps = [k.psum(st, f"fps{i}", [128, 512], F32) for i in range(2)]
            sq_ = dbl("sq", [128, 640], F32)
            ss10_ = dbl("ss10", [128, 2, 10], F32)
            qkn_ = dbl("qkn", [128, 10, 64], F32)
            rp_ = [[k.sbuf(st, f"rp{i}_{j}", [128, 10, 2, 16], F32) for i in range(4)] for j in range(2)]
            qkr_ = dbl("qkr", [128, 640], BF16)
            qst_ = dbl("qst", [128, 5, 128], BF16)
            vst_ = dbl("vst", [128, 384], BF16)
            cs = [k.sbuf(st, f"ropec{i}", [128, 2, 32], F32) for i in range(2)]
            ubT_ = dbl("ubT", [128, 2, 512], BF16)
            fst = [k.sbuf(st, f"fst{i}", [128, 512], BF16) for i in range(2)]
            ust_ = dbl("ust", [128, 512], BF16)
            fi = 0

            macros = [(m * 4, 4) for m in range(8)] + [(32, 2)]
            tiles = []
            for mi, (t0, ntl) in enumerate(macros):
                for tl in range(ntl):
                    tiles.append((mi, t0, ntl, tl))

            def setup(ti):
                mi, t0, ntl, tl = tiles[ti]
                t = t0 + tl
                pz = ti % 2
                return (mi, t0, ntl, tl, t, 0 if t < 32 else 1, pz, hT_[mi % 2], hTv_[mi % 2], xts[ti % 4], junk_[pz], ss_[pz],
                        rstd_[pz], hn_[pz], hb_[pz], tps_[pz], pps_[pz], sq_[pz], ss10_[pz], qkn_[pz], rp_[pz], qkr_[pz],
                        qst_[pz], vst_[pz], tps_[1 - pz])

            def stage_a(ti):
                (mi, t0, ntl, tl, t, r, pz, hT, hTv, xt, junk, ss, rstd, hn, hb, tps, pps, sq, ss10, qkn, rp, qkr, qst, vst,
                 qps) = setup(ti)
                for t2 in ([0, 1, 2] if ti == 0 else [ti + 2]):
                    if t2 < NT:
                        k.dma("pool", xts[t2 % 4][:], xres[t2 * 128:(t2 + 1) * 128, :], writes=[xts[t2 % 4]])
                rms_rstd(xt[:], ss, rstd, junk, D, [xt])
                k.stt("dve", hn[:], xt[:], rstd[:, 0:1], gm[r][:], ALU.mult, ALU.mult, [xt, rstd, gm[r]], [hn])
                k.tt("dve", hb[:], hn[:], sh[r][:], ALU.add, [hn, sh[r]], [hb])
                for dc in range(8):
                    k.tr(tps[:, dc, :], hb[:, dc * 128:(dc + 1) * 128], ident_bf[:], [hb, ident_bf], [tps])
                k.cp("act", hT[:, :, tl * 128:(tl + 1) * 128], tps[:], [tps], [hTv[tl]])
                for (c0, c1, o0) in ((0, 512, 0), (512, 768, 512), (1536, 1792, 768)):
                    for dc in range(8):
                        k.mm(pps[:, o0:o0 + (c1 - c0)], hT[:, dc, tl * 128:(tl + 1) * 128], wsb[:, dc, c0:c1],
                             dc == 0, dc == 7, [hTv[tl], wsb], [pps])
                k.cp("act", vst[:], pps[:, 640:1024], [pps], [vst])
                k.dma("sp", vav[:, t, :], vst[:], reads=[vst])
                k.actf(sq[:], pps[:, 0:640], ACT.Square, [pps], [sq])

            def stage_b(ti):
                (mi, t0, ntl, tl, t, r, pz, hT, hTv, xt, junk, ss, rstd, hn, hb, tps, pps, sq, ss10, qkn, rp, qkr, qst, vst,
                 qps) = setup(ti)
                k.op("dve", lambda: nc.vector.tensor_reduce(out=ss10[:, 0, :], in_=sq[:].rearrange("p (h d) -> p h d", d=64),
                                                            axis=AX.X, op=ALU.add), [sq], [ss10])
                k.actf(ss10[:, 1, :], ss10[:, 0, :], ACT.Sqrt, [ss10, epsb], [ss10], scale=1.0 / 64, bias=epsb[:, 0:1])
                k.op("dve", lambda: nc.vector.reciprocal(out=ss10[:, 0, :], in_=ss10[:, 1, :]), [ss10], [ss10])
                k.tt("dve", qkn[:], pps[:, 0:640].rearrange("p (h d) -> p h d", d=64),
                     ss10[:, 0, :].unsqueeze(2).broadcast_to([128, 10, 64]), ALU.mult, [pps, ss10], [qkn])
                qdst = qkr[:, 0:512].rearrange("p (hh g d) -> p g hh d", hh=4, g=2)
                kdst = qkr[:, 512:640].rearrange("p (g d) -> p g d", g=2)
                if r == 1:
                    k.tt("dve", qdst, qkn[:, 0:8, :].rearrange("p (g hh) d -> p g hh d", g=2),
                         gqk[:, 0:8, :].rearrange("p (g hh) d -> p g hh d", g=2), ALU.mult, [qkn, gqk], [qkr])
                    k.tt("dve", kdst, qkn[:, 8:10, :], gqk[:, 8:10, :], ALU.mult, [qkn, gqk], [qkr])
                else:
                    k.tt("dve", qkn[:], qkn[:], gqk[:], ALU.mult, [qkn, gqk], [qkn])
                    cst = cs[pz]
                    k.dma("act", cst[:, 0, :], I.rope_cos[t * 128:(t + 1) * 128, :], writes=[cst])
                    k.dma("act", cst[:, 1, :], I.rope_sin[t * 128:(t + 1) * 128, :], writes=[cst])
                    qv = qkn[:].rearrange("p h (a s f) -> p h a s f", a=2, s=2)
                    x1 = qv[:, :, :, 0, :]
                    x2 = qv[:, :, :, 1, :]
                    cosb = cst[:, 0, :].rearrange("p (a f) -> p a f", a=2).unsqueeze(1).broadcast_to([128, 10, 2, 16])
                    sinb = cst[:, 1, :].rearrange("p (a f) -> p a f", a=2).unsqueeze(1).broadcast_to([128, 10, 2, 16])
                    k.tt("dve", rp[0][:], x1, cosb, ALU.mult, [qkn, cst], [rp[0]])
                    k.tt("dve", rp[1][:], x2, sinb, ALU.mult, [qkn, cst], [rp[1]])
                    k.tt("dve", rp[2][:], x1, sinb, ALU.mult, [qkn, cst], [rp[2]])
                    k.tt("dve", rp[3][:], x2, cosb, ALU.mult, [qkn, cst], [rp[3]])
                    qd5 = qkr[:, 0:512].rearrange("p (hh g a s f) -> p g hh a s f", hh=4, g=2, a=2, s=2)
                    kd5 = qkr[:, 512:640].rearrange("p (g a s f) -> p g a s f", g=2, a=2, s=2)
                    for s_, (ra, rb, op_) in enumerate(((0, 1, ALU.subtract), (2, 3, ALU.add))):
                        for g in range(2):
                            k.tt("dve", qd5[:, g, :, :, s_, :], rp[ra][:, g * 4:(g + 1) * 4], rp[rb][:, g * 4:(g + 1) * 4],
                                 op_, [rp[ra], rp[rb]], [qkr])
                        k.tt("dve", kd5[:, :, :, s_, :], rp[ra][:, 8:10], rp[rb][:, 8:10], op_, [rp[ra], rp[rb]], [qkr])
                for j5 in range(5):
                    k.tr(qps[:, j5, :], qkr[:, j5 * 128:(j5 + 1) * 128], ident_bf[:], [qkr, ident_bf], [qps])
                k.cp("act", qst[:], qps[:, 0:5, :], [qps], [qst])
                k.dma("sp", qat[:, t, :, :], qst[:, 0:4, :], reads=[qst])
                k.dma("sp", kat[:, t * 128:(t + 1) * 128], qst[:, 4, :], reads=[qst])

            def stage_f(mi):
                t0, ntl = macros[mi]
                ncol = ntl * 128
                hT, hTv, ubT = hT_[mi % 2], hTv_[mi % 2], ubT_[mi % 2]
                for ci, c0 in enumerate((768, 896)):
                    fp = fps[ci % 2]
                    for dc in range(8):
                        k.mm(fp[:, 0:ncol], wsb[:, dc, c0:c0 + 128], hT[:, dc, 0:ncol], dc == 0, dc == 7, [wsb] + hTv[0:ntl], [fp])
                    k.cp("dve", ubT[:, ci, 0:ncol], fp[:, 0:ncol], [fp], [ubT])
                for ci in range(4):
                    c0 = (1024 if ci < 2 else 1280) + 128 * (ci % 2)
                    fp = fps[ci % 2]
                    for dc in range(8):
                        k.mm(fp[:, 0:ncol], wsb[:, dc, c0:c0 + 128], hT[:, dc, 0:ncol], dc == 0, dc == 7, [wsb] + hTv[0:ntl], [fp])
                    fs = fst[ci % 2]
                    k.cp("dve" if ci % 2 else "act", fs[:, 0:ncol], fp[:, 0:ncol], [fp], [fs])
                    dst = qnt if ci < 2 else knt
                    for hh in range(2):
                        k.dma("sp", dst[:, 2 * (ci % 2) + hh, t0 * 128:t0 * 128 + ncol], fs[hh * 64:(hh + 1) * 64, 0:ncol], reads=[fs])
                for tl in range(ntl):
                    t = t0 + tl
                    cps = fps[tl % 2]
                    ust = ust_[tl % 2]
                    for c2 in range(2):
                        k.mm(cps[:], ubT[:, c2, tl * 128:(tl + 1) * 128], cdft[:, c2, :], c2 == 0, c2 == 1, [ubT, cdft], [cps])
                    k.cp("act", ust[:], cps[:], [cps], [ust])
                    k.dma("sp", ud[:, t, :], ust[:], reads=[ust])

            stage_a(0)
            for ti in range(len(tiles)):
                if ti + 1 < len(tiles):
                    stage_a(ti + 1)
                stage_b(ti)
                mi, t0, ntl, tl = tiles[ti]
                if tl == ntl - 1:
                    stage_f(mi)
            k.barrier()

    def phase_attn_a(l, ctx_q):
        with ExitStack() as st:
            KaT = k.sbuf(st, "KaT", [128, NTOK], BF16)
            k.dma("sp", KaT[:], kat, writes=[KaT])
            vstg = k.sbuf(st, "vstg", [128, NT, 384], BF16)
            k.dma("sp", vstg[:], vav, writes=[vstg])
            Vs = k.sbuf(st, "Vs", [128, NT, 2, 65], BF16)
            k.op("dve", lambda: nc.vector.memset(Vs[:, :, :, 64:65], 1.0), [], [Vs])
            k.cp("dve", Vs[:, :, :, 0:64], vstg[:, :, 0:128].rearrange("p t (g d) -> p t g d", g=2), [vstg], [Vs])
            qts = [k.sbuf(st, f"qt{i}", [128, 512], BF16) for i in range(2)]
            sps = [[k.psum(st, f"sps{i}_{g}", [128, 512], F32) for g in range(2)] for i in range(2)]
            ops_ = [[k.psum(st, f"ops{i}_{g}", [128, 4, 65], F32) for g in range(2)] for i in range(2)]
            pts = [[k.sbuf(st, f"pt{i}_{g}", [128, 512], BF16) for g in range(2)] for i in range(3)]
            rec = k.sbuf(st, "rec", [128, 4], F32)
            osts = [k.sbuf(st, f"ost{i}", [128, 4, 64], F32) for i in range(2)]
            iters = []
            nqb = NT if ctx_q else 32
            for qb in range(nqb):
                kts = list(range(NT)) if qb < 32 else [32, 33]
                for i, kt in enumerate(kts):
                    iters.append((qb, i, kt, len(kts)))

            def stage_qk(j):
                qb, i, kt, n = iters[j]
                qt = qts[qb % 2]
                if i == 0:
                    for q2 in ([qb, qb + 1] if qb == 0 else [qb + 1]):
                        if q2 < nqb:
                            k.dma("sp", qts[q2 % 2][:], qat[:, q2, :, :].rearrange("p h n -> p (h n)"), writes=[qts[q2 % 2]])
                for g in range(2):
                    k.mm(sps[j % 2][g][:], KaT[g * 64:(g + 1) * 64, kt * 128:(kt + 1) * 128], qt[g * 64:(g + 1) * 64, :],
                         True, True, [KaT, qt], [sps[j % 2][g]])
                for g in range(2):
                    k.actf(pts[j % 3][g][:], sps[j % 2][g][:], ACT.Exp, [sps[j % 2][g]], [pts[j % 3][g]], scale=0.125)

            def stage_pv(j):
                qb, i, kt, n = iters[j]
                for g in range(2):
                    pt = pts[j % 3][g]
                    op_ = ops_[qb % 2][g]
                    for hh in range(4):
                        k.mm(op_[:, hh, :], pt[:, hh * 128:(hh + 1) * 128], Vs[:, kt, g, :],
                             i == 0 and hh == 0, i == n - 1, [pt, Vs], [op_], skip_group_check=True)
                if i == n - 1:
                    for g in range(2):
                        op_ = ops_[qb % 2][g]
                        ost = osts[g]
                        k.op("dve", lambda: nc.vector.reciprocal(out=rec[:], in_=op_[:, :, 64]), [op_], [rec])
                        k.tt("dve", ost[:], op_[:, :, 0:64], rec[:].unsqueeze(2).broadcast_to([128, 4, 64]), ALU.mult,
                             [op_, rec], [ost])
                        k.dma("sp", od[qb * 128:(qb + 1) * 128, g * 256:(g + 1) * 256], ost[:].rearrange("p h d -> p (h d)"),
                              reads=[ost])

            stage_qk(0)
            for j in range(len(iters)):
                if j + 1 < len(iters):
                    stage_qk(j + 1)
                stage_pv(j)
            k.barrier()

    def phase_fourier(l, ctx_q):
        with ExitStack() as st:
            U = k.sbuf(st, "Ures", [128, NT, 512], BF16)
            k.dma("sp", U[:], ud, writes=[U])
            cb = [k.sbuf(st, f"dftcb{i}", [128, 32, 128], BF16) for i in range(4)]
            sb = [k.sbuf(st, f"dftsb{i}", [128, 32, 128], BF16) for i in range(4)]
            fps_ = [k.psum(st, f"fops{i}", [128, 256], F32) for i in range(2)]
            fos = [k.sbuf(st, f"fos{i}", [128, 256], F32) for i in range(2)]
            jobs = [(kt, 32, 0, I.dftc[kt], I.dfts[kt], 1.0 / 512) for kt in range(32)]
            if ctx_q:
                jobs += [(32 + kt, 2, 32, I.dftxc[kt], I.dftxs[kt], 1.0 / 128) for kt in range(2)]
            def load_job(jj):
                (_ot, nnt_, _t0, cd_, sd_, _scl) = jobs[jj]
                k.dma("sp", cb[jj % 4][:, 0:nnt_, :], cd_, writes=[cb[jj % 4]])
                k.dma("pool", sb[jj % 4][:, 0:nnt_, :], sd_, writes=[sb[jj % 4]])

            for jj in range(min(3, len(jobs))):
                load_job(jj)
            for ji, (ot, nnt, t0, cd, sd, scl) in enumerate(jobs):
                c_, s_ = cb[ji % 4], sb[ji % 4]
                if ji + 3 < len(jobs):
                    load_job(ji + 3)
                fp = fps_[ji % 2]
                for nt_ in range(nnt):
                    k.mm(fp[:], c_[:, nt_, :], U[:, t0 + nt_, 0:256], nt_ == 0, False, [c_, U], [fp])
                    k.mm(fp[:], s_[:, nt_, :], U[:, t0 + nt_, 256:512], False, nt_ == nnt - 1, [s_, U], [fp])
                fo = fos[ji % 2]
                k.actf(fo[:], fp[:], ACT.Copy, [fp], [fo], scale=scl)
                k.dma("sp", od[ot * 128:(ot + 1) * 128, 512:768], fo[:], reads=[fo])
            k.barrier()

    def phase_attn_c(l, ctx_q):
        with ExitStack() as st:
            import os
            CF = os.environ.get("CF", "kqvmcn")
            KnT = k.sbuf(st, "KnT", [64, 4, NTOK], BF16)
            QnT = k.sbuf(st, "QnT", [64, 4, NTOK], BF16)
            if "k" in CF:
                k.dma("sp", KnT[:], knt, writes=[KnT])
            if "q" in CF:
                k.dma("act", QnT[:], qnt, writes=[QnT])
            vstg = k.sbuf(st, "vstg", [128, NT, 384], BF16)
            if "v" in CF:
                k.dma("sp", vstg[:], vav, writes=[vstg])
            Vn = k.sbuf(st, "Vn", [128, NT, 4, 65], BF16)
            if "m" in CF:
                k.op("dve", lambda: nc.vector.memset(Vn[:, :, :, 64:65], 1.0), [], [Vn])
            if "c" in CF:
                k.cp("dve", Vn[:, :, :, 0:64], vstg[:, :, 128:384].rearrange("p t (h d) -> p t h d", h=4), [vstg], [Vn])
            nab_i = k.sbuf(st, "nab_i", [128, 5, 512], F32)
            nab_s = k.sbuf(st, "nab_s", [128, 5, 512], F32)
            for j in range(5):
                k.dma("sp", nab_i[:, j, :], I.na_bias[l, 2, :, j, :], writes=[nab_i])
            sps = [k.psum(st, f"csps{i}", [128, 4, 128], F32) for i in range(2)]
            ops_ = [k.psum(st, f"cops{i}", [128, 4, 65], F32) for i in range(2)]
            scs = [k.sbuf(st, f"csc{i}", [128, 512], F32) for i in range(2)]
            pts = [k.sbuf(st, f"cpt{i}", [128, 512], BF16) for i in range(3)]
            rec = k.sbuf(st, "crec", [128, 4], F32)
            osts = [k.sbuf(st, f"cost{i}", [128, 4, 64], F32) for i in range(2)]
            iters = []
            for qb in (cqb if cqb is not None else range(NT if ctx_q else 32)):
                if qb < 32:
                    s0 = min(max(qb - 2, 0), 27)
                    var = {0: 0, 1: 1, 30: 3, 31: 4}.get(qb, 2)
                    kts = [(s0 + j, j) for j in range(5)] + [(32, None), (33, None)]
                else:
                    var = 2
                    kts = [(32, None), (33, None)]
                for i, (kt, bj) in enumerate(kts):
                    iters.append((qb, i, kt, bj, var, len(kts)))

            def stage_qk(j):
                qb, i, kt, bj, var, n = iters[j]
                sp_, pt, sc = sps[j % 2], pts[j % 3], scs[j % 2]
                nab = nab_i
                if var != 2:
                    nab = nab_s
                    if i == 0:
                        for jj in range(5):
                            k.dma("sp", nab_s[:, jj, :], I.na_bias[l, var, :, jj, :], writes=[nab_s])
                for h in range(4):
                    k.mm(sp_[:, h, :], KnT[:, h, kt * 128:(kt + 1) * 128],
                         QnT[:, h, qb * 128:(qb + 1) * 128], True, True, [KnT, QnT], [sp_])
                spf = sp_[:].rearrange("p h n -> p (h n)")
                if bj is None:
                    k.actf(pt[:], spf, ACT.Exp, [sp_], [pt], scale=0.125)
                else:
                    k.stt("dve", sc[:], spf, 0.125, nab[:, bj, :], ALU.mult, ALU.add, [sp_, nab], [sc])
                    k.actf(pt[:], sc[:], ACT.Exp, [sc], [pt])

            def stage_pv(j):
                qb, i, kt, bj, var, n = iters[j]
                pt = pts[j % 3]
                op_ = ops_[qb % 2]
                for h in range(4):
                    k.mm(op_[:, h, :], pt[:, h * 128:(h + 1) * 128], Vn[:, kt, h, :],
                         i == 0 and h == 0, i == n - 1, [pt, Vn], [op_], skip_group_check=True)
                if i == n - 1:
                    ost = osts[qb % 2]
                    k.op("dve", lambda: nc.vector.reciprocal(out=rec[:], in_=op_[:, :, 64]), [op_], [rec])
                    k.tt("dve", ost[:], op_[:, :, 0:64], rec[:].unsqueeze(2).broadcast_to([128, 4, 64]), ALU.mult,
                         [op_, rec], [ost])
                    k.dma("sp", od[qb * 128:(qb + 1) * 128, 768:1024], ost[:].rearrange("p h d -> p (h d)"), reads=[ost])

            if iters:
                stage_qk(0)
            for j in range(len(iters)):
                if j + 1 < len(iters):
                    stage_qk(j + 1)
                stage_pv(j)
            k.barrier()

    aff_all = k.sbuf(es, "aff_all", [128, NT, NEXP], F32)

    def phase_merge(l, ntiles):
        with ExitStack() as st:
            wo = k.sbuf(st, "w_out_s", [128, 8, D], BF16)
            k.dma("pool", wo[:], I.w_out[l].rearrange("(c p) n -> p c n", p=128), writes=[wo])
            wr = k.sbuf(st, "w_r_s", [128, 8, NEXP], F32)
            k.dma("sp", wr[:], I.w_router[l].rearrange("(c p) e -> p c e", p=128), writes=[wr])
            gout = load_bcast(st, "gout_b", I.g_out[l])
            nr = 2 if ntiles > 32 else 1
            gate2 = [load_bcast(st, f"gate2_{r}", modvec(l, r, 2)) for r in range(nr)]
            gm4 = [make_gm(st, f"gm4_{r}", l, r, 4, I.g_ffn[l]) for r in range(nr)]
            sh4 = [load_bcast(st, f"sh4_{r}", modvec(l, r, 3)) for r in range(nr)]
            inv3 = k.sbuf(st, "inv3", [128, 3], F32)
            k.op("dve", lambda: nc.vector.memset(inv3[:, 0:1], 1.0 / 512), [], [inv3])
            k.op("dve", lambda: nc.vector.memset(inv3[:, 1:3], 1.0 / 256), [], [inv3])
            ots = [k.sbuf(st, f"ot{i}", [128, D], F32) for i in range(4)]
            xts = [k.sbuf(st, f"mxt{i}", [128, D], F32) for i in range(4)]
            def dbl(name, shape, dt):
                return [k.sbuf(st, f"{name}{i}", shape, dt) for i in range(2)]

            junk_ = dbl("mjunk", [128, D], BF16)
            ss3_ = dbl("ss3", [128, 3, 3], F32)
            yb_ = dbl("yb", [128, D], BF16)
            yT_ = dbl("yT", [128, 8, 128], BF16)
            t1_ = dbl("t1", [128, D], F32)
            xn_ = dbl("xn", [128, D], F32)
            ss_ = dbl("mss", [128, 2], F32)
            rstd_ = dbl("mrstd", [128, 1], F32)
            hn2_ = dbl("hn2", [128, D], F32)
            h2f_ = dbl("h2f", [128, D], F32)
            h2b_ = dbl("h2b", [128, D], BF16)
            h2T_ = dbl("h2T", [128, 8, 128], F32)
            sm_ = dbl("smx", [128, 4], F32)
            ex_ = dbl("sex", [128, NEXP], F32)
            tps_ = [k.psum(st, f"mtps{i}", [128, 8, 128], BF16) for i in range(2)]
            pps_ = [k.psum(st, f"mpps{i}", [128, D], F32) for i in range(2)]
            rtp = k.psum(st, "rtp", [128, 4, 128], F32)
            lps1 = k.psum(st, "lps", [128, NEXP], F32)
            lps_ = [lps1, lps1]
            grp = ((0, 512), (512, 768), (768, 1024))
            def setup(t):
                pz = t % 2
                return (0 if t < 32 else 1, ots[t % 4], xts[t % 4], junk_[pz], ss3_[pz], yb_[pz], yT_[pz], t1_[pz], xn_[pz], ss_[pz],
                        rstd_[pz], hn2_[pz], h2f_[pz], h2b_[pz], h2T_[pz], sm_[pz], ex_[pz], tps_[pz], lps_[pz], pps_[pz])

            def stage_a(t):
                (r, ot, xt, junk, ss3, yb, yT, t1, xn, ss, rstd, hn2, h2f, h2b, h2T, sm, ex, tps, lps, pps) = setup(t)
                for t2 in ([0, 1, 2] if t == 0 else [t + 2]):
                    if t2 < ntiles:
                        k.dma("pool", ots[t2 % 4][:], od[t2 * 128:(t2 + 1) * 128, :], writes=[ots[t2 % 4]])
                        k.dma("pool", xts[t2 % 4][:], xres[t2 * 128:(t2 + 1) * 128, :], writes=[xts[t2 % 4]])
                for gi, (a, b) in enumerate(grp):
                    k.actf(junk[:, a:b], ot[:, a:b], ACT.Square, [ot], [junk, ss3], accum_out=ss3[:, 0, gi:gi + 1])
                k.tt("dve", ss3[:, 1, :], ss3[:, 0, :], inv3[:], ALU.mult, [ss3, inv3], [ss3])
                k.actf(ss3[:, 2, :], ss3[:, 1, :], ACT.Sqrt, [ss3, epsb], [ss3], bias=epsb[:, 0:1])
                k.op("dve", lambda: nc.vector.reciprocal(out=ss3[:, 0, :], in_=ss3[:, 2, :]), [ss3], [ss3])
                for gi, (a, b) in enumerate(grp):
                    k.stt("dve", yb[:, a:b], ot[:, a:b], ss3[:, 0, gi:gi + 1], gout[:, a:b],
                          ALU.mult, ALU.mult, [ot, ss3, gout], [yb])
                for dc in range(8):
                    k.tr(tps[:, dc, :], yb[:, dc * 128:(dc + 1) * 128], ident_bf[:], [yb, ident_bf], [tps])
                k.cp("act", yT[:], tps[:], [tps], [yT])
                for hf in range(2):
                    for dc in range(8):
                        k.mm(pps[:, hf * 512:(hf + 1) * 512], yT[:, dc, :], wo[:, dc, hf * 512:(hf + 1) * 512],
                             dc == 0, dc == 7, [yT, wo], [pps])

            def stage_b(t):
                (r, ot, xt, junk, ss3, yb, yT, t1, xn, ss, rstd, hn2, h2f, h2b, h2T, sm, ex, tps, lps, pps) = setup(t)
                k.tt("dve", t1[:], pps[:], gate2[r][:], ALU.mult, [pps, gate2[r]], [t1])
                k.tt("dve", xn[:], t1[:], xt[:], ALU.add, [t1, xt], [xn])
                k.dma("sp", xres[t * 128:(t + 1) * 128, :], xn[:], reads=[xn])
                rms_rstd(xn[:], ss, rstd, junk, D, [xn])
                k.stt("dve", hn2[:], xn[:], rstd[:, 0:1], gm4[r][:], ALU.mult, ALU.mult, [xn, rstd, gm4[r]], [hn2])
                k.tt("dve", h2f[:], hn2[:], sh4[r][:], ALU.add, [hn2, sh4[r]], [h2f])
                k.cp("act", h2b[:], h2f[:], [h2f], [h2b])
                k.dma("sp", hbf[t * 128:(t + 1) * 128, :], h2b[:], reads=[h2b])
                for hp in range(2):
                    for dc in range(4):
                        k.tr(rtp[:, dc, :], h2f[:, (hp * 4 + dc) * 128:(hp * 4 + dc + 1) * 128], ident_f[:], [h2f, ident_f], [rtp])
                    k.cp("dve" if hp else "act", h2T[:, hp * 4:(hp + 1) * 4, :], rtp[:], [rtp], [h2T])
                for dc in range(8):
                    k.mm(lps[:], h2T[:, dc, :], wr[:, dc, :], dc == 0, dc == 7, [h2T, wr], [lps])
                k.op("dve", lambda: nc.vector.reduce_max(out=sm[:, 0:1], in_=lps[:], axis=AX.X), [lps], [sm])
                k.ts("dve", sm[:, 1:2], sm[:, 0:1], -1.0, None, ALU.mult, None, [sm], [sm])
                k.actf(ex[:], lps[:], ACT.Exp, [lps, sm], [ex, sm], bias=sm[:, 1:2], accum_out=sm[:, 2:3])
                k.op("dve", lambda: nc.vector.reciprocal(out=sm[:, 3:4], in_=sm[:, 2:3]), [sm], [sm])
                k.ts("dve", aff_all[:, t, :], ex[:], sm[:, 3:4], None, ALU.mult, None, [ex, sm], [aff_all])

            stage_a(0)
            for t in range(ntiles):
                if t + 1 < ntiles:
                    stage_a(t + 1)
                stage_b(t)
            k.barrier()

    sel_all = k.sbuf(es, "sel_all", [128, NT, NEXP], F32)
    posm_all = k.sbuf(es, "posm_all", [128, NT, NEXP], F32)
    nv = k.sbuf(es, "nv", [128, NT, NEXP, 5], BF16)
    idx_all = k.sbuf(es, "idx_all", [128, NEXP, 5], I32)
    gate_all = k.sbuf(es, "gate_all", [128, NEXP, 5], F32)
    k.op("dve", lambda: nc.vector.memset(idx_all[:], 0), [], [idx_all])
    k.op("dve", lambda: nc.vector.memset(gate_all[:], 0.0), [], [gate_all])
    LAT = (0, 32, 512, 0)
    CTXS = (32, 2, 32, 4)

    def phase_route(l, sets):
        with ExitStack() as st:
            ones_bf = k.sbuf(st, "ones_bf", [128, 128], BF16)
            k.op("dve", lambda: nc.vector.memset(ones_bf[:], 1.0), [], [ones_bf])
            ones_f = k.sbuf(st, "ones_f", [128, 128], F32)
            k.op("dve", lambda: nc.vector.memset(ones_f[:], 1.0), [], [ones_f])
            tri = k.sbuf(st, "tri_s", [128, 128], F32)
            k.dma("sp", tri[:], I.tri_f, writes=[tri])
            tok = k.sbuf(st, "tok_s", [128, NT, 2], F32)
            k.dma("sp", tok[:], I.tokhl, writes=[tok])
            cum = k.sbuf(st, "r_cum", [128, NEXP], F32)
            pps_ = [k.psum(st, f"r_pps{i}", [128, NEXP], F32) for i in range(2)]
            r1 = k.sbuf(st, "r_r1", [128, NT, NEXP], F32)
            r2 = k.sbuf(st, "r_r2", [128, NT, NEXP], F32)
            chains = []
            for (t0, ntl, cap, _c) in sets:
                for (e0, ne) in (((0, 8), (8, 8)) if ntl > 8 else ((0, NEXP),)):
                    ci = len(chains)
                    c = dict(t0=t0, ntl=ntl, cap=cap, e0=e0, ne=ne)
                    for nm in ("lo", "hi", "sm", "mid", "cnt", "t", "u"):
                        c[nm] = k.sbuf(st, f"r_{nm}{ci}", [128, ne], F32)
                    c["cmp"] = k.sbuf(st, f"r_cmp{ci}", [128, ntl, ne], BF16)
                    c["cps"] = k.psum(st, f"r_cps{ci}", [128, 512], F32)
                    chains.append(c)
            for c in chains:
                k.op("dve", lambda: nc.vector.memset(c["lo"][:], 0.0), [], [c["lo"]])
                k.op("dve", lambda: nc.vector.memset(c["hi"][:], 1.0), [], [c["hi"]])

            def affs_of(c):
                return aff_all[:, c["t0"]:c["t0"] + c["ntl"], c["e0"]:c["e0"] + c["ne"]]

            for it in range(32):
                for c in chains:
                    k.tt("dve", c["sm"][:], c["lo"][:], c["hi"][:], ALU.add, [c["lo"], c["hi"]], [c["sm"]])
                for c in chains:
                    k.ts("dve", c["mid"][:], c["sm"][:], 0.5, None, ALU.mult, None, [c["sm"]], [c["mid"]])
                for c in chains:
                    k.tt("dve", c["cmp"][:], affs_of(c), c["mid"][:].unsqueeze(1).broadcast_to([128, c["ntl"], c["ne"]]),
                         ALU.is_ge, [aff_all, c["mid"]], [c["cmp"]])
                for c in chains:
                    n_ = c["ntl"] * c["ne"]
                    k.mm(c["cps"][:, 0:n_], ones_bf[:], c["cmp"][:].rearrange("p t e -> p (t e)"), True, True,
                         [ones_bf, c["cmp"]], [c["cps"]])
                for c in chains:
                    n_ = c["ntl"] * c["ne"]
                    k.op("dve", lambda: nc.vector.tensor_reduce(
                        out=c["cnt"][:], in_=c["cps"][:, 0:n_].rearrange("p (t e) -> p e t", e=c["ne"]), axis=AX.X, op=ALU.add),
                        [c["cps"]], [c["cnt"]])
                for c in chains:
                    k.stt("dve", c["t"][:], c["cnt"][:], float(c["cap"]), c["mid"][:], ALU.is_ge, ALU.mult, [c["cnt"], c["mid"]], [c["t"]])
                for c in chains:
                    k.stt("dve", c["u"][:], c["cnt"][:], float(c["cap"]), c["mid"][:], ALU.is_ge, ALU.max, [c["cnt"], c["mid"]], [c["u"]])
                for c in chains:
                    k.tt("dve", c["lo"][:], c["lo"][:], c["t"][:], ALU.max, [c["lo"], c["t"]], [c["lo"]])
                for c in chains:
                    k.tt("dve", c["hi"][:], c["hi"][:], c["u"][:], ALU.min, [c["hi"], c["u"]], [c["hi"]])
            for c in chains:
                k.tt("dve", sel_all[:, c["t0"]:c["t0"] + c["ntl"], c["e0"]:c["e0"] + c["ne"]], affs_of(c),
                     c["lo"][:].unsqueeze(1).broadcast_to([128, c["ntl"], c["ne"]]), ALU.is_ge, [aff_all, c["lo"]], [sel_all])
            for (t0, ntl, cap, _c) in sets:
                k.op("dve", lambda: nc.vector.memset(cum[:], 0.0), [], [cum])
                for ti in range(ntl):
                    t = t0 + ti
                    pp = pps_[ti % 2]
                    k.mm(pp[:], tri[:], sel_all[:, t, :], True, False, [tri, sel_all], [pp])
                    k.mm(pp[:], ones_f[:], cum[:], False, True, [ones_f, cum], [pp])
                    k.stt("dve", posm_all[:, t, :], pp[:], 1.0, sel_all[:, t, :], ALU.add, ALU.mult, [pp, sel_all], [posm_all])
                    k.tt("dve", cum[:], cum[:], sel_all[:, t, :], ALU.add, [cum, sel_all], [cum])
                k.ts("dve", posm_all[:, t0:t0 + ntl, :], posm_all[:, t0:t0 + ntl, :], -1.0, None, ALU.add, None,
                     [posm_all], [posm_all])
            tl = NT if len(sets) > 1 else 32
            k.cp("dve", nv[:, 0:tl, :, 0:2], tok[:, 0:tl, :].unsqueeze(2).broadcast_to([128, tl, NEXP, 2]), [tok], [nv])
            k.cp("dve", nv[:, 0:tl, :, 2], aff_all[:, 0:tl, :], [aff_all], [nv])
            k.tt("dve", r1[:, 0:tl, :], aff_all[:, 0:tl, :], nv[:, 0:tl, :, 2], ALU.subtract, [aff_all, nv], [r1])
            k.cp("dve", nv[:, 0:tl, :, 3], r1[:, 0:tl, :], [r1], [nv])
            k.tt("dve", r2[:, 0:tl, :], r1[:, 0:tl, :], nv[:, 0:tl, :, 3], ALU.subtract, [r1, nv], [r2])
            k.cp("dve", nv[:, 0:tl, :, 4], r2[:, 0:tl, :], [r2], [nv])
            k.barrier()

    def phase_dispatch(l, sets):
        with ExitStack() as st:
            iota = k.sbuf(st, "iota_s", [128, 512], F32)
            k.dma("sp", iota[:], I.iota_slots, writes=[iota])
            ohs = [k.sbuf(st, f"oh{i}", [128, 512], BF16) for i in range(4)]
            igp = [k.psum(st, f"igp{i}", [128, 4, 8], F32) for i in range(2)]
            igs = k.sbuf(st, "igs", [128, 4, 8], F32)
            idxf = k.sbuf(st, "idxf", [128, 4], F32)
            g2 = k.sbuf(st, "g2", [128, 4], F32)
            Xs = [k.sbuf(st, f"Xs{i}", [128, 4, D], BF16) for i in range(2)]
            tpp = [k.psum(st, f"dtp{i}", [128, 512], BF16) for i in range(2)]
            XTs = [k.sbuf(st, f"XTs{i}", [128, 8, 512], BF16) for i in range(2)]
            oi = 0
            jobs = [(e, st_) for e in range(NEXP) for st_ in sets]
            idxv = [Buf(idx_all.t) for _ in jobs]
            oi_ = [0]

            def part1(ji):
                e, (t0, ntl, cap, c0) = jobs[ji]
                nsc = (cap + 127) // 128
                ig, X = igp[ji % 2], Xs[ji % 2]
                for ti in range(ntl):
                    t = t0 + ti
                    oh = ohs[oi_[0] % 4]
                    oi_[0] += 1
                    k.ts("dve", oh[:, 0:cap], iota[:, 0:cap], posm_all[:, t, e:e + 1], None,
                         ALU.is_equal, None, [iota, posm_all], [oh])
                    for sc in range(nsc):
                        sl = min(128, cap - sc * 128)
                        k.mm(ig[0:sl, sc, 0:5], oh[:, sc * 128:sc * 128 + sl], nv[:, t, e, :],
                             ti == 0 and sc == 0, ti == ntl - 1, [oh, nv], [ig], inc=(sc == nsc - 1),
                             skip_group_check=True)
                sl0 = min(128, cap)
                k.cp("dve", igs[0:sl0, 0:nsc, 0:5], ig[0:sl0, 0:nsc, 0:5], [ig], [igs])
                k.stt("dve", idxf[0:sl0, 0:nsc], igs[0:sl0, 0:nsc, 0], 64.0, igs[0:sl0, 0:nsc, 1], ALU.mult, ALU.add,
                      [igs], [idxf])
                k.cp("dve", idx_all[0:sl0, e, c0:c0 + nsc], idxf[0:sl0, 0:nsc], [idxf], [idxv[ji]])
                k.tt("dve", g2[0:sl0, 0:nsc], igs[0:sl0, 0:nsc, 2], igs[0:sl0, 0:nsc, 3], ALU.add, [igs], [g2])
                k.tt("dve", gate_all[0:sl0, e, c0:c0 + nsc], g2[0:sl0, 0:nsc], igs[0:sl0, 0:nsc, 4], ALU.add,
                     [g2, igs], [gate_all])
                for sc in range(nsc):
                    sl = min(128, cap - sc * 128)
                    k.dma("pool", None, None, reads=[idxv[ji]], writes=[X], fn=lambda: nc.gpsimd.indirect_dma_start(
                        out=X[0:sl, sc, :], out_offset=None, in_=hbf,
                        in_offset=bass.IndirectOffsetOnAxis(ap=idx_all[0:sl, e, c0 + sc:c0 + sc + 1], axis=0),
                        bounds_check=breg, oob_is_err=False))

            def part2(ji):
                e, (t0, ntl, cap, c0) = jobs[ji]
                nsc = (cap + 127) // 128
                X, XT = Xs[ji % 2], XTs[ji % 2]
                for dc in range(8):
                    tp = tpp[dc % 2]
                    for sc in range(nsc):
                        sl = min(128, cap - sc * 128)
                        k.tr(tp[:, sc * 128:sc * 128 + sl], X[0:sl, sc, dc * 128:(dc + 1) * 128], ident_bf[0:sl, 0:sl],
                             [X, ident_bf], [tp])
                    k.cp("act", XT[:, dc, 0:cap], tp[:, 0:cap], [tp], [XT])
                col0 = 0 if c0 == 0 else 512
                k.dma("sp", xtd[e, :, :, col0:col0 + cap], XT[:, :, 0:cap], reads=[XT])

            part1(0)
            for ji in range(len(jobs)):
                if ji + 1 < len(jobs):
                    part1(ji + 1)
                part2(ji)
            k.barrier()

    def phase_ffn(l, sets):
        with ExitStack() as st:
            g5 = [load_bcast(st, f"gate5_{r}", modvec(l, r, 5)) for r in range(len(sets))]
            xts = [k.sbuf(st, f"fxt{i}", [128, 8, 544], BF16) for i in range(2)]
            AT = k.sbuf(st, "AT", [128, 16, 544], BF16)
            stg = [k.sbuf(st, f"wstg{i}", [128, 2048], F32) for i in range(5)]
            wgb = [k.sbuf(st, f"wgb{i}", [128, 8, 512], BF16) for i in range(2)]
            wub = [k.sbuf(st, f"wub{i}", [128, 8, 512], BF16) for i in range(2)]
            wdb = k.sbuf(st, "wdb", [128, 16, D], BF16)
            sgs = [k.sbuf(st, f"sg{i}", [128, 512], F32) for i in range(2)]
            ysb = [k.sbuf(st, f"ysb{i}", [128, D], F32) for i in range(10)]
            gps = [k.psum(st, f"gps{i}", [128, 512], F32) for i in range(2)]
            ups = [k.psum(st, f"ups{i}", [128, 512], F32) for i in range(2)]
            cps_ = k.psum(st, "fcps", [128, 2, 32], F32)
            yps = [k.psum(st, f"yps{i}", [128, 512], F32) for i in range(2)]
            nctx = 32 if len(sets) > 1 else 0
            ncols = 512 + nctx
            si = [0]
            pending = []
            prev_sc_toks = []

            def stage_cast(src_ap, dst_ap, dstbuf, shape3):
                sg_ = stg[si[0] % 5]
                si[0] += 1
                v = sg_[:].rearrange("p (a b) -> p a b", a=shape3[0])
                k.dma("sp", v, src_ap, writes=[sg_])
                k.cp("dve" if si[0] % 2 else "act", dst_ap, v, [sg_], [dstbuf])

            def flush():
                nonlocal prev_sc_toks
                if not pending:
                    return
                for tok in prev_sc_toks:
                    k.wait("pool", tok)
                toks = []
                for (fn_, rd) in pending:
                    toks.append(k.dma("pool", None, None, reads=rd, fn=fn_))
                prev_sc_toks = toks
                pending.clear()

            it = 0
            yi = 0
            for e in range(NEXP):
                xt = xts[e % 2]
                k.dma("act", xt[:, :, 0:ncols], xtd[e, :, :, 0:ncols], writes=[xt])
                wgv = I.w_gate[l, e].rearrange("(c p) f -> p c f", p=128)
                wuv = I.w_up[l, e].rearrange("(c p) f -> p c f", p=128)
                wdv = I.w_down[l, e].rearrange("(c p) d -> p c d", p=128)
                for fb in range(4):
                    wg_, wu_ = wgb[fb % 2], wub[fb % 2]
                    for ch in range(2):
                        stage_cast(wgv[:, ch * 4:(ch + 1) * 4, fb * 512:(fb + 1) * 512], wg_[:, ch * 4:(ch + 1) * 4, :], wg_, (4, 512))
                    for ch in range(2):
                        stage_cast(wuv[:, ch * 4:(ch + 1) * 4, fb * 512:(fb + 1) * 512], wu_[:, ch * 4:(ch + 1) * 4, :], wu_, (4, 512))
                    for u in (2 * fb, 2 * fb + 1):
                        stage_cast(wdv[:, 2 * u:2 * u + 2, :], wdb[:, 2 * u:2 * u + 2, :], wdb, (2, 1024))
                    if fb == 1:
                        flush()
                    for fc in range(4):
                        f = fb * 4 + fc
                        gp, up = gps[it % 2], ups[it % 2]
                        sg = sgs[it % 2]
                        it += 1
                        for dc in range(8):
                            k.mm(gp[:], wg_[:, dc, fc * 128:(fc + 1) * 128], xt[:, dc, 0:512], dc == 0, dc == 7, [wg_, xt], [gp])
                        for dc in range(8):
                            k.mm(up[:], wu_[:, dc, fc * 128:(fc + 1) * 128], xt[:, dc, 0:512], dc == 0, dc == 7, [wu_, xt], [up])
                        k.actf(sg[:], gp[:], ACT.Silu, [gp], [sg])
                        k.tt("dve", AT[:, f, 0:512], sg[:], up[:], ALU.mult, [sg, up], [AT])
                        if nctx:
                            for dc in range(8):
                                k.mm(cps_[:, 0, :], wg_[:, dc, fc * 128:(fc + 1) * 128], xt[:, dc, 512:544], dc == 0, dc == 7,
                                     [wg_, xt], [cps_])
                            for dc in range(8):
                                k.mm(cps_[:, 1, :], wu_[:, dc, fc * 128:(fc + 1) * 128], xt[:, dc, 512:544], dc == 0, dc == 7,
                                     [wu_, xt], [cps_])
                            k.actf(sg[:, 0:32], cps_[:, 0, :], ACT.Silu, [cps_], [sg])
                            k.tt("dve", AT[:, f, 512:544], sg[:, 0:32], cps_[:, 1, :], ALU.mult, [sg, cps_], [AT])
                for si_, (t0, ntl, cap, c0) in enumerate(sets):
                    nsc = (cap + 127) // 128
                    acol = 0 if c0 == 0 else 512
                    for sc in range(nsc):
                        sl = min(128, cap - sc * 128)
                        ys = ysb[yi % 10]
                        yi += 1
                        for hf in range(2):
                            yp = yps[hf]
                            for f in range(16):
                                k.mm(yp[0:sl, :], AT[:, f, acol + sc * 128:acol + sc * 128 + sl], wdb[:, f, hf * 512:(hf + 1) * 512],
                                     f == 0, f == 15, [AT, wdb], [yp])
                            k.stt("dve", ys[0:sl, hf * 512:(hf + 1) * 512], yp[0:sl, :], gate_all[0:sl, e, c0 + sc:c0 + sc + 1],
                                  g5[si_][0:sl, hf * 512:(hf + 1) * 512], ALU.mult, ALU.mult, [yp, gate_all, g5[si_]], [ys])

                        def mk(ys=ys, sl=sl, e=e, col=c0 + sc):
                            return lambda: nc.gpsimd.indirect_dma_start(
                                out=xres, out_offset=bass.IndirectOffsetOnAxis(ap=idx_all[0:sl, e, col:col + 1], axis=0),
                                in_=ys[0:sl, :], in_offset=None, compute_op=ALU.add, bounds_check=breg)
                        pending.append((mk(), [ys, idx_all]))
            flush()
            k.barrier()

    def phase_final():
        with ExitStack() as st:
            gf = load_bcast(st, "gfin_b", I.g_final)
            xts = [k.sbuf(st, f"fnx{i}", [128, D], F32) for i in range(2)]
            junk = k.sbuf(st, "fnjunk", [128, D], BF16)
            ss = k.sbuf(st, "fnss", [128, 2], F32)
            rstd = k.sbuf(st, "fnrstd", [128, 1], F32)
            ys = [k.sbuf(st, f"fny{i}", [128, D], F32) for i in range(2)]
            for t in range(32):
                xt, y = xts[t % 2], ys[t % 2]
                k.dma("sp", xt[:], xres[t * 128:(t + 1) * 128, :], writes=[xt])
                rms_rstd(xt[:], ss, rstd, junk, D, [xt])
                k.stt("dve", y[:], xt[:], rstd[:, 0:1], gf[:], ALU.mult, ALU.mult, [xt, rstd, gf], [y])
                k.dma("act", out_d[t * 128:(t + 1) * 128, :], y[:], reads=[y])
            k.barrier()

    for l in range(layers):
        last = l == DEPTH - 1
        if "proj" not in skip:
            phase_proj(l)
        if stop_after == f"proj{l}":
            return finish(nc, k, es)
        if "attna" not in skip:
            phase_attn_a(l, not last)
        if stop_after == f"attna{l}":
            return finish(nc, k, es)
        if "fourier" not in skip:
            phase_fourier(l, not last)
        if stop_after == f"fourier{l}":
            return finish(nc, k, es)
        if "attnc" not in skip:
            phase_attn_c(l, not last)
        if stop_after == f"attnc{l}":
            return finish(nc, k, es)
        if "merge" not in skip:
            phase_merge(l, 32 if last else NT)
        if stop_after == f"merge{l}":
            if debug:
                k.dma("sp", dbg_aff, aff_all[:], reads=[aff_all])
                k.dma("sp", dbg_x.rearrange("(p a) d -> p (a d)", p=128), xres.rearrange("(p a) d -> p (a d)", p=128))
            return finish(nc, k, es)
        sets = [LAT] if last else [LAT, CTXS]
        if "moe" not in skip:
            phase_route(l, sets)
            if stop_after == f"route{l}":
                if debug:
                    k.dma("sp", dbg_pos, posm_all[:], reads=[posm_all])
                return finish(nc, k, es)
            phase_dispatch(l, sets)
            if stop_after == f"disp{l}":
                return finish(nc, k, es)
            phase_ffn(l, sets)
        if stop_after == f"moe{l}":
            if debug:
                k.dma("sp", dbg_x.rearrange("(p a) d -> p (a d)", p=128), xres.rearrange("(p a) d -> p (a d)", p=128))
                k.dma("sp", dbg_aff, aff_all[:], reads=[aff_all])
                k.dma("sp", dbg_pos, posm_all[:], reads=[posm_all])
                k.dma("sp", dbg_idx, idx_all[:], reads=[idx_all])
                k.dma("sp", dbg_gate, gate_all[:], reads=[gate_all])
            return finish(nc, k, es)
    if "final" not in skip:
        phase_final()
    return finish(nc, k, es)


def finish(nc, k, es):
    k.barrier()
    es.close()
    return nc


def make_in_maps(inputs, ncores=8):
    cst = host_consts()
    shared = {}
    for nm in ("c_ctx", "w_ada", "b_ada", "g_mix", "g_ffn", "w_in", "g_q", "g_k", "w_out", "w_router",
               "w_gate", "w_up", "w_down", "g_final"):
        shared[nm] = np.ascontiguousarray(inputs[nm], dtype=np.float32)
    shared["g_out"] = np.ascontiguousarray(
        np.concatenate([inputs["g_out_a"], inputs["g_out_b"], inputs["g_out_c"]], axis=-1), dtype=np.float32)
    shared["na_bias"] = na_bias_layout(inputs["rel_bias"])
    shared.update(cst)
    maps = []
    for b in range(ncores):
        m = dict(shared)
        m["x"] = np.ascontiguousarray(inputs["x"][b], dtype=np.float32)
        m["c"] = np.ascontiguousarray(inputs["c"][b], dtype=np.float32)
        m["ctx"] = np.ascontiguousarray(inputs["ctx"][b], dtype=np.float32)
        maps.append(m)
    return maps


def kernel(**inputs):
    nc = build_program()
    maps = make_in_maps(inputs, 8)
    maps = [{n: m[n] for n in nc._used_inputs} for m in maps]
    res = run_bass_kernel_spmd(nc, maps, core_ids=list(range(8)))
    return np.stack([np.asarray(r["out"], dtype=np.float32) for r in res.results], axis=0)
```
